# Optimizing a Trainium2 kernel written in Bass

```python
import jax, jax.numpy as jnp
from jax import lax
import numpy as np

D_MODEL = 1024
BATCH = 8
SEQ = 4096
DEPTH = 2

HEAD_DIM = 64
ROPE_THETA = 10000.0
NORM_EPS = 1e-6
BLOCK = 128

A_HEADS = 6
IDX_HEADS = 8
IDX_DIM = 64
TOPK_MAX = 256

B_GROUPS = ((128, 1), (512, 4), (2048, 16))
B_HEADS_PER_GROUP = 4
B_HEADS = B_HEADS_PER_GROUP * len(B_GROUPS)

C_HEADS = 8
C_KV_HEADS = 2
C_WINDOW = 128

N_BRANCHES = 3
D_FF = 4 * D_MODEL

IN_WIDTHS = (A_HEADS * HEAD_DIM, HEAD_DIM, HEAD_DIM,
             IDX_HEADS * IDX_DIM, IDX_DIM, IDX_HEADS,
             B_HEADS * HEAD_DIM, B_HEADS * HEAD_DIM, B_HEADS * HEAD_DIM,
             C_HEADS * HEAD_DIM, C_KV_HEADS * HEAD_DIM, C_KV_HEADS * HEAD_DIM,
             N_BRANCHES * D_MODEL)
IN_WIDTH = sum(IN_WIDTHS)

kernel_name = "hybrid_gated_dsa_dilated_sinkswa"


def rmsnorm(x, g):
    xf = x.astype(jnp.float32)
    y = xf * lax.rsqrt(jnp.mean(xf * xf, axis=-1, keepdims=True) + NORM_EPS)
    return (y * g.astype(jnp.float32)).astype(x.dtype)


def rope(x, pos):
    half = x.shape[-1] // 2
    inv = ROPE_THETA ** (-jnp.arange(half, dtype=jnp.float32) / half)
    ang = pos.astype(jnp.float32)[..., None] * inv
    cos = jnp.cos(ang)[:, :, None, :].astype(x.dtype)
    sin = jnp.sin(ang)[:, :, None, :].astype(x.dtype)
    x1, x2 = x[..., :half], x[..., half:]
    return jnp.concatenate([x1 * cos - x2 * sin, x2 * cos + x1 * sin], axis=-1)


def split_columns(z):
    parts, off = [], 0
    for w in IN_WIDTHS:
        parts.append(z[..., off:off + w])
        off += w
    return parts


def over_query_blocks(fn, seq_len):
    starts = jnp.arange(seq_len // BLOCK, dtype=jnp.int32) * BLOCK
    out = lax.map(fn, starts)
    out = jnp.moveaxis(out, 0, 1)
    return out.reshape(out.shape[0], seq_len, *out.shape[3:])


def dsa_attention(q, k, v, iq, ik, iw):
    L = q.shape[1]
    topk = min(TOPK_MAX, L // 4)
    scale = HEAD_DIM ** -0.5
    idx_scale = IDX_DIM ** -0.5
    s_all = jnp.arange(L)

    def block(t0):
        t = t0 + jnp.arange(BLOCK)
        qb = lax.dynamic_slice_in_dim(q, t0, BLOCK, 1)
        iqb = lax.dynamic_slice_in_dim(iq, t0, BLOCK, 1)
        iwb = lax.dynamic_slice_in_dim(iw, t0, BLOCK, 1).astype(jnp.float32)
        rel = jax.nn.relu(jnp.einsum('bqhd,bsd->bqhs', iqb, ik).astype(jnp.float32) * idx_scale)
        score = jnp.einsum('bqh,bqhs->bqs', iwb, rel)
        causal = s_all[None, :] <= t[:, None]
        score = jnp.where(causal[None], score, -jnp.inf)
        _, idx = lax.top_k(score, topk)
        ks = jax.vmap(lambda kk, ii: kk[ii])(k, idx)
        vs = jax.vmap(lambda vv, ii: vv[ii])(v, idx)
        valid = idx <= t[None, :, None]
        logits = jnp.einsum('bqhd,bqkd->bqhk', qb, ks).astype(jnp.float32) * scale
        logits = jnp.where(valid[:, :, None, :], logits, -jnp.inf)
        p = jax.nn.softmax(logits, axis=-1)
        return jnp.einsum('bqhk,bqkd->bqhd', p.astype(vs.dtype), vs)

    return over_query_blocks(block, L)


def dilated_attention(q, k, v):
    L = q.shape[1]
    scale = HEAD_DIM ** -0.5
    hg = B_HEADS_PER_GROUP
    qs = [q[:, :, g * hg:(g + 1) * hg] for g in range(len(B_GROUPS))]
    kss = [k[:, :, g * hg:(g + 1) * hg] for g in range(len(B_GROUPS))]
    vss = [v[:, :, g * hg:(g + 1) * hg] for g in range(len(B_GROUPS))]

    def block(t0):
        t = t0 + jnp.arange(BLOCK)
        outs, lses = [], []
        for g, (window, dil) in enumerate(B_GROUPS):
            qb = lax.dynamic_slice_in_dim(qs[g], t0, BLOCK, 1)
            pos = t[:, None] - dil * jnp.arange(window // dil + 1)[None, :]
            valid = pos >= 0
            pos_c = jnp.maximum(pos, 0)
            kg = kss[g][:, pos_c]
            vg = vss[g][:, pos_c]
            logits = jnp.einsum('bqhd,bqkhd->bqhk', qb, kg).astype(jnp.float32) * scale
            logits = jnp.where(valid[None, :, None, :], logits, -jnp.inf)
            m = jnp.max(logits, axis=-1, keepdims=True)
            e = jnp.exp(logits - m)
            den = jnp.sum(e, axis=-1)
            o = jnp.einsum('bqhk,bqkhd->bqhd', (e / den[..., None]).astype(vg.dtype), vg)
            outs.append(o)
            lses.append(m[..., 0] + jnp.log(den))
        alpha = jax.nn.softmax(jnp.stack(lses, 0), axis=0)
        return jnp.einsum('gbqh,gbqhd->bqhd', alpha.astype(outs[0].dtype), jnp.stack(outs, 0))

    return over_query_blocks(block, L)


def sink_window_attention(q, k, v, sinks):
    Bsz, L = q.shape[0], q.shape[1]
    grp = C_HEADS // C_KV_HEADS
    scale = HEAD_DIM ** -0.5
    pad = ((0, 0), (BLOCK, 0), (0, 0), (0, 0))
    kp = jnp.pad(k, pad)
    vp = jnp.pad(v, pad)
    qg = q.reshape(Bsz, L, C_KV_HEADS, grp, HEAD_DIM)
    sink = sinks.astype(jnp.float32).reshape(C_KV_HEADS, grp)[:, :, None, None]

    def block(t0):
        t = t0 + jnp.arange(BLOCK)
        s = t0 - BLOCK + jnp.arange(2 * BLOCK)
        qb = lax.dynamic_slice_in_dim(qg, t0, BLOCK, 1)
        kb = lax.dynamic_slice_in_dim(kp, t0, 2 * BLOCK, 1)
        vb = lax.dynamic_slice_in_dim(vp, t0, 2 * BLOCK, 1)
        mask = (s[None, :] <= t[:, None]) & (s[None, :] > t[:, None] - C_WINDOW) & (s[None, :] >= 0)
        logits = jnp.einsum('bqkgd,bskd->bkgqs', qb, kb).astype(jnp.float32) * scale
        logits = jnp.where(mask, logits, -jnp.inf)
        m = jnp.maximum(jnp.max(logits, axis=-1, keepdims=True), sink)
        e = jnp.exp(logits - m)
        den = jnp.sum(e, axis=-1, keepdims=True) + jnp.exp(sink - m)
        p = (e / den).astype(vb.dtype)
        o = jnp.einsum('bkgqs,bskd->bqkgd', p, vb)
        return o.reshape(Bsz, BLOCK, C_HEADS, HEAD_DIM)

    return over_query_blocks(block, L)


def hybrid_mixer(u, pos, w_in, idx_k_norm, sinks, w_a, w_b, w_c, w_o):
    Bsz, L, _ = u.shape
    (aq, ak, av, iq, ik, iw, bq, bk, bv, cq, ck, cv, gates) = split_columns(u @ w_in)
    qa = rope(aq.reshape(Bsz, L, A_HEADS, HEAD_DIM), pos)
    ka = rope(ak[:, :, None, :], pos)[:, :, 0]
    iqr = rope(iq.reshape(Bsz, L, IDX_HEADS, IDX_DIM), pos)
    ikr = rope(rmsnorm(ik, idx_k_norm)[:, :, None, :], pos)[:, :, 0]
    o_a = dsa_attention(qa, ka, av, iqr, ikr, iw * IDX_HEADS ** -0.5)
    o_b = dilated_attention(rope(bq.reshape(Bsz, L, B_HEADS, HEAD_DIM), pos),
                            rope(bk.reshape(Bsz, L, B_HEADS, HEAD_DIM), pos),
                            bv.reshape(Bsz, L, B_HEADS, HEAD_DIM))
    o_c = sink_window_attention(rope(cq.reshape(Bsz, L, C_HEADS, HEAD_DIM), pos),
                                rope(ck.reshape(Bsz, L, C_KV_HEADS, HEAD_DIM), pos),
                                cv.reshape(Bsz, L, C_KV_HEADS, HEAD_DIM), sinks)
    g = jax.nn.sigmoid(gates.reshape(Bsz, L, N_BRANCHES, D_MODEL))
    merged = (g[:, :, 0] * (o_a.reshape(Bsz, L, -1) @ w_a)
              + g[:, :, 1] * (o_b.reshape(Bsz, L, -1) @ w_b)
              + g[:, :, 2] * (o_c.reshape(Bsz, L, -1) @ w_c))
    return merged @ w_o


def setup_inputs(seed: int = 0) -> dict:
    key = jax.random.key(seed)
    ks = jax.random.split(key, 15)

    def normal(k, shape, scale):
        return jax.random.normal(k, shape, jnp.float32) * scale

    def gain(k, shape):
        return 1.0 + 0.05 * jax.random.normal(k, shape, jnp.float32)

    x = normal(ks[0], (BATCH, SEQ, D_MODEL), 1.0)
    positions = jnp.tile(jnp.arange(SEQ, dtype=jnp.int32)[None, :], (BATCH, 1))
    return {
        "x": x,
        "positions": positions,
        "attn_norm": gain(ks[1], (DEPTH, D_MODEL)),
        "w_in": normal(ks[2], (DEPTH, D_MODEL, IN_WIDTH), D_MODEL ** -0.5),
        "idx_k_norm": gain(ks[3], (DEPTH, IDX_DIM)),
        "sinks": normal(ks[4], (DEPTH, C_HEADS), 0.5),
        "w_a": normal(ks[5], (DEPTH, A_HEADS * HEAD_DIM, D_MODEL), (A_HEADS * HEAD_DIM) ** -0.5),
        "w_b": normal(ks[6], (DEPTH, B_HEADS_PER_GROUP * HEAD_DIM, D_MODEL), (B_HEADS_PER_GROUP * HEAD_DIM) ** -0.5),
        "w_c": normal(ks[7], (DEPTH, C_HEADS * HEAD_DIM, D_MODEL), (C_HEADS * HEAD_DIM) ** -0.5),
        "w_o": normal(ks[8], (DEPTH, D_MODEL, D_MODEL), D_MODEL ** -0.5),
        "mlp_norm": gain(ks[9], (DEPTH, D_MODEL)),
        "w_up": normal(ks[10], (DEPTH, D_MODEL, D_FF), D_MODEL ** -0.5),
        "w_down": normal(ks[11], (DEPTH, D_FF, D_MODEL), D_FF ** -0.5),
        "final_norm": gain(ks[12], (D_MODEL,)),
    }


def reference(x, positions, attn_norm, w_in, idx_k_norm, sinks, w_a, w_b, w_c, w_o,
              mlp_norm, w_up, w_down, final_norm):
    h = x
    for l in range(DEPTH):
        u = rmsnorm(h, attn_norm[l])
        h = h + hybrid_mixer(u, positions, w_in[l], idx_k_norm[l], sinks[l],
                             w_a[l], w_b[l], w_c[l], w_o[l])
        u = rmsnorm(h, mlp_norm[l])
        h = h + jnp.square(jax.nn.relu(u @ w_up[l])) @ w_down[l]
    return rmsnorm(h, final_norm)
```

```python
import math
import os
from contextlib import ExitStack

import numpy as np
import concourse.bass as bass
import concourse.mybir as mybir
from concourse.bass_utils import run_bass_kernel_spmd

F32 = mybir.dt.float32
BF16 = mybir.dt.bfloat16
I32 = mybir.dt.int32
AF = mybir.ActivationFunctionType
ALU = mybir.AluOpType

L = 4096
D = 1024
DEPTH = 2
NEG = -30000.0
EPS = 1e-6
N_BISECT = 14

C_AQ, C_AK, C_AV, C_IQ, C_IK, C_IW = 0, 384, 448, 512, 1024, 1088
C_BQ, C_BK, C_BV, C_CQ, C_CK, C_CV, C_G = 1096, 1864, 2632, 3400, 3912, 4040, 4168

R_AQ, R_AKIK, R_IQ, R_BQ, R_BK, R_CQ, R_CK = 0, 384, 512, 1024, 1792, 2560, 3072
N_ROPED = 3200


class Tok:
    __slots__ = ("w", "rs", "rd")

    def __init__(self):
        self.w = None
        self.rs = {}
        self.rd = []


def toks(n):
    return [Tok() for _ in range(n)]


class _Op:
    __slots__ = ("eng", "fn", "waits", "sem", "val", "inc", "is_dma")


class Prog:
    ENGS = ("pe", "act", "dve", "pool", "sp")

    def __init__(self, nc, ndma=40):
        self.nc = nc
        self.ops = {e: [] for e in self.ENGS}
        self.cnt = {e: 0 for e in self.ENGS}
        self.waited = {}
        self.ndma = ndma
        self.dma_uses = [0] * ndma
        self.dma_rr = 0
        self.nops = 0

    def _wait(self, X, sem, val):
        key = (X.eng, sem)
        if self.waited.get(key, 0) >= val:
            return
        self.waited[key] = val
        X.waits.append((sem, val))

    def op(self, eng, fn, reads=(), writes=(), dma=False):
        X = _Op()
        X.eng = eng
        X.fn = fn
        X.waits = []
        X.is_dma = dma
        deps = []
        for t in reads:
            if t.w is not None:
                deps.append((t.w, 0))
        for t in writes:
            if t.w is not None:
                deps.append((t.w, 1))
            for r in t.rs.values():
                deps.append((r, 1))
            for r in t.rd:
                deps.append((r, 1))
        if dma:
            j = self.dma_rr
            self.dma_rr = (j + 1) % self.ndma
            k = self.dma_uses[j]
            self.dma_uses[j] += 1
            X.sem = ("d", j)
            X.val = 16 * (k + 1)
            X.inc = 16
            if k > 0:
                self._wait(X, ("d", j), 16 * k)
        else:
            self.cnt[eng] += 1
            X.sem = ("e", eng)
            X.val = self.cnt[eng]
            X.inc = 1
        for d, hz in deps:
            if d is X:
                continue
            if (not d.is_dma) and (not dma) and d.eng == eng and hz == 1:
                continue
            self._wait(X, d.sem, d.val)
        for t in reads:
            if dma:
                t.rd.append(X)
            else:
                t.rs[eng] = X
        for t in writes:
            t.w = X
            t.rs = {}
            t.rd = []
        self.ops[eng].append(X)
        self.nops += 1
        return X

    def barrier(self):
        snap = dict(self.cnt)
        sd = list(self.dma_uses)
        for e in self.ENGS:
            X = _Op()
            X.eng = e
            X.fn = None
            X.waits = []
            X.is_dma = False
            X.sem = None
            X.val = 0
            X.inc = 0
            for e2 in self.ENGS:
                if e2 != e and snap[e2] > 0:
                    self._wait(X, ("e", e2), snap[e2])
            for j in range(self.ndma):
                if sd[j] > 0:
                    self._wait(X, ("d", j), 16 * sd[j])
            self.ops[e].append(X)

    def emit(self):
        nc = self.nc
        with ExitStack() as es:
            sems = {}
            for e in self.ENGS:
                sems[("e", e)] = es.enter_context(nc.semaphore("s_" + e))
            for j in range(self.ndma):
                sems[("d", j)] = es.enter_context(nc.semaphore("d_%d" % j))
            block = es.enter_context(nc.Block())

            def run(ename):
                def body(eng):
                    for X in self.ops[ename]:
                        for (s, v) in X.waits:
                            eng.wait_ge(sems[s], v)
                        if X.fn is None:
                            continue
                        ins = X.fn(eng)
                        ins.then_inc(sems[X.sem], X.inc)
                return body

            block.tensor(run("pe"))
            block.scalar(run("act"))
            block.vector(run("dve"))
            block.gpsimd(run("pool"))
            block.sync(run("sp"))


def sap(t, off, dims, npart=128, pstart=0):
    fs = 1
    for s in list(t.shape)[1:]:
        fs *= int(s)
    return bass.AP(t, pstart * fs + off, [[fs, npart]] + [list(d) for d in dims])


class Builder:
    def __init__(self, dbg=None, stop_after=None, skip=()):
        self.skip = skip
        self.dbg = dbg or ()
        self.stop_after = stop_after
        nc = bass.Bass("TRN2", target_bir_lowering=False)
        self.nc = nc
        self.P = Prog(nc)
        self.outs = []

        def din(name, shape, dt=F32):
            return nc.dram_tensor(name, list(shape), dt, kind="ExternalInput").ap()

        self.x = din("x", [L, D])
        self.pos = din("pos", [L], I32)
        self.attn_norm = din("attn_norm", [DEPTH, D])
        self.w_in = din("w_in", [DEPTH, D, 7240])
        self.idx_k_norm = din("idx_k_norm", [DEPTH, 64])
        self.sinks = din("sinks", [DEPTH, 8])
        self.w_a = din("w_a", [DEPTH, 384, D])
        self.w_b = din("w_b", [DEPTH, 256, D])
        self.w_c = din("w_c", [DEPTH, 512, D])
        self.w_o = din("w_o", [DEPTH, D, D])
        self.mlp_norm = din("mlp_norm", [DEPTH, D])
        self.w_up = din("w_up", [DEPTH, D, 4 * D])
        self.w_down = din("w_down", [DEPTH, 4 * D, D])
        self.final_norm = din("final_norm", [D])
        self.cvec = din("cvec", [128, 4])
        self.cmat = din("cmat", [128, 5, 128])
        self.masks = din("masks", [128, 9, 128])

        self.out = nc.dram_tensor("out", [L, D], F32, kind="ExternalOutput").ap()

        self.cos_d = self.scr("cos_d", [128, L], F32)
        self.sin_d = self.scr("sin_d", [128, L], F32)
        self.hT_d = self.scr("hT_d", [8, 128, L], F32)
        self.uT_d = self.scr("uT_d", [8, 128, L], BF16)
        self.zT_d = self.scr("zT_d", [N_ROPED, L], BF16)
        self.bv_d = self.scr("bv_d", [L, 12, 65], BF16)
        self.cv_d = self.scr("cv_d", [L, 2, 65], BF16)
        self.av_d = self.scr("av_d", [L, 65], BF16)
        self.iw_d = self.scr("iw_d", [L, 8], F32)
        self.oaT_d = self.scr("oaT_d", [64, 6, L], BF16)
        self.obT_d = self.scr("obT_d", [64, 4, L], BF16)
        self.ocT_d = self.scr("ocT_d", [64, 8, L], BF16)

    def scr(self, name, shape, dt):
        kind = "ExternalOutput" if name in self.dbg else "Internal"
        t = self.nc.dram_tensor(name, list(shape), dt, kind=kind)
        if name in self.dbg:
            self.outs.append(name)
        return t.ap()

    def sb(self, es, name, shape, dt):
        self._sbn = getattr(self, "_sbn", 0) + 1
        return es.enter_context(self.nc.sbuf_tensor("%s_%d" % (name, self._sbn), list(shape), dt))

    def build(self):
        nc, P = self.nc, self.P
        with ExitStack() as es:
            self.ps = [es.enter_context(nc.psum_tensor("ps%d" % i, [128, 512], F32)) for i in range(8)]
            self.pst = toks(8)
            self.cvec_sb = self.sb(es, "cvec_sb", [128, 4], F32)
            self.cmat_sb = self.sb(es, "cmat_sb", [128, 5, 128], F32)
            self.ident_f = self.cmat_sb[:, 0, :]
            self.cb = self.sb(es, "cb", [128, 5, 128], BF16)
            self.ones_bf = self.sb(es, "ones_bf", [128, 128], BF16)
            self.ones_f = self.sb(es, "ones_f", [128, 128], F32)
            self.g_attn = self.sb(es, "g_attn", [128, DEPTH, 8], F32)
            self.g_mlp = self.sb(es, "g_mlp", [128, DEPTH, 8], F32)
            self.g_fin = self.sb(es, "g_fin", [128, 8], F32)
            self.gk = self.sb(es, "gk", [128, DEPTH, 2], F32)
            self.t_const = Tok()
            self.eps_t = self.sb(es, "eps_t", [128, 1], F32)
            P.op("dve", lambda e: e.memset(self.eps_t[:], EPS), writes=[self.t_const])
            self.phase_const()
            P.barrier()
            self.dump("d_gk", self.gk[:], [128, DEPTH, 2], F32, self.t_const)
            self.dump("d_gattn", self.g_attn[:], [128, DEPTH, 8], F32, self.t_const)
            if self.stop_after == "const":
                return self.finish()
            self.attn_consts(es)
            P.barrier()
            for l in range(DEPTH):
                self.phase_A(l)
                P.barrier()
                if self.stop_after in (("A", l), ("A1", l), ("A2a", l)):
                    return self.finish()
                for nm, fn in (("aC", self.phase_attnC), ("aB", self.phase_attnB), ("aA", self.phase_attnA),
                               ("M", self.phase_M), ("F", self.phase_F)):
                    if nm not in self.skip:
                        fn(l)
                    if self.stop_after == (nm, l):
                        return self.finish()
            self.phase_O()
            return self.finish()

    def dump(self, name, src_ap, shape, dt, tok):
        if name not in self.dbg:
            return
        t = self.nc.dram_tensor(name, list(shape), dt, kind="ExternalOutput").ap()
        self.outs.append(name)
        self.P.op("sp", lambda e: e.dma_start(out=t, in_=src_ap), reads=[tok], dma=True)

    def finish(self):
        self.P.barrier()
        self.P.emit()
        return self.nc

    def phase_const(self):
        nc, P = self.nc, self.P
        tc_ = self.t_const
        with ExitStack() as es:
            cosT = self.sb(es, "cosT", [128, L], F32)
            sinS = self.sb(es, "sinS", [128, L], F32)
            posi = self.sb(es, "posi", [128, L], I32)
            ang = self.sb(es, "ang", [128, L], F32)
            kk = self.sb(es, "kk", [128, L], F32)
            ki = self.sb(es, "ki", [128, L], I32)
            t1 = Tok(); t2 = Tok(); t3 = Tok(); t4 = Tok()
            P.op("sp", lambda e: e.dma_start(out=self.cvec_sb[:], in_=self.cvec), writes=[tc_], dma=True)
            P.op("sp", lambda e: e.dma_start(out=self.cmat_sb[:], in_=self.cmat), writes=[tc_], dma=True)
            P.op("sp", lambda e: e.dma_start(out=posi[:], in_=self.pos.partition_broadcast(128)), writes=[t1], dma=True)
            for (dst, src) in ((self.g_attn, self.attn_norm), (self.g_mlp, self.mlp_norm)):
                P.op("sp", lambda e, dst=dst, src=src: e.dma_start(
                    out=dst[:], in_=src.rearrange("l (c p) -> p l c", p=128),
                    allow_slow_non_contiguous=True), writes=[tc_], dma=True)
            P.op("sp", lambda e: e.dma_start(out=self.g_fin[:], in_=self.final_norm.rearrange("(c p) -> p c", p=128),
                                             allow_slow_non_contiguous=True), writes=[tc_], dma=True)
            P.op("dve", lambda e: e.memset(self.gk[:], 1.0), writes=[tc_])
            for l in range(DEPTH):
                src = self.idx_k_norm[l]
                P.op("sp", lambda e, l=l, src=src: e.dma_start(
                    out=self.gk[64:128, l, 0:1], in_=src.rearrange("(p o) -> p o", o=1),
                    allow_slow_non_contiguous=True), writes=[tc_], dma=True)
                P.op("sp", lambda e, l=l, src=src: e.dma_start(
                    out=self.gk[64:96, l, 1:2], in_=src[32:64].rearrange("(p o) -> p o", o=1),
                    allow_slow_non_contiguous=True), writes=[tc_], dma=True)
                P.op("sp", lambda e, l=l, src=src: e.dma_start(
                    out=self.gk[96:128, l, 1:2], in_=src[0:32].rearrange("(p o) -> p o", o=1),
                    allow_slow_non_contiguous=True), writes=[tc_], dma=True)
            P.op("dve", lambda e: e.memset(self.ones_bf[:], 1.0), writes=[tc_])
            P.op("dve", lambda e: e.memset(self.ones_f[:], 1.0), writes=[tc_])
            P.op("dve", lambda e: e.tensor_copy(out=self.cb[:], in_=self.cmat_sb[:]), reads=[tc_], writes=[tc_])
            P.op("dve", lambda e: e.tensor_copy(out=ang[:], in_=posi[:]), reads=[t1], writes=[t2])
            P.op("dve", lambda e: e.tensor_scalar(out=ang[:], in0=ang[:], scalar1=self.cvec_sb[:, 0:1], scalar2=None,
                                                  op0=ALU.mult), reads=[t2, tc_], writes=[t2])
            P.op("dve", lambda e: e.tensor_scalar(out=kk[:], in0=ang[:], scalar1=1.0 / (2 * math.pi), scalar2=0.5,
                                                  op0=ALU.mult, op1=ALU.add), reads=[t2], writes=[t3])
            P.op("dve", lambda e: e.tensor_copy(out=ki[:], in_=kk[:]), reads=[t3], writes=[t4])
            P.op("dve", lambda e: e.tensor_copy(out=kk[:], in_=ki[:]), reads=[t4], writes=[t3])
            C1 = 6.28125
            C2 = 2 * math.pi - C1
            P.op("dve", lambda e: e.scalar_tensor_tensor(out=ang[:], in0=kk[:], scalar=-C1, in1=ang[:],
                                                         op0=ALU.mult, op1=ALU.add), reads=[t3, t2], writes=[t2])
            P.op("dve", lambda e: e.scalar_tensor_tensor(out=ang[:], in0=kk[:], scalar=-C2, in1=ang[:],
                                                         op0=ALU.mult, op1=ALU.add), reads=[t3, t2], writes=[t2])
            P.op("dve", lambda e: e.tensor_scalar(out=kk[:], in0=ang[:], scalar1=-math.pi, scalar2=2 * math.pi,
                                                  op0=ALU.is_lt, op1=ALU.mult), reads=[t2], writes=[t3])
            P.op("dve", lambda e: e.tensor_tensor(out=ang[:], in0=ang[:], in1=kk[:], op=ALU.add),
                 reads=[t2, t3], writes=[t2])
            P.op("dve", lambda e: e.tensor_scalar(out=kk[:], in0=ang[:], scalar1=math.pi, scalar2=-2 * math.pi,
                                                  op0=ALU.is_gt, op1=ALU.mult), reads=[t2], writes=[t3])
            P.op("dve", lambda e: e.tensor_tensor(out=ang[:], in0=ang[:], in1=kk[:], op=ALU.add),
                 reads=[t2, t3], writes=[t2])
            P.op("dve", lambda e: e.tensor_scalar(out=ang[:], in0=ang[:], scalar1=-3.1415925, scalar2=3.1415925,
                                                  op0=ALU.max, op1=ALU.min), reads=[t2], writes=[t2])
            P.op("act", lambda e: e.activation(out=sinS[:], in_=ang[:], func=AF.Sin), reads=[t2], writes=[tc_])
            P.op("dve", lambda e: e.tensor_scalar(out=sinS[:], in0=sinS[:], scalar1=self.cvec_sb[:, 1:2],
                                                  scalar2=None, op0=ALU.mult), reads=[tc_], writes=[tc_])
            P.op("dve", lambda e: e.tensor_scalar(out=kk[:], in0=ang[:], scalar1=-1.0, scalar2=None,
                                                  op0=ALU.mult), reads=[t2], writes=[t3])
            P.op("dve", lambda e: e.tensor_tensor(out=kk[:], in0=kk[:], in1=ang[:], op=ALU.max),
                 reads=[t2, t3], writes=[t3])
            P.op("dve", lambda e: e.tensor_scalar(out=kk[:], in0=kk[:], scalar1=-1.0, scalar2=math.pi / 2,
                                                  op0=ALU.mult, op1=ALU.add), reads=[t3], writes=[t3])
            P.op("act", lambda e: e.activation(out=cosT[:], in_=kk[:], func=AF.Sin), reads=[t3], writes=[tc_])
            P.op("sp", lambda e: e.dma_start(out=self.cos_d, in_=cosT[:]), reads=[tc_], dma=True)
            P.op("sp", lambda e: e.dma_start(out=self.sin_d, in_=sinS[:]), reads=[tc_], dma=True)
            P.barrier()

    def load_w(self, dst, l_w_ap, col0, ncols, dcol0, tok):
        src = l_w_ap[:, col0:col0 + ncols].rearrange("(kc p) c -> p kc c", p=128)
        self.P.op("pool", lambda e: e.dma_start(out=dst[:, :, dcol0:dcol0 + ncols], in_=src),
                  writes=[tok], dma=True)

    def norm_chunk(self, es_names, hT, t_h, gcol, uT_out_fn, t_u, sq, t_sq, rs, t_rs, psb, l_tag):
        P = self.P
        P.op("act", lambda e: e.activation(out=sq[:], in_=hT[:], func=AF.Square), reads=[t_h], writes=[t_sq])
        for c in range(8):
            P.op("pe", lambda e, c=c: e.matmul(self.ps[psb][:], lhsT=self.ones_bf[:], rhs=sq[:, c, :],
                                               start=(c == 0), stop=(c == 7)),
                 reads=[t_sq, self.t_const], writes=[self.pst[psb]])
        P.op("act", lambda e: e.activation(out=rs[:], in_=self.ps[psb][:], func=AF.Ln, scale=1.0 / D, bias=self.eps_t[:, 0:1]),
             reads=[self.pst[psb], self.t_const], writes=[t_rs])
        P.op("act", lambda e: e.activation(out=rs[:], in_=rs[:], func=AF.Exp, scale=-0.5), reads=[t_rs], writes=[t_rs])
        for c in range(8):
            P.op("dve", lambda e, c=c: e.scalar_tensor_tensor(out=uT_out_fn(c), in0=hT[:, c, :], scalar=gcol(c),
                                                              in1=rs[:], op0=ALU.mult, op1=ALU.mult),
                 reads=[t_h, t_rs, self.t_const], writes=[t_u])

    def phase_A(self, l):
        nc, P = self.nc, self.P
        w_in = self.w_in[l]
        with ExitStack() as es:
            uT = self.sb(es, "uT", [128, 8, L], BF16)
            t_uT = toks(8)
            with ExitStack() as es1:
                hT = [self.sb(es1, "hT%d" % i, [128, 8, 512], F32) for i in range(2)]
                t_h = toks(2)
                sq = [self.sb(es1, "sq%d" % i, [128, 8, 512], BF16) for i in range(2)]
                t_sq = toks(2)
                rs = [self.sb(es1, "rs%d" % i, [128, 512], F32) for i in range(2)]
                t_rs = toks(2)
                if l == 0:
                    xt = [self.sb(es1, "xt%d" % i, [128, D], F32) for i in range(3)]
                    t_x = toks(3)
                xi = 0
                for tc in range(8):
                    b = tc % 2
                    if l == 0:
                        for j in range(4):
                            ti = tc * 4 + j
                            xb = xi % 3
                            xi += 1
                            P.op("sp", lambda e, xb=xb, ti=ti: e.dma_start(out=xt[xb][:], in_=self.x[ti * 128:(ti + 1) * 128, :]),
                                 writes=[t_x[xb]], dma=True)
                            for half in range(2):
                                pb = (2 * j + half) % 4
                                for q in range(4):
                                    c = half * 4 + q
                                    P.op("pe", lambda e, xb=xb, c=c, pb=pb, q=q: e.transpose(
                                        out=self.ps[pb][:, q * 128:(q + 1) * 128], in_=xt[xb][:, c * 128:(c + 1) * 128],
                                        identity=self.ident_f), reads=[t_x[xb], self.t_const], writes=[self.pst[pb]])
                                eng = "dve" if half == 0 else "act"
                                if eng == "dve":
                                    P.op("dve", lambda e, b=b, half=half, j=j, pb=pb: e.tensor_copy(
                                        out=hT[b][:, half * 4:half * 4 + 4, j * 128:(j + 1) * 128],
                                        in_=self.ps[pb][:].rearrange("p (q t) -> p q t", q=4)),
                                        reads=[self.pst[pb]], writes=[t_h[b]])
                                else:
                                    P.op("act", lambda e, b=b, half=half, j=j, pb=pb: e.activation(
                                        out=hT[b][:, half * 4:half * 4 + 4, j * 128:(j + 1) * 128],
                                        in_=self.ps[pb][:].rearrange("p (q t) -> p q t", q=4), func=AF.Copy),
                                        reads=[self.pst[pb]], writes=[t_h[b]])
                        P.op("sp", lambda e, b=b, tc=tc: e.dma_start(
                            out=self.hT_d[:, :, tc * 512:(tc + 1) * 512].rearrange("c p t -> p c t"), in_=hT[b][:]),
                            reads=[t_h[b]], dma=True)
                    else:
                        P.op("sp", lambda e, b=b, tc=tc: e.dma_start(
                            out=hT[b][:], in_=self.hT_d[:, :, tc * 512:(tc + 1) * 512].rearrange("c p t -> p c t")),
                            writes=[t_h[b]], dma=True)
                    self.norm_chunk(None, hT[b], t_h[b], lambda c: self.g_attn[:, l, c:c + 1],
                                    lambda c, tc=tc: uT[:, c, tc * 512:(tc + 1) * 512], t_uT[tc],
                                    sq[b], t_sq[b], rs[b], t_rs[b], 4 + b, l)
                    P.op("sp", lambda e, tc=tc: e.dma_start(
                        out=self.uT_d[:, :, tc * 512:(tc + 1) * 512].rearrange("c p t -> p c t"),
                        in_=uT[:, :, tc * 512:(tc + 1) * 512]), reads=[t_uT[tc]], dma=True)
                P.barrier()
            if self.stop_after == ("A1", l):
                return
            with ExitStack() as es2:
                cosT = self.sb(es2, "cosT", [128, L], F32)
                sinS = self.sb(es2, "sinS", [128, L], F32)
                P.op("sp", lambda e: e.dma_start(out=cosT[:], in_=self.cos_d), writes=[self.t_const], dma=True)
                P.op("sp", lambda e: e.dma_start(out=sinS[:], in_=self.sin_d), writes=[self.t_const], dma=True)
                W = [self.sb(es2, "W%d" % i, [128, 8, 512], BF16) for i in range(2)]
                Ws = [self.sb(es2, "Ws%d" % i, [128, 8, 512], BF16) for i in range(2)]
                t_W = toks(2)
                t_Ws = toks(2)
                r1 = [self.sb(es2, "r1_%d" % i, [128, 512], F32) for i in range(2)]
                r2 = [self.sb(es2, "r2_%d" % i, [128, 512], F32) for i in range(2)]
                t_r1 = toks(2)
                t_r2 = toks(2)
                ro = [self.sb(es2, "ro%d" % i, [128, 512], BF16) for i in range(3)]
                t_ro = toks(3)
                sqk = self.sb(es2, "sqk", [128, 512], BF16)
                t_sqk = Tok()
                fk = self.sb(es2, "fk", [128, 512], F32)
                t_fk = Tok()
                groups = [
                    (R_AQ, [(C_AQ, 384), (C_AK, 64), (C_IK, 64)]),
                    (R_IQ, [(C_IQ, 512)]),
                    (R_BQ, [(C_BQ, 512)]),
                    (R_BQ + 512, [(C_BQ + 512, 256), (C_BK, 256)]),
                    (R_BK + 256, [(C_BK + 256, 512)]),
                    (R_CQ, [(C_CQ, 512)]),
                    (R_CK, [(C_CK, 128)]),
                ]
                rr = 0
                ri = 0
                for gi, (row0, pieces) in enumerate(groups):
                    wb = gi % 2
                    dc = 0
                    for (c0, ncol) in pieces:
                        self.load_w(W[wb], w_in, c0, ncol, dc, t_W[wb])
                        dc += ncol
                    ncols = dc
                    nh = ncols // 64
                    wv = W[wb][:, :, 0:ncols].rearrange("p k (h two d) -> p k h two d", two=2, d=32)
                    wsv = Ws[wb][:, :, 0:ncols].rearrange("p k (h two d) -> p k h two d", two=2, d=32)
                    for k in range(8):
                        P.op("act", lambda e, k=k, wv=wv, wsv=wsv: e.activation(out=wsv[:, k, :, 0, :], in_=wv[:, k, :, 1, :], func=AF.Copy),
                             reads=[t_W[wb]], writes=[t_Ws[wb]])
                        P.op("pool", lambda e, k=k, wv=wv, wsv=wsv: e.tensor_copy(out=wsv[:, k, :, 1, :], in_=wv[:, k, :, 0, :]),
                             reads=[t_W[wb]], writes=[t_Ws[wb]])
                    for tc in range(8):
                        tsl = slice(tc * 512, (tc + 1) * 512)
                        for j in range(ncols // 128):
                            row = row0 + j * 128
                            is_kik = (row == R_AKIK)
                            pa, pb_ = 0 + (rr % 2) * 2, 1 + (rr % 2) * 2
                            rb = rr % 2
                            rr += 1
                            for k in range(8):
                                P.op("pe", lambda e, k=k, j=j, pa=pa, wb=wb, tsl=tsl: e.matmul(
                                    self.ps[pa][:], lhsT=W[wb][:, k, j * 128:(j + 1) * 128], rhs=uT[:, k, tsl],
                                    start=(k == 0), stop=(k == 7)), reads=[t_W[wb], t_uT[tc]], writes=[self.pst[pa]])
                            for k in range(8):
                                P.op("pe", lambda e, k=k, j=j, pb_=pb_, wb=wb, tsl=tsl: e.matmul(
                                    self.ps[pb_][:], lhsT=Ws[wb][:, k, j * 128:(j + 1) * 128], rhs=uT[:, k, tsl],
                                    start=(k == 0), stop=(k == 7)), reads=[t_Ws[wb], t_uT[tc]], writes=[self.pst[pb_]])
                            ob = ri % 3
                            ri += 1
                            if not is_kik:
                                P.op("dve", lambda e, pa=pa, rb=rb, tsl=tsl: e.tensor_tensor(
                                    out=r1[rb][:], in0=self.ps[pa][:], in1=cosT[:, tsl], op=ALU.mult),
                                    reads=[self.pst[pa], self.t_const], writes=[t_r1[rb]])
                                P.op("dve", lambda e, pb_=pb_, rb=rb, tsl=tsl: e.tensor_tensor(
                                    out=r2[rb][:], in0=self.ps[pb_][:], in1=sinS[:, tsl], op=ALU.mult),
                                    reads=[self.pst[pb_], self.t_const], writes=[t_r2[rb]])
                                P.op("pool", lambda e, rb=rb, ob=ob: e.tensor_tensor(
                                    out=ro[ob][:], in0=r1[rb][:], in1=r2[rb][:], op=ALU.add),
                                    reads=[t_r1[rb], t_r2[rb]], writes=[t_ro[ob]])
                            else:
                                P.op("act", lambda e, pa=pa: e.activation(out=sqk[:], in_=self.ps[pa][:], func=AF.Square),
                                     reads=[self.pst[pa]], writes=[t_sqk])
                                P.op("pe", lambda e: e.matmul(self.ps[6][:], lhsT=self.cb[:, 4, :], rhs=sqk[:],
                                                              start=True, stop=True),
                                     reads=[t_sqk, self.t_const], writes=[self.pst[6]])
                                P.op("act", lambda e: e.activation(out=fk[:], in_=self.ps[6][:], func=AF.Ln,
                                                                   scale=1.0 / 64, bias=self.eps_t[:, 0:1]),
                                     reads=[self.pst[6], self.t_const], writes=[t_fk])
                                P.op("act", lambda e: e.activation(out=fk[:], in_=fk[:], func=AF.Exp, scale=-0.5),
                                     reads=[t_fk], writes=[t_fk])
                                P.op("dve", lambda e: e.tensor_scalar(out=fk[:], in0=fk[:], scalar1=self.cvec_sb[:, 2:3],
                                                                      scalar2=self.cvec_sb[:, 3:4], op0=ALU.mult, op1=ALU.add),
                                     reads=[t_fk, self.t_const], writes=[t_fk])
                                P.op("dve", lambda e, pa=pa, rb=rb, tsl=tsl: e.scalar_tensor_tensor(
                                    out=r1[rb][:], in0=self.ps[pa][:], scalar=self.gk[:, l, 0:1], in1=cosT[:, tsl],
                                    op0=ALU.mult, op1=ALU.mult),
                                    reads=[self.pst[pa], self.t_const], writes=[t_r1[rb]])
                                P.op("dve", lambda e, pb_=pb_, rb=rb, tsl=tsl: e.scalar_tensor_tensor(
                                    out=r2[rb][:], in0=self.ps[pb_][:], scalar=self.gk[:, l, 1:2], in1=sinS[:, tsl],
                                    op0=ALU.mult, op1=ALU.mult),
                                    reads=[self.pst[pb_], self.t_const], writes=[t_r2[rb]])
                                P.op("pool", lambda e, rb=rb: e.tensor_tensor(
                                    out=r1[rb][:], in0=r1[rb][:], in1=r2[rb][:], op=ALU.add),
                                    reads=[t_r1[rb], t_r2[rb]], writes=[t_r1[rb]])
                                P.op("pool", lambda e, rb=rb, ob=ob: e.tensor_tensor(
                                    out=ro[ob][:], in0=r1[rb][:], in1=fk[:], op=ALU.mult),
                                    reads=[t_r1[rb], t_fk], writes=[t_ro[ob]])
                            P.op("sp", lambda e, ob=ob, row=row, tsl=tsl: e.dma_start(
                                out=self.zT_d[row:row + 128, tsl], in_=ro[ob][:]), reads=[t_ro[ob]], dma=True)
                P.barrier()
            if self.stop_after == ("A2a", l):
                return
            with ExitStack() as es3:
                WV = [self.sb(es3, "WV%d" % i, [128, 8, 512], BF16) for i in range(2)]
                t_WV = toks(2)
                self.load_w(WV[0], w_in, C_BV, 512, 0, t_WV[0])
                self.load_w(WV[1], w_in, C_BV + 512, 256, 0, t_WV[1])
                self.load_w(WV[1], w_in, C_CV, 128, 256, t_WV[1])
                self.load_w(WV[1], w_in, C_AV, 64, 384, t_WV[1])
                self.load_w(WV[1], w_in, C_IW, 8, 448, t_WV[1])
                bvs = [self.sb(es3, "bvs%d" % i, [128, 12, 65], BF16) for i in range(2)]
                cvs = [self.sb(es3, "cvs%d" % i, [128, 2, 65], BF16) for i in range(2)]
                avs = [self.sb(es3, "avs%d" % i, [128, 65], BF16) for i in range(2)]
                iws = [self.sb(es3, "iws%d" % i, [128, 8], F32) for i in range(2)]
                t_st = toks(2)
                for i in range(2):
                    P.op("dve", lambda e, i=i: e.memset(bvs[i][:], 1.0), writes=[t_st[i]])
                    P.op("dve", lambda e, i=i: e.memset(cvs[i][:], 1.0), writes=[t_st[i]])
                    P.op("dve", lambda e, i=i: e.memset(avs[i][:], 1.0), writes=[t_st[i]])
                for ti in range(32):
                    b = ti % 2
                    tcs = ti // 4
                    tk = slice(ti * 128, (ti + 1) * 128)
                    for k in range(8):
                        P.op("pe", lambda e, k=k, tk=tk, b=b: e.matmul(self.ps[b * 2][:], lhsT=uT[:, k, tk], rhs=WV[0][:, k, :],
                                                                     start=(k == 0), stop=(k == 7)),
                             reads=[t_uT[tcs], t_WV[0]], writes=[self.pst[b * 2]])
                    for k in range(8):
                        P.op("pe", lambda e, k=k, tk=tk, b=b: e.matmul(self.ps[b * 2 + 1][:, 0:456], lhsT=uT[:, k, tk], rhs=WV[1][:, k, 0:456],
                                                                     start=(k == 0), stop=(k == 7)),
                             reads=[t_uT[tcs], t_WV[1]], writes=[self.pst[b * 2 + 1]])
                    p0, p1 = self.ps[b * 2], self.ps[b * 2 + 1]
                    P.op("dve", lambda e, b=b, p0=p0: e.tensor_copy(out=bvs[b][:, 0:8, 0:64], in_=p0[:].rearrange("p (h d) -> p h d", d=64)),
                         reads=[self.pst[b * 2]], writes=[t_st[b]])
                    P.op("act", lambda e, b=b, p1=p1: e.activation(out=bvs[b][:, 8:12, 0:64], in_=p1[:, 0:256].rearrange("p (h d) -> p h d", d=64), func=AF.Copy),
                         reads=[self.pst[b * 2 + 1]], writes=[t_st[b]])
                    P.op("dve", lambda e, b=b, p1=p1: e.tensor_copy(out=cvs[b][:, :, 0:64], in_=p1[:, 256:384].rearrange("p (h d) -> p h d", d=64)),
                         reads=[self.pst[b * 2 + 1]], writes=[t_st[b]])
                    P.op("act", lambda e, b=b, p1=p1: e.activation(out=avs[b][:, 0:64], in_=p1[:, 384:448], func=AF.Copy),
                         reads=[self.pst[b * 2 + 1]], writes=[t_st[b]])
                    P.op("dve", lambda e, b=b, p1=p1: e.tensor_copy(out=iws[b][:], in_=p1[:, 448:456]),
                         reads=[self.pst[b * 2 + 1]], writes=[t_st[b]])
                    P.op("sp", lambda e, b=b, tk=tk: e.dma_start(out=self.bv_d[tk], in_=bvs[b][:]), reads=[t_st[b]], dma=True)
                    P.op("sp", lambda e, b=b, tk=tk: e.dma_start(out=self.cv_d[tk], in_=cvs[b][:]), reads=[t_st[b]], dma=True)
                    P.op("sp", lambda e, b=b, tk=tk: e.dma_start(out=self.av_d[tk], in_=avs[b][:]), reads=[t_st[b]], dma=True)
                    P.op("sp", lambda e, b=b, tk=tk: e.dma_start(out=self.iw_d[tk], in_=iws[b][:]), reads=[t_st[b]], dma=True)
                P.barrier()


    def attn_consts(self, es):
        P = self.P
        self.mrep = self.sb(es, "mrep", [128, 9, 4, 128], BF16)
        self.irep = self.sb(es, "irep", [128, 3, 128], BF16)
        self.zeros_bf = self.sb(es, "zeros_bf", [128, 64], BF16)
        mf = self.sb(es, "masks_f", [128, 9, 128], F32)
        t = Tok()
        P.op("sp", lambda e: e.dma_start(out=mf[:], in_=self.masks), writes=[t], dma=True)
        P.op("dve", lambda e: e.tensor_copy(out=self.mrep[:], in_=sap(mf, 0, [[128, 9], [0, 4], [1, 128]])),
             reads=[t], writes=[self.t_const])
        P.op("dve", lambda e: e.tensor_copy(out=self.irep[:], in_=sap(self.cmat_sb, 0, [[0, 3], [1, 128]])),
             reads=[self.t_const], writes=[self.t_const])
        P.op("dve", lambda e: e.memset(self.zeros_bf[:], 0.0), writes=[self.t_const])

    def normalize_out(self, psO, psD, tO, tD, n, rc, t_rc, out_ap, t_out):
        P = self.P
        P.op("act", lambda e: e.activation(out=rc[0:64, 0:n], in_=psD[0:64, 0:n], func=AF.Ln), reads=[tD], writes=[t_rc])
        P.op("act", lambda e: e.activation(out=rc[0:64, 0:n], in_=rc[0:64, 0:n], func=AF.Exp, scale=-1.0),
             reads=[t_rc], writes=[t_rc])
        P.op("dve", lambda e: e.tensor_tensor(out=out_ap, in0=psO[0:64, 0:n], in1=rc[0:64, 0:n], op=ALU.mult),
             reads=[tO, t_rc], writes=[t_out])

    def phase_attnC(self, l):
        P = self.P
        with ExitStack() as es:
            ckT = self.sb(es, "ckT", [64, 2, L], BF16)
            cv = self.sb(es, "cv", [128, 32, 2, 65], BF16)
            t_k = Tok(); t_v = Tok()
            P.op("sp", lambda e: e.dma_start(out=ckT[:], in_=self.zT_d[R_CK:R_CK + 128, :].rearrange("(g d) t -> d g t", d=64)),
                 writes=[t_k], dma=True)
            P.op("sp", lambda e: e.dma_start(out=cv[:], in_=self.cv_d.rearrange("(b p) g e -> p b g e", p=128)),
                 writes=[t_v], dma=True)
            sk = self.sb(es, "sk", [1, 8], F32)
            skrow = self.sb(es, "skrow", [1, 8, 128], F32)
            t_sk = Tok()
            P.op("sp", lambda e: e.dma_start(out=sk[:], in_=self.sinks[l:l + 1, :]), writes=[t_sk], dma=True)
            P.op("act", lambda e: e.activation(out=sk[:], in_=sk[:], func=AF.Exp), reads=[t_sk], writes=[t_sk])
            P.op("dve", lambda e: e.tensor_copy(out=skrow[:], in_=sap(sk, 0, [[1, 8], [0, 128]], npart=1)),
                 reads=[t_sk], writes=[t_sk])
            q = [self.sb(es, "cq%d" % i, [64, 8, 128], BF16) for i in range(3)]
            t_q = toks(3)
            pT = [self.sb(es, "pT%d" % i, [128, 512], BF16) for i in range(3)]
            t_pT = toks(3)
            rc = [self.sb(es, "rc%d" % i, [64, 512], F32) for i in range(2)]
            t_rc = toks(2)
            oT = [self.sb(es, "oT%d" % i, [64, 8, 128], BF16) for i in range(2)]
            t_oT = toks(2)
            pi = 0
            it = 0
            for qb in range(32):
                qs = slice(qb * 128, (qb + 1) * 128)
                qi = qb % 3
                P.op("sp", lambda e, qi=qi, qs=qs: e.dma_start(
                    out=q[qi][:], in_=self.zT_d[R_CQ:R_CQ + 512, qs].rearrange("(h d) t -> d h t", d=64)),
                    writes=[t_q[qi]], dma=True)
                ob = qb % 2
                for g in range(2):
                    bS = it % 2
                    bO = 2 + it % 2
                    bD = 4 + it % 2
                    it += 1
                    kbs = ([(qb - 1, 8)] if qb > 0 else []) + [(qb, 0)]
                    for n_, (kb, mi) in enumerate(kbs):
                        psS = self.ps[bS if n_ == 0 else 6 + (it % 2)]
                        tS = self.pst[bS if n_ == 0 else 6 + (it % 2)]
                        P.op("pe", lambda e, psS=psS, mi=mi: e.matmul(psS[:], lhsT=self.cb[:, 0, :],
                                                                     rhs=self.mrep[:, mi].rearrange("p h t -> p (h t)"),
                                                                     start=True, stop=False),
                             reads=[self.t_const], writes=[tS])
                        P.op("pe", lambda e, psS=psS, kb=kb, g=g, qi=qi: e.matmul(
                            psS[:], lhsT=ckT[:, g, kb * 128:(kb + 1) * 128], rhs=q[qi][:, 4 * g:4 * g + 4, :],
                            start=False, stop=True), reads=[t_k, t_q[qi]], writes=[tS])
                        pb = pi % 3
                        pi += 1
                        P.op("act", lambda e, psS=psS, pb=pb: e.activation(out=pT[pb][:], in_=psS[:], func=AF.Exp, scale=0.125),
                             reads=[tS], writes=[t_pT[pb]])
                        P.op("pe", lambda e, pb=pb, kb=kb, g=g, bO=bO, n_=n_: e.matmul(
                            self.ps[bO][0:64, :], lhsT=cv[:, kb, g, 0:64], rhs=pT[pb][:], start=(n_ == 0), stop=(n_ == len(kbs) - 1)),
                            reads=[t_v, t_pT[pb]], writes=[self.pst[bO]])
                        P.op("pe", lambda e, pb=pb, bD=bD, n_=n_: e.matmul(
                            self.ps[bD][0:64, :], lhsT=self.ones_bf[:, 0:64], rhs=pT[pb][:], start=(n_ == 0), stop=False),
                            reads=[self.t_const, t_pT[pb]], writes=[self.pst[bD]])
                    P.op("pe", lambda e, bD=bD, g=g: e.matmul(
                        self.ps[bD][0:64, :], lhsT=self.ones_f[0:1, 0:64], rhs=skrow[0:1, 4 * g:4 * g + 4, :],
                        start=False, stop=True), reads=[self.t_const, t_sk], writes=[self.pst[bD]])
                    self.normalize_out(self.ps[bO], self.ps[bD], self.pst[bO], self.pst[bD], 512, rc[g], t_rc[g],
                                       oT[ob][:, 4 * g:4 * g + 4, :], t_oT[ob])
                P.op("sp", lambda e, ob=ob, qs=qs: e.dma_start(out=self.ocT_d[:, :, qs], in_=oT[ob][:]),
                     reads=[t_oT[ob]], dma=True)
            P.barrier()

    def phase_attnB(self, l):
        P = self.P
        with ExitStack() as es:
            bkT = self.sb(es, "bkT", [64, 12, L], BF16)
            bv = self.sb(es, "bv", [128, 32, 12, 65], BF16)
            t_k = Tok(); t_v = Tok()
            for g in range(3):
                P.op("sp", lambda e, g=g: e.dma_start(
                    out=bkT[:, 4 * g:4 * g + 4, :],
                    in_=self.zT_d[R_BK + 256 * g:R_BK + 256 * (g + 1), :].rearrange("(h d) t -> d h t", d=64)),
                    writes=[t_k], dma=True)
            for b4 in range(4):
                P.op("sp", lambda e, b4=b4: e.dma_start(
                    out=bv[:, 8 * b4:8 * b4 + 8],
                    in_=self.bv_d[1024 * b4:1024 * (b4 + 1)].rearrange("(b p) h e -> p b h e", p=128)),
                    writes=[t_v], dma=True)
            q = [self.sb(es, "bq%d" % i, [64, 12, 128], BF16) for i in range(3)]
            t_q = toks(3)
            pT = [self.sb(es, "pT%d" % i, [128, 4, 128], BF16) for i in range(3)]
            t_pT = toks(3)
            rc = [self.sb(es, "rc%d" % i, [64, 512], F32) for i in range(2)]
            t_rc = toks(2)
            oT = [self.sb(es, "oT%d" % i, [64, 4, 128], BF16) for i in range(2)]
            t_oT = toks(2)
            pi = 0
            si = 0
            for qb in range(32):
                qs = slice(qb * 128, (qb + 1) * 128)
                qi = qb % 3
                P.op("sp", lambda e, qi=qi, qs=qs: e.dma_start(
                    out=q[qi][:], in_=self.zT_d[R_BQ:R_BQ + 768, qs].rearrange("(h d) t -> d h t", d=64)),
                    writes=[t_q[qi]], dma=True)
                ob = qb % 2
                bO = 4 + qb % 2
                bD = 6 + qb % 2
                for bb in (bO, bD):
                    P.op("pe", lambda e, bb=bb: e.matmul(self.ps[bb][0:64, :], lhsT=self.zeros_bf[:, 0:64],
                                                       rhs=self.mrep[:, 0].rearrange("p h t -> p (h t)"), start=True, stop=False),
                         reads=[self.t_const], writes=[self.pst[bb]])
                items = []
                for g, Dl in enumerate((1, 4, 16)):
                    for o in range(Dl + 1):
                        kb = qb - o
                        if kb < 0:
                            break
                        if Dl == 1:
                            mi = 0 if o == 0 else 1
                        else:
                            base = 2 if Dl == 4 else 5
                            mi = base + (0 if o == 0 else (2 if o == Dl else 1))
                        items.append((g, kb, mi))
                for n_, (g, kb, mi) in enumerate(items):
                    last = (n_ == len(items) - 1)
                    bS = si % 4
                    si += 1
                    psS, tS = self.ps[bS], self.pst[bS]
                    P.op("pe", lambda e, psS=psS, mi=mi: e.matmul(psS[:], lhsT=self.cb[:, 0, :],
                                                                 rhs=self.mrep[:, mi].rearrange("p h t -> p (h t)"),
                                                                 start=True, stop=False),
                         reads=[self.t_const], writes=[tS])
                    for j in range(4):
                        P.op("pe", lambda e, psS=psS, kb=kb, g=g, j=j, qi=qi: e.matmul(
                            psS[:, j * 128:(j + 1) * 128], lhsT=bkT[:, 4 * g + j, kb * 128:(kb + 1) * 128],
                            rhs=q[qi][:, 4 * g + j, :], start=False, stop=(j == 3)), reads=[t_k, t_q[qi]], writes=[tS])
                    pb = pi % 3
                    pi += 1
                    P.op("act", lambda e, psS=psS, pb=pb: e.activation(out=pT[pb][:].rearrange("p h t -> p (h t)"), in_=psS[:],
                                                                       func=AF.Exp, scale=0.125),
                         reads=[tS], writes=[t_pT[pb]])
                    for j in range(4):
                        P.op("pe", lambda e, pb=pb, kb=kb, g=g, j=j, bO=bO, last=last: e.matmul(
                            self.ps[bO][0:64, j * 128:(j + 1) * 128], lhsT=bv[:, kb, 4 * g + j, 0:64], rhs=pT[pb][:, j, :],
                            start=False, stop=(last and j == 3)), reads=[t_v, t_pT[pb]], writes=[self.pst[bO]])
                    P.op("pe", lambda e, pb=pb, bD=bD, last=last: e.matmul(
                        self.ps[bD][0:64, :], lhsT=self.ones_bf[:, 0:64], rhs=pT[pb][:].rearrange("p h t -> p (h t)"),
                        start=False, stop=last), reads=[self.t_const, t_pT[pb]], writes=[self.pst[bD]])
                self.normalize_out(self.ps[bO], self.ps[bD], self.pst[bO], self.pst[bD], 512, rc[ob], t_rc[ob],
                                   oT[ob][:].rearrange("p h t -> p (h t)"), t_oT[ob])
                P.op("sp", lambda e, ob=ob, qs=qs: e.dma_start(out=self.obT_d[:, :, qs], in_=oT[ob][:]),
                     reads=[t_oT[ob]], dma=True)
            self.dump("d_bkT", bkT[:], [64, 12, L], BF16, t_k)
            self.dump("d_bv", bv[:], [128, 32, 12, 65], BF16, t_v)
            P.barrier()

    def phase_attnA(self, l):
        P = self.P
        with ExitStack() as es:
            kaT = self.sb(es, "kaT", [64, L], BF16)
            ikT = self.sb(es, "ikT", [64, L], BF16)
            av = self.sb(es, "av", [128, 32, 65], BF16)
            iw = self.sb(es, "iw", [128, 32, 8], F32)
            t_k = Tok(); t_ik = Tok(); t_v = Tok(); t_iw = Tok()
            P.op("sp", lambda e: e.dma_start(out=kaT[:], in_=self.zT_d[R_AKIK:R_AKIK + 64, :]), writes=[t_k], dma=True)
            P.op("sp", lambda e: e.dma_start(out=ikT[:], in_=self.zT_d[R_AKIK + 64:R_AKIK + 128, :]), writes=[t_ik], dma=True)
            P.op("sp", lambda e: e.dma_start(out=av[:], in_=self.av_d.rearrange("(b p) e -> p b e", p=128)), writes=[t_v], dma=True)
            P.op("sp", lambda e: e.dma_start(out=iw[:], in_=self.iw_d.rearrange("(b p) h -> p b h", p=128)), writes=[t_iw], dma=True)
            aq = [self.sb(es, "aq%d" % i, [64, 6, 128], BF16) for i in range(3)]
            iq = [self.sb(es, "iq%d" % i, [64, 8, 128], BF16) for i in range(3)]
            t_aq = toks(3); t_iq = toks(3)
            Dg = [self.sb(es, "Dg%d" % i, [128, 8, 128], BF16) for i in range(2)]
            t_Dg = toks(2)
            R = [self.sb(es, "R%d" % i, [128, 512], BF16) for i in range(16)]
            t_R = toks(16)
            score = [self.sb(es, "score%d" % i, [128, L], F32) for i in range(2)]
            t_sc = toks(2)
            junk = self.sb(es, "junk", [128, L], BF16)
            t_junk = Tok()
            negm = [self.sb(es, "negm%d" % i, [128, L], BF16) for i in range(2)]
            t_ng = toks(2)
            st = [self.sb(es, "st%d" % i, [128, 8], F32) for i in range(2)]
            thr = [self.sb(es, "thr%d" % i, [128, 2], F32) for i in range(2)]
            rtab = [self.sb(es, "rtab%d" % i, [128, N_BISECT], F32) for i in range(2)]
            t_st = toks(2)
            p2 = self.sb(es, "p2", [128, N_BISECT], F32)
            for i in range(N_BISECT):
                P.op("dve", lambda e, i=i: e.memset(p2[:, i:i + 1], 2.0 ** -(i + 1)), writes=[self.t_const])
            pT = [self.sb(es, "pT%d" % i, [128, 384], BF16) for i in range(3)]
            t_pT = toks(3)
            rc = [self.sb(es, "rc%d" % i, [64, 512], F32) for i in range(2)]
            t_rc = toks(2)
            oT = [self.sb(es, "oT%d" % i, [64, 6, 128], BF16) for i in range(2)]
            t_oT = toks(2)
            ri = 0
            pi = 0
            si = 0
            for qb in range(32):
                qs = slice(qb * 128, (qb + 1) * 128)
                qi = qb % 3
                b2 = qb % 2
                nk = (qb + 1) * 128
                P.op("sp", lambda e, qi=qi, qs=qs: e.dma_start(
                    out=aq[qi][:], in_=self.zT_d[R_AQ:R_AQ + 384, qs].rearrange("(h d) t -> d h t", d=64)),
                    writes=[t_aq[qi]], dma=True)
                ng, t_n = negm[b2], t_ng[b2]
                if qb < 2:
                    if qb == 1:
                        P.op("dve", lambda e, ng=ng: e.memset(ng[:, 0:128], 0.0), writes=[t_n])
                    P.op("dve", lambda e, ng=ng, qb=qb: e.tensor_copy(out=ng[:, qb * 128:(qb + 1) * 128], in_=self.cb[:, 1, :]),
                         reads=[self.t_const], writes=[t_n])
                else:
                    P.op("sp", lambda e, qi=qi, qs=qs: e.dma_start(
                        out=iq[qi][:], in_=self.zT_d[R_IQ:R_IQ + 512, qs].rearrange("(h d) t -> d h t", d=64)),
                        writes=[t_iq[qi]], dma=True)
                    sc, t_s = score[b2], t_sc[b2]
                    for h in range(8):
                        P.op("pool", lambda e, h=h, b2=b2, qb=qb: e.tensor_scalar(
                            out=Dg[b2][:, h, :], in0=self.cb[:, 0, :], scalar1=iw[:, qb, h:h + 1], scalar2=None, op0=ALU.mult),
                            reads=[self.t_const, t_iw], writes=[t_Dg[b2]])
                    for c0 in range(0, nk, 512):
                        w = min(512, nk - c0)
                        lastc = (c0 + w == nk)
                        rbase = (ri % 2) * 8
                        ri += 1
                        for h in range(8):
                            bR = h % 2
                            P.op("pe", lambda e, h=h, bR=bR, qi=qi, c0=c0, w=w: e.matmul(
                                self.ps[bR][:, 0:w], lhsT=iq[qi][:, h, :], rhs=ikT[:, c0:c0 + w], start=True, stop=True),
                                reads=[t_iq[qi], t_ik], writes=[self.pst[bR]])
                            rb = rbase + h
                            if h % 2 == 0:
                                P.op("act", lambda e, bR=bR, rb=rb, w=w: e.activation(out=R[rb][:, 0:w], in_=self.ps[bR][:, 0:w], func=AF.Relu),
                                     reads=[self.pst[bR]], writes=[t_R[rb]])
                            else:
                                P.op("dve", lambda e, bR=bR, rb=rb, w=w: e.tensor_scalar(
                                    out=R[rb][:, 0:w], in0=self.ps[bR][:, 0:w], scalar1=0.0, scalar2=None, op0=ALU.max),
                                    reads=[self.pst[bR]], writes=[t_R[rb]])
                        for h in range(8):
                            rb = rbase + h
                            P.op("pe", lambda e, h=h, rb=rb, b2=b2, w=w, lastc=lastc: e.matmul(
                                self.ps[2][:, 0:w], lhsT=Dg[b2][:, h, :], rhs=R[rb][:, 0:w], start=(h == 0),
                                stop=(h == 7 and not lastc)), reads=[t_Dg[b2], t_R[rb]], writes=[self.pst[2]])
                        if lastc:
                            P.op("pe", lambda e, w=w: e.matmul(self.ps[2][:, w - 128:w], lhsT=self.cb[:, 0, :], rhs=self.cb[:, 1, :],
                                                               start=False, stop=True),
                                 reads=[self.t_const], writes=[self.pst[2]])
                        P.op("act", lambda e, sc=sc, c0=c0, w=w: e.activation(out=sc[:, c0:c0 + w], in_=self.ps[2][:, 0:w], func=AF.Copy),
                             reads=[self.pst[2]], writes=[t_s])
                    S_, T_, RT = st[b2], thr[b2], rtab[b2]
                    t_t = t_st[b2]
                    P.op("dve", lambda e, sc=sc, S_=S_, nk=nk: e.tensor_reduce(out=S_[:, 0:1], in_=sc[:, 0:nk], axis=mybir.AxisListType.X, op=ALU.max),
                         reads=[t_s], writes=[t_t])
                    P.op("dve", lambda e, sc=sc, S_=S_, nk=nk: e.tensor_reduce(out=S_[:, 1:2], in_=sc[:, 0:nk - 128], axis=mybir.AxisListType.X, op=ALU.min),
                         reads=[t_s], writes=[t_t])
                    P.op("dve", lambda e, S_=S_: e.tensor_tensor(out=S_[:, 2:3], in0=S_[:, 0:1], in1=S_[:, 1:2], op=ALU.subtract),
                         reads=[t_t], writes=[t_t])
                    P.op("dve", lambda e, S_=S_, RT=RT: e.tensor_scalar(out=RT[:], in0=p2[:], scalar1=S_[:, 2:3], scalar2=None, op0=ALU.mult),
                         reads=[t_t, self.t_const], writes=[t_t])
                    P.op("dve", lambda e, S_=S_, T_=T_: e.scalar_tensor_tensor(out=T_[:, 0:1], in0=S_[:, 2:3], scalar=0.5, in1=S_[:, 1:2],
                                                                               op0=ALU.mult, op1=ALU.add),
                         reads=[t_t], writes=[t_t])
                    cur = 0
                    for i in range(N_BISECT):
                        P.op("dve", lambda e, sc=sc, T_=T_, S_=S_, nk=nk, cur=cur: e.tensor_scalar(
                            out=junk[:, 0:nk], in0=sc[:, 0:nk], scalar1=T_[:, cur:cur + 1], scalar2=None, op0=ALU.is_ge, op1=ALU.add,
                            accum_out=S_[:, 3:4]), reads=[t_s, t_t], writes=[t_t, t_junk])
                        P.op("dve", lambda e, S_=S_: e.tensor_scalar(out=S_[:, 4:5], in0=S_[:, 3:4], scalar1=255.5, scalar2=-0.5,
                                                                     op0=ALU.is_ge, op1=ALU.add), reads=[t_t], writes=[t_t])
                        P.op("dve", lambda e, S_=S_, T_=T_, RT=RT, i=i, cur=cur: e.scalar_tensor_tensor(
                            out=T_[:, 1 - cur:2 - cur], in0=S_[:, 4:5], scalar=RT[:, i:i + 1], in1=T_[:, cur:cur + 1],
                            op0=ALU.mult, op1=ALU.add), reads=[t_t], writes=[t_t])
                        cur = 1 - cur
                    P.op("dve", lambda e, sc=sc, ng=ng, T_=T_, nk=nk, cur=cur: e.tensor_scalar(
                        out=ng[:, 0:nk], in0=sc[:, 0:nk], scalar1=T_[:, cur:cur + 1], scalar2=NEG, op0=ALU.is_lt, op1=ALU.mult),
                        reads=[t_s, t_t], writes=[t_n])
                ob = qb % 2
                for half in range(2):
                    bO, bD = 5, 6
                    for kb in range(qb + 1):
                        bS = 3 + si % 2
                        si += 1
                        psS, tS = self.ps[bS], self.pst[bS]
                        P.op("pe", lambda e, psS=psS, ng=ng, kb=kb: e.matmul(
                            psS[:, 0:384], lhsT=ng[:, kb * 128:(kb + 1) * 128], rhs=self.irep[:].rearrange("p h t -> p (h t)"),
                            start=True, stop=False), reads=[t_n, self.t_const], writes=[tS])
                        P.op("pe", lambda e, psS=psS, kb=kb, qi=qi, half=half: e.matmul(
                            psS[:, 0:384], lhsT=kaT[:, kb * 128:(kb + 1) * 128], rhs=aq[qi][:, 3 * half:3 * half + 3, :],
                            start=False, stop=True), reads=[t_k, t_aq[qi]], writes=[tS])
                        pb = pi % 3
                        pi += 1
                        P.op("act", lambda e, psS=psS, pb=pb: e.activation(out=pT[pb][:], in_=psS[:, 0:384], func=AF.Exp, scale=0.125),
                             reads=[tS], writes=[t_pT[pb]])
                        P.op("pe", lambda e, pb=pb, kb=kb, qb=qb: e.matmul(
                            self.ps[bO][0:64, 0:384], lhsT=av[:, kb, 0:64], rhs=pT[pb][:], start=(kb == 0), stop=(kb == qb)),
                            reads=[t_v, t_pT[pb]], writes=[self.pst[bO]])
                        P.op("pe", lambda e, pb=pb, kb=kb, qb=qb: e.matmul(
                            self.ps[bD][0:64, 0:384], lhsT=self.ones_bf[:, 0:64], rhs=pT[pb][:], start=(kb == 0), stop=(kb == qb)),
                            reads=[self.t_const, t_pT[pb]], writes=[self.pst[bD]])
                    self.normalize_out(self.ps[bO], self.ps[bD], self.pst[bO], self.pst[bD], 384, rc[half], t_rc[half],
                                       oT[ob][:, 3 * half:3 * half + 3, :].rearrange("p h t -> p (h t)"), t_oT[ob])
                P.op("sp", lambda e, ob=ob, qs=qs: e.dma_start(out=self.oaT_d[:, :, qs], in_=oT[ob][:]),
                     reads=[t_oT[ob]], dma=True)
            P.barrier()


    def phase_M(self, l):
        P = self.P
        TW = 256
        with ExitStack() as es:
            Wa = self.sb(es, "Wa", [64, 6, D], BF16)
            Wb = self.sb(es, "Wb", [64, 4, D], BF16)
            Wc = self.sb(es, "Wc", [64, 8, D], BF16)
            Wg = self.sb(es, "Wg", [128, 8, 3 * D], BF16)
            Wo = self.sb(es, "Wo", [128, 8, D], BF16)
            t_w = Tok()
            for (dst, src) in ((Wa, self.w_a[l]), (Wb, self.w_b[l]), (Wc, self.w_c[l])):
                P.op("pool", lambda e, dst=dst, src=src: e.dma_start(out=dst[:], in_=src.rearrange("(h d) m -> d h m", d=64)),
                     writes=[t_w], dma=True)
            for i in range(3):
                for hh in range(2):
                    c0 = C_G + i * D + hh * 512
                    self.load_w(Wg, self.w_in[l], c0, 512, i * D + hh * 512, t_w)
            for hh in range(2):
                self.load_w(Wo, self.w_o[l], hh * 512, 512, hh * 512, t_w)
            oa = [self.sb(es, "oa%d" % i, [64, 6, TW], BF16) for i in range(2)]
            ob_ = [self.sb(es, "ob%d" % i, [64, 4, TW], BF16) for i in range(2)]
            oc = [self.sb(es, "oc%d" % i, [64, 8, TW], BF16) for i in range(2)]
            uT = [self.sb(es, "uTm%d" % i, [128, 8, TW], BF16) for i in range(2)]
            hT = [self.sb(es, "hTm%d" % i, [128, 8, TW], F32) for i in range(2)]
            t_in = toks(2)
            t_h = toks(2)
            sig = [self.sb(es, "sig%d" % i, [128, TW], F32) for i in range(2)]
            t_sig = toks(2)
            mm_ = [self.sb(es, "mm%d" % i, [128, TW], F32) for i in range(3)]
            t_mm = toks(3)
            mg = [self.sb(es, "mg%d" % i, [128, 8, TW], BF16) for i in range(2)]
            t_mg = toks(2)
            branches = ((Wa, oa, 6), (Wb, ob_, 4), (Wc, oc, 8))
            k_ = 0
            for tc in range(L // TW):
                b = tc % 2
                tsl = slice(tc * TW, (tc + 1) * TW)
                P.op("sp", lambda e, b=b, tsl=tsl: e.dma_start(out=oa[b][:], in_=self.oaT_d[:, :, tsl]), writes=[t_in[b]], dma=True)
                P.op("sp", lambda e, b=b, tsl=tsl: e.dma_start(out=ob_[b][:], in_=self.obT_d[:, :, tsl]), writes=[t_in[b]], dma=True)
                P.op("sp", lambda e, b=b, tsl=tsl: e.dma_start(out=oc[b][:], in_=self.ocT_d[:, :, tsl]), writes=[t_in[b]], dma=True)
                P.op("sp", lambda e, b=b, tsl=tsl: e.dma_start(out=uT[b][:], in_=self.uT_d[:, :, tsl].rearrange("c p t -> p c t")),
                     writes=[t_in[b]], dma=True)
                P.op("sp", lambda e, b=b, tsl=tsl: e.dma_start(out=hT[b][:], in_=self.hT_d[:, :, tsl].rearrange("c p t -> p c t")),
                     writes=[t_h[b]], dma=True)
                for c in range(8):
                    cs = slice(c * 128, (c + 1) * 128)
                    for i, (Wi, oi, nh) in enumerate(branches):
                        bY = (k_ % 2) * 2
                        bG = (k_ % 2) * 2 + 1
                        sb_ = k_ % 2
                        k_ += 1
                        for h in range(nh):
                            P.op("pe", lambda e, Wi=Wi, oi=oi, h=h, cs=cs, b=b, bY=bY, nh=nh: e.matmul(
                                self.ps[bY][:, 0:TW], lhsT=Wi[:, h, cs], rhs=oi[b][:, h, :], start=(h == 0), stop=(h == nh - 1)),
                                reads=[t_w, t_in[b]], writes=[self.pst[bY]])
                        for k in range(8):
                            P.op("pe", lambda e, k=k, i=i, c=c, b=b, bG=bG: e.matmul(
                                self.ps[bG][:, 0:TW], lhsT=Wg[:, k, i * D + c * 128:i * D + (c + 1) * 128], rhs=uT[b][:, k, :],
                                start=(k == 0), stop=(k == 7)), reads=[t_w, t_in[b]], writes=[self.pst[bG]])
                        P.op("act", lambda e, bG=bG, sb_=sb_: e.activation(out=sig[sb_][:], in_=self.ps[bG][:, 0:TW], func=AF.Sigmoid),
                             reads=[self.pst[bG]], writes=[t_sig[sb_]])
                        P.op("dve", lambda e, bY=bY, sb_=sb_, i=i: e.tensor_tensor(out=mm_[i][:], in0=self.ps[bY][:, 0:TW], in1=sig[sb_][:], op=ALU.mult),
                             reads=[self.pst[bY], t_sig[sb_]], writes=[t_mm[i]])
                    P.op("pool", lambda e: e.tensor_tensor(out=mm_[0][:], in0=mm_[0][:], in1=mm_[1][:], op=ALU.add),
                         reads=[t_mm[0], t_mm[1]], writes=[t_mm[0]])
                    P.op("pool", lambda e, b=b, c=c: e.tensor_tensor(out=mg[b][:, c, :], in0=mm_[0][:], in1=mm_[2][:], op=ALU.add),
                         reads=[t_mm[0], t_mm[2]], writes=[t_mg[b]])
                for c2 in range(8):
                    bD = 4 + c2 % 2
                    for c in range(8):
                        P.op("pe", lambda e, c=c, c2=c2, b=b, bD=bD: e.matmul(
                            self.ps[bD][:, 0:TW], lhsT=Wo[:, c, c2 * 128:(c2 + 1) * 128], rhs=mg[b][:, c, :], start=(c == 0), stop=(c == 7)),
                            reads=[t_w, t_mg[b]], writes=[self.pst[bD]])
                    P.op("dve", lambda e, c2=c2, b=b, bD=bD: e.tensor_tensor(out=hT[b][:, c2, :], in0=self.ps[bD][:, 0:TW], in1=hT[b][:, c2, :], op=ALU.add),
                         reads=[self.pst[bD], t_h[b]], writes=[t_h[b]])
                P.op("sp", lambda e, b=b, tsl=tsl: e.dma_start(out=self.hT_d[:, :, tsl].rearrange("c p t -> p c t"), in_=hT[b][:]),
                     reads=[t_h[b]], dma=True)
            P.barrier()

    def phase_F(self, l):
        P = self.P
        for half in range(2):
            with ExitStack() as es:
                Wu = self.sb(es, "Wu", [128, 8, 2048], BF16)
                Wd = self.sb(es, "Wd", [128, 16, D], BF16)
                t_w = Tok()
                for q4 in range(4):
                    self.load_w(Wu, self.w_up[l], half * 2048 + q4 * 512, 512, q4 * 512, t_w)
                srcd = self.w_down[l][half * 2048:(half + 1) * 2048, :].rearrange("(kc p) m -> p kc m", p=128)
                for q2 in range(2):
                    P.op("pool", lambda e, q2=q2, srcd=srcd, Wd=Wd: e.dma_start(out=Wd[:, q2 * 8:(q2 + 1) * 8, :], in_=srcd[:, q2 * 8:(q2 + 1) * 8, :]),
                         writes=[t_w], dma=True)
                hT = [self.sb(es, "hTf%d" % i, [128, 8, 512], F32) for i in range(2)]
                t_h = toks(2)
                u2 = [self.sb(es, "u2%d" % i, [128, 8, 512], BF16) for i in range(2)]
                t_u = toks(2)
                sq = [self.sb(es, "sqf%d" % i, [128, 8, 512], BF16) for i in range(2)]
                t_sq = toks(2)
                rs = [self.sb(es, "rsf%d" % i, [128, 512], F32) for i in range(2)]
                t_rs = toks(2)
                hid = [self.sb(es, "hid%d" % i, [128, 16, 512], BF16) for i in range(2)]
                t_hid = toks(2)
                rl = [self.sb(es, "rl%d" % i, [128, 512], F32) for i in range(2)]
                t_rl = toks(2)
                k_ = 0
                for tc in range(8):
                    b = tc % 2
                    tsl = slice(tc * 512, (tc + 1) * 512)
                    P.op("sp", lambda e, b=b, tsl=tsl: e.dma_start(out=hT[b][:], in_=self.hT_d[:, :, tsl].rearrange("c p t -> p c t")),
                         writes=[t_h[b]], dma=True)
                    if half == 0:
                        self.norm_chunk(None, hT[b], t_h[b], lambda c: self.g_mlp[:, l, c:c + 1],
                                        lambda c, b=b: u2[b][:, c, :], t_u[b], sq[b], t_sq[b], rs[b], t_rs[b], 6 + b, l)
                        P.op("sp", lambda e, b=b, tsl=tsl: e.dma_start(out=self.uT_d[:, :, tsl].rearrange("c p t -> p c t"), in_=u2[b][:]),
                             reads=[t_u[b]], dma=True)
                    else:
                        P.op("sp", lambda e, b=b, tsl=tsl: e.dma_start(out=u2[b][:], in_=self.uT_d[:, :, tsl].rearrange("c p t -> p c t")),
                             writes=[t_u[b]], dma=True)
                    for f in range(16):
                        bU = k_ % 4
                        rb = k_ % 2
                        k_ += 1
                        for k in range(8):
                            P.op("pe", lambda e, k=k, f=f, b=b, bU=bU: e.matmul(
                                self.ps[bU][:], lhsT=Wu[:, k, f * 128:(f + 1) * 128], rhs=u2[b][:, k, :], start=(k == 0), stop=(k == 7)),
                                reads=[t_w, t_u[b]], writes=[self.pst[bU]])
                        P.op("act", lambda e, bU=bU, rb=rb: e.activation(out=rl[rb][:], in_=self.ps[bU][:], func=AF.Relu),
                             reads=[self.pst[bU]], writes=[t_rl[rb]])
                        eng = "dve" if f % 2 == 0 else "pool"
                        P.op(eng, lambda e, rb=rb, b=b, f=f: e.tensor_tensor(out=hid[b][:, f, :], in0=rl[rb][:], in1=rl[rb][:], op=ALU.mult),
                             reads=[t_rl[rb]], writes=[t_hid[b]])
                    for c2 in range(8):
                        bD = 4 + c2 % 2
                        for f in range(16):
                            P.op("pe", lambda e, f=f, c2=c2, b=b, bD=bD: e.matmul(
                                self.ps[bD][:], lhsT=Wd[:, f, c2 * 128:(c2 + 1) * 128], rhs=hid[b][:, f, :], start=(f == 0), stop=(f == 15)),
                                reads=[t_w, t_hid[b]], writes=[self.pst[bD]])
                        P.op("dve", lambda e, c2=c2, b=b, bD=bD: e.tensor_tensor(out=hT[b][:, c2, :], in0=self.ps[bD][:], in1=hT[b][:, c2, :], op=ALU.add),
                             reads=[self.pst[bD], t_h[b]], writes=[t_h[b]])
                    P.op("sp", lambda e, b=b, tsl=tsl: e.dma_start(out=self.hT_d[:, :, tsl].rearrange("c p t -> p c t"), in_=hT[b][:]),
                         reads=[t_h[b]], dma=True)
                if half == 1 and l == 0:
                    self.dump("d_hid", hid[0][:], [128, 16, 512], BF16, t_hid[0])
                    self.dump("d_wu", Wu[:], [128, 8, 2048], BF16, t_w)
                    self.dump("d_wd", Wd[:], [128, 16, D], BF16, t_w)
                    self.dump("d_u2", u2[0][:], [128, 8, 512], BF16, t_u[0])
                P.barrier()

    def phase_O(self):
        P = self.P
        with ExitStack() as es:
            hT = [self.sb(es, "hTo%d" % i, [128, 8, 512], F32) for i in range(2)]
            t_h = toks(2)
            y = [self.sb(es, "yo%d" % i, [128, 8, 512], F32) for i in range(2)]
            t_y = toks(2)
            sq = [self.sb(es, "sqo%d" % i, [128, 8, 512], BF16) for i in range(2)]
            t_sq = toks(2)
            rs = [self.sb(es, "rso%d" % i, [128, 512], F32) for i in range(2)]
            t_rs = toks(2)
            ot = [self.sb(es, "ot%d" % i, [128, D], F32) for i in range(3)]
            t_ot = toks(3)
            oi = 0
            for tc in range(8):
                b = tc % 2
                tsl = slice(tc * 512, (tc + 1) * 512)
                P.op("sp", lambda e, b=b, tsl=tsl: e.dma_start(out=hT[b][:], in_=self.hT_d[:, :, tsl].rearrange("c p t -> p c t")),
                     writes=[t_h[b]], dma=True)
                self.norm_chunk(None, hT[b], t_h[b], lambda c: self.g_fin[:, c:c + 1],
                                lambda c, b=b: y[b][:, c, :], t_y[b], sq[b], t_sq[b], rs[b], t_rs[b], 6 + b, 0)
                for j in range(4):
                    o3 = oi % 3
                    oi += 1
                    for hh in range(2):
                        pb = (2 * j + hh) % 4
                        for q in range(4):
                            c = hh * 4 + q
                            P.op("pe", lambda e, b=b, c=c, j=j, q=q, pb=pb: e.transpose(
                                out=self.ps[pb][:, q * 128:(q + 1) * 128], in_=y[b][:, c, j * 128:(j + 1) * 128], identity=self.ident_f),
                                reads=[t_y[b], self.t_const], writes=[self.pst[pb]])
                        if hh == 0:
                            P.op("dve", lambda e, o3=o3, pb=pb: e.tensor_copy(out=ot[o3][:, 0:512], in_=self.ps[pb][:]),
                                 reads=[self.pst[pb]], writes=[t_ot[o3]])
                        else:
                            P.op("act", lambda e, o3=o3, pb=pb: e.activation(out=ot[o3][:, 512:1024], in_=self.ps[pb][:], func=AF.Copy),
                                 reads=[self.pst[pb]], writes=[t_ot[o3]])
                    r0 = tc * 512 + j * 128
                    P.op("sp", lambda e, o3=o3, r0=r0: e.dma_start(out=self.out[r0:r0 + 128, :], in_=ot[o3][:]),
                         reads=[t_ot[o3]], dma=True)
            P.barrier()


def make_consts():
    p = np.arange(128)
    half = 32
    inv = (10000.0 ** (-(np.arange(half, dtype=np.float32)) / half)).astype(np.float32)
    cvec = np.zeros((128, 4), np.float32)
    cvec[:, 0] = inv[p % 32]
    cvec[:, 1] = np.where((p % 64) < 32, -1.0, 1.0)
    cmat = np.zeros((128, 5, 128), np.float32)
    cmat[:, 0, :] = np.eye(128, dtype=np.float32)
    r = np.arange(128)[:, None]
    c = np.arange(128)[None, :]
    cmat[:, 1, :] = np.where(c > r, NEG, 0.0)
    cmat[:, 2, :] = np.where(r > c, NEG, 0.0)
    cmat[:, 3, :] = np.where(r < c, NEG, 0.0)
    cmat[64:, 4, :] = 1.0
    cvec[:, 2] = (p >= 64).astype(np.float32)
    cvec[:, 3] = (p < 64).astype(np.float32)
    masks = np.zeros((128, 9, 128), np.float32)
    masks[:, 0, :] = np.where(r > c, NEG, 0.0)
    masks[:, 1, :] = np.where(r < c, NEG, 0.0)
    for base, dl in ((2, 4), (5, 16)):
        res = ((c - r) % dl) == 0
        masks[:, base + 0, :] = np.where(res & (r <= c), 0.0, NEG)
        masks[:, base + 1, :] = np.where(res, 0.0, NEG)
        masks[:, base + 2, :] = np.where(res & (c <= r), 0.0, NEG)
    masks[:, 8, :] = np.where(r <= c, NEG, 0.0)
    return cvec, cmat, masks


def build_inputs(inputs, b):
    cvec, cmat, masks = make_consts()
    m = {
        "x": np.ascontiguousarray(inputs["x"][b]),
        "pos": np.ascontiguousarray(inputs["positions"][b]).astype(np.int32),
        "cvec": cvec, "cmat": cmat, "masks": masks,
    }
    for k in ("attn_norm", "w_in", "idx_k_norm", "sinks", "w_a", "w_b", "w_c", "w_o", "mlp_norm", "w_up",
              "w_down", "final_norm"):
        m[k] = np.ascontiguousarray(np.asarray(inputs[k], dtype=np.float32))
    return m


def kernel(**inputs):
    bld = Builder()
    nc = bld.build()
    n = 8
    in_maps = [build_inputs(inputs, b) for b in range(n)]
    res = run_bass_kernel_spmd(nc, in_maps, core_ids=list(range(n)))
    return np.stack([r["out"] for r in res.results], axis=0)
```

```python
import math
import os
from contextlib import ExitStack

import numpy as np
import concourse.bass as bass
import concourse.mybir as mybir
from concourse.bass_utils import run_bass_kernel_spmd

F32 = mybir.dt.float32
BF16 = mybir.dt.bfloat16
I32 = mybir.dt.int32
AF = mybir.ActivationFunctionType
ALU = mybir.AluOpType

L = 4096
D = 1024
DEPTH = 2
NEG = -30000.0
EPS = 1e-6
N_BISECT = 14

C_AQ, C_AK, C_AV, C_IQ, C_IK, C_IW = 0, 384, 448, 512, 1024, 1088
C_BQ, C_BK, C_BV, C_CQ, C_CK, C_CV, C_G = 1096, 1864, 2632, 3400, 3912, 4040, 4168

R_AQ, R_AKIK, R_IQ, R_BQ, R_BK, R_CQ, R_CK = 0, 384, 512, 1024, 1792, 2560, 3072
N_ROPED = 3200


class Tok:
    __slots__ = ("w", "rs", "rd")

    def __init__(self):
        self.w = None
        self.rs = {}
        self.rd = []


def toks(n):
    return [Tok() for _ in range(n)]


class _Op:
    __slots__ = ("eng", "fn", "waits", "sem", "val", "inc", "is_dma")


class Prog:
    ENGS = ("pe", "act", "dve", "pool", "sp")

    def __init__(self, nc, ndma=40):
        self.nc = nc
        self.ops = {e: [] for e in self.ENGS}
        self.cnt = {e: 0 for e in self.ENGS}
        self.waited = {}
        self.ndma = ndma
        self.dma_uses = [0] * ndma
        self.dma_rr = 0
        self.nops = 0

    def _wait(self, X, sem, val):
        key = (X.eng, sem)
        if self.waited.get(key, 0) >= val:
            return
        self.waited[key] = val
        X.waits.append((sem, val))

    def op(self, eng, fn, reads=(), writes=(), dma=False):
        X = _Op()
        X.eng = eng
        X.fn = fn
        X.waits = []
        X.is_dma = dma
        deps = []
        for t in reads:
            if t.w is not None:
                deps.append((t.w, 0))
        for t in writes:
            if t.w is not None:
                deps.append((t.w, 1))
            for r in t.rs.values():
                deps.append((r, 1))
            for r in t.rd:
                deps.append((r, 1))
        if dma:
            j = self.dma_rr
            self.dma_rr = (j + 1) % self.ndma
            k = self.dma_uses[j]
            self.dma_uses[j] += 1
            X.sem = ("d", j)
            X.val = 16 * (k + 1)
            X.inc = 16
            if k > 0:
                self._wait(X, ("d", j), 16 * k)
        else:
            self.cnt[eng] += 1
            X.sem = ("e", eng)
            X.val = self.cnt[eng]
            X.inc = 1
        for d, hz in deps:
            if d is X:
                continue
            if (not d.is_dma) and (not dma) and d.eng == eng and hz == 1:
                continue
            self._wait(X, d.sem, d.val)
        for t in reads:
            if dma:
                t.rd.append(X)
            else:
                t.rs[eng] = X
        for t in writes:
            t.w = X
            t.rs = {}
            t.rd = []
        self.ops[eng].append(X)
        self.nops += 1
        return X

    def barrier(self):
        snap = dict(self.cnt)
        sd = list(self.dma_uses)
        for e in self.ENGS:
            X = _Op()
            X.eng = e
            X.fn = None
            X.waits = []
            X.is_dma = False
            X.sem = None
            X.val = 0
            X.inc = 0
            for e2 in self.ENGS:
                if e2 != e and snap[e2] > 0:
                    self._wait(X, ("e", e2), snap[e2])
            for j in range(self.ndma):
                if sd[j] > 0:
                    self._wait(X, ("d", j), 16 * sd[j])
            self.ops[e].append(X)

    def emit(self):
        nc = self.nc
        with ExitStack() as es:
            sems = {}
            for e in self.ENGS:
                sems[("e", e)] = es.enter_context(nc.semaphore("s_" + e))
            for j in range(self.ndma):
                sems[("d", j)] = es.enter_context(nc.semaphore("d_%d" % j))
            block = es.enter_context(nc.Block())

            def run(ename):
                def body(eng):
                    for X in self.ops[ename]:
                        for (s, v) in X.waits:
                            eng.wait_ge(sems[s], v)
                        if X.fn is None:
                            continue
                        ins = X.fn(eng)
                        ins.then_inc(sems[X.sem], X.inc)
                return body

            block.tensor(run("pe"))
            block.scalar(run("act"))
            block.vector(run("dve"))
            block.gpsimd(run("pool"))
            block.sync(run("sp"))


def sap(t, off, dims, npart=128, pstart=0):
    fs = 1
    for s in list(t.shape)[1:]:
        fs *= int(s)
    return bass.AP(t, pstart * fs + off, [[fs, npart]] + [list(d) for d in dims])


class Builder:
    def __init__(self, dbg=None, stop_after=None, skip=()):
        self.skip = skip
        self.dbg = dbg or ()
        self.stop_after = stop_after
        nc = bass.Bass("TRN2", target_bir_lowering=False)
        self.nc = nc
        self.P = Prog(nc)
        self.outs = []

        def din(name, shape, dt=F32):
            return nc.dram_tensor(name, list(shape), dt, kind="ExternalInput").ap()

        self.x = din("x", [L, D])
        self.pos = din("pos", [L], I32)
        self.attn_norm = din("attn_norm", [DEPTH, D])
        self.w_in = din("w_in", [DEPTH, D, 7240])
        self.idx_k_norm = din("idx_k_norm", [DEPTH, 64])
        self.sinks = din("sinks", [DEPTH, 8])
        self.w_a = din("w_a", [DEPTH, 384, D])
        self.w_b = din("w_b", [DEPTH, 256, D])
        self.w_c = din("w_c", [DEPTH, 512, D])
        self.w_o = din("w_o", [DEPTH, D, D])
        self.mlp_norm = din("mlp_norm", [DEPTH, D])
        self.w_up = din("w_up", [DEPTH, D, 4 * D])
        self.w_down = din("w_down", [DEPTH, 4 * D, D])
        self.final_norm = din("final_norm", [D])
        self.cvec = din("cvec", [128, 4])
        self.cmat = din("cmat", [128, 5, 128])
        self.masks = din("masks", [128, 9, 128])

        self.out = nc.dram_tensor("out", [L, D], F32, kind="ExternalOutput").ap()

        self.cos_d = self.scr("cos_d", [128, L], F32)
        self.sin_d = self.scr("sin_d", [128, L], F32)
        self.hT_d = self.scr("hT_d", [8, 128, L], F32)
        self.uT_d = self.scr("uT_d", [8, 128, L], BF16)
        self.zT_d = self.scr("zT_d", [N_ROPED, L], BF16)
        self.bv_d = self.scr("bv_d", [L, 12, 65], BF16)
        self.cv_d = self.scr("cv_d", [L, 2, 65], BF16)
        self.av_d = self.scr("av_d", [L, 65], BF16)
        self.iw_d = self.scr("iw_d", [L, 8], F32)
        self.oaT_d = self.scr("oaT_d", [64, 6, L], BF16)
        self.obT_d = self.scr("obT_d", [64, 4, L], BF16)
        self.ocT_d = self.scr("ocT_d", [64, 8, L], BF16)

    def scr(self, name, shape, dt):
        kind = "ExternalOutput" if name in self.dbg else "Internal"
        t = self.nc.dram_tensor(name, list(shape), dt, kind=kind)
        if name in self.dbg:
            self.outs.append(name)
        return t.ap()

    def sb(self, es, name, shape, dt):
        self._sbn = getattr(self, "_sbn", 0) + 1
        return es.enter_context(self.nc.sbuf_tensor("%s_%d" % (name, self._sbn), list(shape), dt))

    def build(self):
        nc, P = self.nc, self.P
        with ExitStack() as es:
            self.ps = [es.enter_context(nc.psum_tensor("ps%d" % i, [128, 512], F32)) for i in range(8)]
            self.pst = toks(8)
            self.cvec_sb = self.sb(es, "cvec_sb", [128, 4], F32)
            self.cmat_sb = self.sb(es, "cmat_sb", [128, 5, 128], F32)
            self.ident_f = self.cmat_sb[:, 0, :]
            self.cb = self.sb(es, "cb", [128, 5, 128], BF16)
            self.ones_bf = self.sb(es, "ones_bf", [128, 128], BF16)
            self.ones_f = self.sb(es, "ones_f", [128, 128], F32)
            self.g_attn = self.sb(es, "g_attn", [128, DEPTH, 8], F32)
            self.g_mlp = self.sb(es, "g_mlp", [128, DEPTH, 8], F32)
            self.g_fin = self.sb(es, "g_fin", [128, 8], F32)
            self.gk = self.sb(es, "gk", [128, DEPTH, 2], F32)
            self.t_const = Tok()
            self.eps_t = self.sb(es, "eps_t", [128, 1], F32)
            P.op("dve", lambda e: e.memset(self.eps_t[:], EPS), writes=[self.t_const])
            self.phase_const()
            P.barrier()
            self.dump("d_gk", self.gk[:], [128, DEPTH, 2], F32, self.t_const)
            self.dump("d_gattn", self.g_attn[:], [128, DEPTH, 8], F32, self.t_const)
            if self.stop_after == "const":
                return self.finish()
            self.attn_consts(es)
            P.barrier()
            for l in range(DEPTH):
                self.phase_A(l)
                P.barrier()
                if self.stop_after in (("A", l), ("A1", l), ("A2a", l)):
                    return self.finish()
                for nm, fn in (("aC", self.phase_attnC), ("aB", self.phase_attnB), ("aA", self.phase_attnA),
                               ("M", self.phase_M), ("F", self.phase_F)):
                    if nm not in self.skip:
                        fn(l)
                    if self.stop_after == (nm, l):
                        return self.finish()
            self.phase_O()
            return self.finish()

    def dump(self, name, src_ap, shape, dt, tok):
        if name not in self.dbg:
            return
        t = self.nc.dram_tensor(name, list(shape), dt, kind="ExternalOutput").ap()
        self.outs.append(name)
        self.P.op("sp", lambda e: e.dma_start(out=t, in_=src_ap), reads=[tok], dma=True)

    def finish(self):
        self.P.barrier()
        self.P.emit()
        return self.nc

    def phase_const(self):
        nc, P = self.nc, self.P
        tc_ = self.t_const
        with ExitStack() as es:
            cosT = self.sb(es, "cosT", [128, L], F32)
            sinS = self.sb(es, "sinS", [128, L], F32)
            posi = self.sb(es, "posi", [128, L], I32)
            ang = self.sb(es, "ang", [128, L], F32)
            kk = self.sb(es, "kk", [128, L], F32)
            ki = self.sb(es, "ki", [128, L], I32)
            t1 = Tok(); t2 = Tok(); t3 = Tok(); t4 = Tok()
            P.op("sp", lambda e: e.dma_start(out=self.cvec_sb[:], in_=self.cvec), writes=[tc_], dma=True)
            P.op("sp", lambda e: e.dma_start(out=self.cmat_sb[:], in_=self.cmat), writes=[tc_], dma=True)
            P.op("sp", lambda e: e.dma_start(out=posi[:], in_=self.pos.partition_broadcast(128)), writes=[t1], dma=True)
            for (dst, src) in ((self.g_attn, self.attn_norm), (self.g_mlp, self.mlp_norm)):
                P.op("sp", lambda e, dst=dst, src=src: e.dma_start(
                    out=dst[:], in_=src.rearrange("l (c p) -> p l c", p=128),
                    allow_slow_non_contiguous=True), writes=[tc_], dma=True)
            P.op("sp", lambda e: e.dma_start(out=self.g_fin[:], in_=self.final_norm.rearrange("(c p) -> p c", p=128),
                                             allow_slow_non_contiguous=True), writes=[tc_], dma=True)
            P.op("dve", lambda e: e.memset(self.gk[:], 1.0), writes=[tc_])
            for l in range(DEPTH):
                src = self.idx_k_norm[l]
                P.op("sp", lambda e, l=l, src=src: e.dma_start(
                    out=self.gk[64:128, l, 0:1], in_=src.rearrange("(p o) -> p o", o=1),
                    allow_slow_non_contiguous=True), writes=[tc_], dma=True)
                P.op("sp", lambda e, l=l, src=src: e.dma_start(
                    out=self.gk[64:96, l, 1:2], in_=src[32:64].rearrange("(p o) -> p o", o=1),
                    allow_slow_non_contiguous=True), writes=[tc_], dma=True)
                P.op("sp", lambda e, l=l, src=src: e.dma_start(
                    out=self.gk[96:128, l, 1:2], in_=src[0:32].rearrange("(p o) -> p o", o=1),
                    allow_slow_non_contiguous=True), writes=[tc_], dma=True)
            P.op("dve", lambda e: e.memset(self.ones_bf[:], 1.0), writes=[tc_])
            P.op("dve", lambda e: e.memset(self.ones_f[:], 1.0), writes=[tc_])
            P.op("dve", lambda e: e.tensor_copy(out=self.cb[:], in_=self.cmat_sb[:]), reads=[tc_], writes=[tc_])
            P.op("dve", lambda e: e.tensor_copy(out=ang[:], in_=posi[:]), reads=[t1], writes=[t2])
            P.op("dve", lambda e: e.tensor_scalar(out=ang[:], in0=ang[:], scalar1=self.cvec_sb[:, 0:1], scalar2=None,
                                                  op0=ALU.mult), reads=[t2, tc_], writes=[t2])
            P.op("dve", lambda e: e.tensor_scalar(out=kk[:], in0=ang[:], scalar1=1.0 / (2 * math.pi), scalar2=0.5,
                                                  op0=ALU.mult, op1=ALU.add), reads=[t2], writes=[t3])
            P.op("dve", lambda e: e.tensor_copy(out=ki[:], in_=kk[:]), reads=[t3], writes=[t4])
            P.op("dve", lambda e: e.tensor_copy(out=kk[:], in_=ki[:]), reads=[t4], writes=[t3])
            C1 = 6.28125
            C2 = 2 * math.pi - C1
            P.op("dve", lambda e: e.scalar_tensor_tensor(out=ang[:], in0=kk[:], scalar=-C1, in1=ang[:],
                                                         op0=ALU.mult, op1=ALU.add), reads=[t3, t2], writes=[t2])
            P.op("dve", lambda e: e.scalar_tensor_tensor(out=ang[:], in0=kk[:], scalar=-C2, in1=ang[:],
                                                         op0=ALU.mult, op1=ALU.add), reads=[t3, t2], writes=[t2])
            P.op("dve", lambda e: e.tensor_scalar(out=kk[:], in0=ang[:], scalar1=-math.pi, scalar2=2 * math.pi,
                                                  op0=ALU.is_lt, op1=ALU.mult), reads=[t2], writes=[t3])
            P.op("dve", lambda e: e.tensor_tensor(out=ang[:], in0=ang[:], in1=kk[:], op=ALU.add),
                 reads=[t2, t3], writes=[t2])
            P.op("dve", lambda e: e.tensor_scalar(out=kk[:], in0=ang[:], scalar1=math.pi, scalar2=-2 * math.pi,
                                                  op0=ALU.is_gt, op1=ALU.mult), reads=[t2], writes=[t3])
            P.op("dve", lambda e: e.tensor_tensor(out=ang[:], in0=ang[:], in1=kk[:], op=ALU.add),
                 reads=[t2, t3], writes=[t2])
            P.op("dve", lambda e: e.tensor_scalar(out=ang[:], in0=ang[:], scalar1=-3.1415925, scalar2=3.1415925,
                                                  op0=ALU.max, op1=ALU.min), reads=[t2], writes=[t2])
            P.op("act", lambda e: e.activation(out=sinS[:], in_=ang[:], func=AF.Sin), reads=[t2], writes=[tc_])
            P.op("dve", lambda e: e.tensor_scalar(out=sinS[:], in0=sinS[:], scalar1=self.cvec_sb[:, 1:2],
                                                  scalar2=None, op0=ALU.mult), reads=[tc_], writes=[tc_])
            P.op("dve", lambda e: e.tensor_scalar(out=kk[:], in0=ang[:], scalar1=-1.0, scalar2=None,
                                                  op0=ALU.mult), reads=[t2], writes=[t3])
            P.op("dve", lambda e: e.tensor_tensor(out=kk[:], in0=kk[:], in1=ang[:], op=ALU.max),
                 reads=[t2, t3], writes=[t3])
            P.op("dve", lambda e: e.tensor_scalar(out=kk[:], in0=kk[:], scalar1=-1.0, scalar2=math.pi / 2,
                                                  op0=ALU.mult, op1=ALU.add), reads=[t3], writes=[t3])
            P.op("act", lambda e: e.activation(out=cosT[:], in_=kk[:], func=AF.Sin), reads=[t3], writes=[tc_])
            P.op("sp", lambda e: e.dma_start(out=self.cos_d, in_=cosT[:]), reads=[tc_], dma=True)
            P.op("sp", lambda e: e.dma_start(out=self.sin_d, in_=sinS[:]), reads=[tc_], dma=True)
            P.barrier()

    def load_w(self, dst, l_w_ap, col0, ncols, dcol0, tok):
        src = l_w_ap[:, col0:col0 + ncols].rearrange("(kc p) c -> p kc c", p=128)
        self.P.op("pool", lambda e: e.dma_start(out=dst[:, :, dcol0:dcol0 + ncols], in_=src),
                  writes=[tok], dma=True)

    def norm_chunk(self, es_names, hT, t_h, gcol, uT_out_fn, t_u, sq, t_sq, rs, t_rs, psb, l_tag):
        P = self.P
        P.op("act", lambda e: e.activation(out=sq[:], in_=hT[:], func=AF.Square), reads=[t_h], writes=[t_sq])
        for c in range(8):
            P.op("pe", lambda e, c=c: e.matmul(self.ps[psb][:], lhsT=self.ones_bf[:], rhs=sq[:, c, :],
                                               start=(c == 0), stop=(c == 7)),
                 reads=[t_sq, self.t_const], writes=[self.pst[psb]])
        P.op("act", lambda e: e.activation(out=rs[:], in_=self.ps[psb][:], func=AF.Ln, scale=1.0 / D, bias=self.eps_t[:, 0:1]),
             reads=[self.pst[psb], self.t_const], writes=[t_rs])
        P.op("act", lambda e: e.activation(out=rs[:], in_=rs[:], func=AF.Exp, scale=-0.5), reads=[t_rs], writes=[t_rs])
        for c in range(8):
            P.op("dve", lambda e, c=c: e.scalar_tensor_tensor(out=uT_out_fn(c), in0=hT[:, c, :], scalar=gcol(c),
                                                              in1=rs[:], op0=ALU.mult, op1=ALU.mult),
                 reads=[t_h, t_rs, self.t_const], writes=[t_u])

    def phase_A(self, l):
        nc, P = self.nc, self.P
        w_in = self.w_in[l]
        with ExitStack() as es:
            uT = self.sb(es, "uT", [128, 8, L], BF16)
            t_uT = toks(8)
            with ExitStack() as es1:
                hT = [self.sb(es1, "hT%d" % i, [128, 8, 512], F32) for i in range(2)]
                t_h = toks(2)
                sq = [self.sb(es1, "sq%d" % i, [128, 8, 512], BF16) for i in range(2)]
                t_sq = toks(2)
                rs = [self.sb(es1, "rs%d" % i, [128, 512], F32) for i in range(2)]
                t_rs = toks(2)
                if l == 0:
                    xt = [self.sb(es1, "xt%d" % i, [128, D], F32) for i in range(3)]
                    t_x = toks(3)
                xi = 0
                for tc in range(8):
                    b = tc % 2
                    if l == 0:
                        for j in range(4):
                            ti = tc * 4 + j
                            xb = xi % 3
                            xi += 1
                            P.op("sp", lambda e, xb=xb, ti=ti: e.dma_start(out=xt[xb][:], in_=self.x[ti * 128:(ti + 1) * 128, :]),
                                 writes=[t_x[xb]], dma=True)
                            for half in range(2):
                                pb = (2 * j + half) % 4
                                for q in range(4):
                                    c = half * 4 + q
                                    P.op("pe", lambda e, xb=xb, c=c, pb=pb, q=q: e.transpose(
                                        out=self.ps[pb][:, q * 128:(q + 1) * 128], in_=xt[xb][:, c * 128:(c + 1) * 128],
                                        identity=self.ident_f), reads=[t_x[xb], self.t_const], writes=[self.pst[pb]])
                                eng = "dve" if half == 0 else "act"
                                if eng == "dve":
                                    P.op("dve", lambda e, b=b, half=half, j=j, pb=pb: e.tensor_copy(
                                        out=hT[b][:, half * 4:half * 4 + 4, j * 128:(j + 1) * 128],
                                        in_=self.ps[pb][:].rearrange("p (q t) -> p q t", q=4)),
                                        reads=[self.pst[pb]], writes=[t_h[b]])
                                else:
                                    P.op("act", lambda e, b=b, half=half, j=j, pb=pb: e.activation(
                                        out=hT[b][:, half * 4:half * 4 + 4, j * 128:(j + 1) * 128],
                                        in_=self.ps[pb][:].rearrange("p (q t) -> p q t", q=4), func=AF.Copy),
                                        reads=[self.pst[pb]], writes=[t_h[b]])
                        P.op("sp", lambda e, b=b, tc=tc: e.dma_start(
                            out=self.hT_d[:, :, tc * 512:(tc + 1) * 512].rearrange("c p t -> p c t"), in_=hT[b][:]),
                            reads=[t_h[b]], dma=True)
                    else:
                        P.op("sp", lambda e, b=b, tc=tc: e.dma_start(
                            out=hT[b][:], in_=self.hT_d[:, :, tc * 512:(tc + 1) * 512].rearrange("c p t -> p c t")),
                            writes=[t_h[b]], dma=True)
                    self.norm_chunk(None, hT[b], t_h[b], lambda c: self.g_attn[:, l, c:c + 1],
                                    lambda c, tc=tc: uT[:, c, tc * 512:(tc + 1) * 512], t_uT[tc],
                                    sq[b], t_sq[b], rs[b], t_rs[b], 4 + b, l)
                    P.op("sp", lambda e, tc=tc: e.dma_start(
                        out=self.uT_d[:, :, tc * 512:(tc + 1) * 512].rearrange("c p t -> p c t"),
                        in_=uT[:, :, tc * 512:(tc + 1) * 512]), reads=[t_uT[tc]], dma=True)
                P.barrier()
            if self.stop_after == ("A1", l):
                return
            with ExitStack() as es2:
                cosT = self.sb(es2, "cosT", [128, L], F32)
                sinS = self.sb(es2, "sinS", [128, L], F32)
                P.op("sp", lambda e: e.dma_start(out=cosT[:], in_=self.cos_d), writes=[self.t_const], dma=True)
                P.op("sp", lambda e: e.dma_start(out=sinS[:], in_=self.sin_d), writes=[self.t_const], dma=True)
                W = [self.sb(es2, "W%d" % i, [128, 8, 512], BF16) for i in range(2)]
                Ws = [self.sb(es2, "Ws%d" % i, [128, 8, 512], BF16) for i in range(2)]
                t_W = toks(2)
                t_Ws = toks(2)
                r1 = [self.sb(es2, "r1_%d" % i, [128, 512], F32) for i in range(2)]
                r2 = [self.sb(es2, "r2_%d" % i, [128, 512], F32) for i in range(2)]
                t_r1 = toks(2)
                t_r2 = toks(2)
                ro = [self.sb(es2, "ro%d" % i, [128, 512], BF16) for i in range(3)]
                t_ro = toks(3)
                sqk = self.sb(es2, "sqk", [128, 512], BF16)
                t_sqk = Tok()
                fk = self.sb(es2, "fk", [128, 512], F32)
                t_fk = Tok()
                groups = [
                    (R_AQ, [(C_AQ, 384), (C_AK, 64), (C_IK, 64)]),
                    (R_IQ, [(C_IQ, 512)]),
                    (R_BQ, [(C_BQ, 512)]),
                    (R_BQ + 512, [(C_BQ + 512, 256), (C_BK, 256)]),
                    (R_BK + 256, [(C_BK + 256, 512)]),
                    (R_CQ, [(C_CQ, 512)]),
                    (R_CK, [(C_CK, 128)]),
                ]
                rr = 0
                ri = 0
                for gi, (row0, pieces) in enumerate(groups):
                    wb = gi % 2
                    dc = 0
                    for (c0, ncol) in pieces:
                        self.load_w(W[wb], w_in, c0, ncol, dc, t_W[wb])
                        dc += ncol
                    ncols = dc
                    nh = ncols // 64
                    wv = W[wb][:, :, 0:ncols].rearrange("p k (h two d) -> p k h two d", two=2, d=32)
                    wsv = Ws[wb][:, :, 0:ncols].rearrange("p k (h two d) -> p k h two d", two=2, d=32)
                    for k in range(8):
                        P.op("act", lambda e, k=k, wv=wv, wsv=wsv: e.activation(out=wsv[:, k, :, 0, :], in_=wv[:, k, :, 1, :], func=AF.Copy),
                             reads=[t_W[wb]], writes=[t_Ws[wb]])
                        P.op("pool", lambda e, k=k, wv=wv, wsv=wsv: e.tensor_copy(out=wsv[:, k, :, 1, :], in_=wv[:, k, :, 0, :]),
                             reads=[t_W[wb]], writes=[t_Ws[wb]])
                    for tc in range(8):
                        tsl = slice(tc * 512, (tc + 1) * 512)
                        for j in range(ncols // 128):
                            row = row0 + j * 128
                            is_kik = (row == R_AKIK)
                            pa, pb_ = 0 + (rr % 2) * 2, 1 + (rr % 2) * 2
                            rb = rr % 2
                            rr += 1
                            for k in range(8):
                                P.op("pe", lambda e, k=k, j=j, pa=pa, wb=wb, tsl=tsl: e.matmul(
                                    self.ps[pa][:], lhsT=W[wb][:, k, j * 128:(j + 1) * 128], rhs=uT[:, k, tsl],
                                    start=(k == 0), stop=(k == 7)), reads=[t_W[wb], t_uT[tc]], writes=[self.pst[pa]])
                            for k in range(8):
                                P.op("pe", lambda e, k=k, j=j, pb_=pb_, wb=wb, tsl=tsl: e.matmul(
                                    self.ps[pb_][:], lhsT=Ws[wb][:, k, j * 128:(j + 1) * 128], rhs=uT[:, k, tsl],
                                    start=(k == 0), stop=(k == 7)), reads=[t_Ws[wb], t_uT[tc]], writes=[self.pst[pb_]])
                            ob = ri % 3
                            ri += 1
                            if not is_kik:
                                P.op("dve", lambda e, pa=pa, rb=rb, tsl=tsl: e.tensor_tensor(
                                    out=r1[rb][:], in0=self.ps[pa][:], in1=cosT[:, tsl], op=ALU.mult),
                                    reads=[self.pst[pa], self.t_const], writes=[t_r1[rb]])
                                P.op("dve", lambda e, pb_=pb_, rb=rb, tsl=tsl: e.tensor_tensor(
                                    out=r2[rb][:], in0=self.ps[pb_][:], in1=sinS[:, tsl], op=ALU.mult),
                                    reads=[self.pst[pb_], self.t_const], writes=[t_r2[rb]])
                                P.op("pool", lambda e, rb=rb, ob=ob: e.tensor_tensor(
                                    out=ro[ob][:], in0=r1[rb][:], in1=r2[rb][:], op=ALU.add),
                                    reads=[t_r1[rb], t_r2[rb]], writes=[t_ro[ob]])
                            else:
                                P.op("act", lambda e, pa=pa: e.activation(out=sqk[:], in_=self.ps[pa][:], func=AF.Square),
                                     reads=[self.pst[pa]], writes=[t_sqk])
                                P.op("pe", lambda e: e.matmul(self.ps[6][:], lhsT=self.cb[:, 4, :], rhs=sqk[:],
                                                              start=True, stop=True),
                                     reads=[t_sqk, self.t_const], writes=[self.pst[6]])
                                P.op("act", lambda e: e.activation(out=fk[:], in_=self.ps[6][:], func=AF.Ln,
                                                                   scale=1.0 / 64, bias=self.eps_t[:, 0:1]),
                                     reads=[self.pst[6], self.t_const], writes=[t_fk])
                                P.op("act", lambda e: e.activation(out=fk[:], in_=fk[:], func=AF.Exp, scale=-0.5),
                                     reads=[t_fk], writes=[t_fk])
                                P.op("dve", lambda e: e.tensor_scalar(out=fk[:], in0=fk[:], scalar1=self.cvec_sb[:, 2:3],
                                                                      scalar2=self.cvec_sb[:, 3:4], op0=ALU.mult, op1=ALU.add),
                                     reads=[t_fk, self.t_const], writes=[t_fk])
                                P.op("dve", lambda e, pa=pa, rb=rb, tsl=tsl: e.scalar_tensor_tensor(
                                    out=r1[rb][:], in0=self.ps[pa][:], scalar=self.gk[:, l, 0:1], in1=cosT[:, tsl],
                                    op0=ALU.mult, op1=ALU.mult),
                                    reads=[self.pst[pa], self.t_const], writes=[t_r1[rb]])
                                P.op("dve", lambda e, pb_=pb_, rb=rb, tsl=tsl: e.scalar_tensor_tensor(
                                    out=r2[rb][:], in0=self.ps[pb_][:], scalar=self.gk[:, l, 1:2], in1=sinS[:, tsl],
                                    op0=ALU.mult, op1=ALU.mult),
                                    reads=[self.pst[pb_], self.t_const], writes=[t_r2[rb]])
                                P.op("pool", lambda e, rb=rb: e.tensor_tensor(
                                    out=r1[rb][:], in0=r1[rb][:], in1=r2[rb][:], op=ALU.add),
                                    reads=[t_r1[rb], t_r2[rb]], writes=[t_r1[rb]])
                                P.op("pool", lambda e, rb=rb, ob=ob: e.tensor_tensor(
                                    out=ro[ob][:], in0=r1[rb][:], in1=fk[:], op=ALU.mult),
                                    reads=[t_r1[rb], t_fk], writes=[t_ro[ob]])
                            P.op("sp", lambda e, ob=ob, row=row, tsl=tsl: e.dma_start(
                                out=self.zT_d[row:row + 128, tsl], in_=ro[ob][:]), reads=[t_ro[ob]], dma=True)
                P.barrier()
            if self.stop_after == ("A2a", l):
                return
            with ExitStack() as es3:
                WV = [self.sb(es3, "WV%d" % i, [128, 8, 512], BF16) for i in range(2)]
                t_WV = toks(2)
                self.load_w(WV[0], w_in, C_BV, 512, 0, t_WV[0])
                self.load_w(WV[1], w_in, C_BV + 512, 256, 0, t_WV[1])
                self.load_w(WV[1], w_in, C_CV, 128, 256, t_WV[1])
                self.load_w(WV[1], w_in, C_AV, 64, 384, t_WV[1])
                self.load_w(WV[1], w_in, C_IW, 8, 448, t_WV[1])
                bvs = [self.sb(es3, "bvs%d" % i, [128, 12, 65], BF16) for i in range(2)]
                cvs = [self.sb(es3, "cvs%d" % i, [128, 2, 65], BF16) for i in range(2)]
                avs = [self.sb(es3, "avs%d" % i, [128, 65], BF16) for i in range(2)]
                iws = [self.sb(es3, "iws%d" % i, [128, 8], F32) for i in range(2)]
                t_st = toks(2)
                for i in range(2):
                    P.op("dve", lambda e, i=i: e.memset(bvs[i][:], 1.0), writes=[t_st[i]])
                    P.op("dve", lambda e, i=i: e.memset(cvs[i][:], 1.0), writes=[t_st[i]])
                    P.op("dve", lambda e, i=i: e.memset(avs[i][:], 1.0), writes=[t_st[i]])
                for ti in range(32):
                    b = ti % 2
                    tcs = ti // 4
                    tk = slice(ti * 128, (ti + 1) * 128)
                    for k in range(8):
                        P.op("pe", lambda e, k=k, tk=tk, b=b: e.matmul(self.ps[b * 2][:], lhsT=uT[:, k, tk], rhs=WV[0][:, k, :],
                                                                     start=(k == 0), stop=(k == 7)),
                             reads=[t_uT[tcs], t_WV[0]], writes=[self.pst[b * 2]])
                    for k in range(8):
                        P.op("pe", lambda e, k=k, tk=tk, b=b: e.matmul(self.ps[b * 2 + 1][:, 0:456], lhsT=uT[:, k, tk], rhs=WV[1][:, k, 0:456],
                                                                     start=(k == 0), stop=(k == 7)),
                             reads=[t_uT[tcs], t_WV[1]], writes=[self.pst[b * 2 + 1]])
                    p0, p1 = self.ps[b * 2], self.ps[b * 2 + 1]
                    P.op("dve", lambda e, b=b, p0=p0: e.tensor_copy(out=bvs[b][:, 0:8, 0:64], in_=p0[:].rearrange("p (h d) -> p h d", d=64)),
                         reads=[self.pst[b * 2]], writes=[t_st[b]])
                    P.op("act", lambda e, b=b, p1=p1: e.activation(out=bvs[b][:, 8:12, 0:64], in_=p1[:, 0:256].rearrange("p (h d) -> p h d", d=64), func=AF.Copy),
                         reads=[self.pst[b * 2 + 1]], writes=[t_st[b]])
                    P.op("dve", lambda e, b=b, p1=p1: e.tensor_copy(out=cvs[b][:, :, 0:64], in_=p1[:, 256:384].rearrange("p (h d) -> p h d", d=64)),
                         reads=[self.pst[b * 2 + 1]], writes=[t_st[b]])
                    P.op("act", lambda e, b=b, p1=p1: e.activation(out=avs[b][:, 0:64], in_=p1[:, 384:448], func=AF.Copy),
                         reads=[self.pst[b * 2 + 1]], writes=[t_st[b]])
                    P.op("dve", lambda e, b=b, p1=p1: e.tensor_copy(out=iws[b][:], in_=p1[:, 448:456]),
                         reads=[self.pst[b * 2 + 1]], writes=[t_st[b]])
                    P.op("sp", lambda e, b=b, tk=tk: e.dma_start(out=self.bv_d[tk], in_=bvs[b][:]), reads=[t_st[b]], dma=True)
                    P.op("sp", lambda e, b=b, tk=tk: e.dma_start(out=self.cv_d[tk], in_=cvs[b][:]), reads=[t_st[b]], dma=True)
                    P.op("sp", lambda e, b=b, tk=tk: e.dma_start(out=self.av_d[tk], in_=avs[b][:]), reads=[t_st[b]], dma=True)
                    P.op("sp", lambda e, b=b, tk=tk: e.dma_start(out=self.iw_d[tk], in_=iws[b][:]), reads=[t_st[b]], dma=True)
                P.barrier()


    def attn_consts(self, es):
        P = self.P
        self.mrep = self.sb(es, "mrep", [128, 9, 4, 128], BF16)
        self.irep = self.sb(es, "irep", [128, 3, 128], BF16)
        self.zeros_bf = self.sb(es, "zeros_bf", [128, 64], BF16)
        mf = self.sb(es, "masks_f", [128, 9, 128], F32)
        t = Tok()
        P.op("sp", lambda e: e.dma_start(out=mf[:], in_=self.masks), writes=[t], dma=True)
        P.op("dve", lambda e: e.tensor_copy(out=self.mrep[:], in_=sap(mf, 0, [[128, 9], [0, 4], [1, 128]])),
             reads=[t], writes=[self.t_const])
        P.op("dve", lambda e: e.tensor_copy(out=self.irep[:], in_=sap(self.cmat_sb, 0, [[0, 3], [1, 128]])),
             reads=[self.t_const], writes=[self.t_const])
        P.op("dve", lambda e: e.memset(self.zeros_bf[:], 0.0), writes=[self.t_const])

    def normalize_out(self, psO, psD, tO, tD, n, rc, t_rc, out_ap, t_out):
        P = self.P
        P.op("act", lambda e: e.activation(out=rc[0:64, 0:n], in_=psD[0:64, 0:n], func=AF.Ln), reads=[tD], writes=[t_rc])
        P.op("act", lambda e: e.activation(out=rc[0:64, 0:n], in_=rc[0:64, 0:n], func=AF.Exp, scale=-1.0),
             reads=[t_rc], writes=[t_rc])
        P.op("dve", lambda e: e.tensor_tensor(out=out_ap, in0=psO[0:64, 0:n], in1=rc[0:64, 0:n], op=ALU.mult),
             reads=[tO, t_rc], writes=[t_out])


    def run_pipe(self, stages):
        pending = None
        for (s_fn, pv_fn) in stages:
            s_fn()
            if pending is not None:
                pending()
            pending = pv_fn
        if pending is not None:
            pending()

    def normalize_out2(self, psO, psD, tO, tD, n, rc, t_rc, osb, t_osb, out_ap, t_out):
        P = self.P
        P.op("act", lambda e: e.activation(out=rc[0:64, 0:n], in_=psD[0:64, 0:n], func=AF.Ln), reads=[tD], writes=[t_rc])
        P.op("act", lambda e: e.activation(out=rc[0:64, 0:n], in_=rc[0:64, 0:n], func=AF.Exp, scale=-1.0),
             reads=[t_rc], writes=[t_rc])
        P.op("act", lambda e: e.activation(out=osb[0:64, 0:n], in_=psO[0:64, 0:n], func=AF.Copy), reads=[tO], writes=[t_osb])
        P.op("pool", lambda e: e.tensor_tensor(out=out_ap, in0=osb[0:64, 0:n], in1=rc[0:64, 0:n], op=ALU.mult),
             reads=[t_osb, t_rc], writes=[t_out])

    def phase_attnC(self, l):
        P = self.P
        with ExitStack() as es:
            ckT = self.sb(es, "ckT", [64, 2, L], BF16)
            cv = self.sb(es, "cv", [128, 32, 2, 65], BF16)
            t_k = Tok(); t_v = Tok()
            P.op("sp", lambda e: e.dma_start(out=ckT[:], in_=self.zT_d[R_CK:R_CK + 128, :].rearrange("(g d) t -> d g t", d=64)),
                 writes=[t_k], dma=True)
            P.op("sp", lambda e: e.dma_start(out=cv[:], in_=self.cv_d.rearrange("(b p) g e -> p b g e", p=128)),
                 writes=[t_v], dma=True)
            sk = self.sb(es, "sk", [1, 8], F32)
            skrow = self.sb(es, "skrow", [1, 8, 128], F32)
            t_sk = Tok()
            P.op("sp", lambda e: e.dma_start(out=sk[:], in_=self.sinks[l:l + 1, :]), writes=[t_sk], dma=True)
            P.op("act", lambda e: e.activation(out=sk[:], in_=sk[:], func=AF.Exp), reads=[t_sk], writes=[t_sk])
            P.op("dve", lambda e: e.tensor_copy(out=skrow[:], in_=sap(sk, 0, [[1, 8], [0, 128]], npart=1)),
                 reads=[t_sk], writes=[t_sk])
            q = [self.sb(es, "cq%d" % i, [64, 8, 128], BF16) for i in range(3)]
            t_q = toks(3)
            pT = [self.sb(es, "pT%d" % i, [128, 512], BF16) for i in range(3)]
            t_pT = toks(3)
            rc = [self.sb(es, "rc%d" % i, [64, 512], F32) for i in range(2)]
            t_rc = toks(2)
            osb = [self.sb(es, "osb%d" % i, [64, 512], F32) for i in range(2)]
            t_osb = toks(2)
            oT = [self.sb(es, "oT%d" % i, [64, 8, 128], BF16) for i in range(2)]
            t_oT = toks(2)
            SB = (0, 1, 6, 7)

            def load_q(qb):
                qi = qb % 3
                qs = slice(qb * 128, (qb + 1) * 128)
                P.op("sp", lambda e: e.dma_start(out=q[qi][:], in_=self.zT_d[R_CQ:R_CQ + 512, qs].rearrange("(h d) t -> d h t", d=64)),
                     writes=[t_q[qi]], dma=True)

            def make(n, m, qb, g, n_, nkb, kb, mi):
                qi, ob = qb % 3, qb % 2
                bS, pb = SB[n % 4], n % 3
                bO, bD, r2 = 2 + m % 2, 4 + m % 2, m % 2
                psS, tS = self.ps[bS], self.pst[bS]
                qs = slice(qb * 128, (qb + 1) * 128)

                def s_fn():
                    if g == 0 and n_ == 0 and qb + 1 < 32:
                        load_q(qb + 1)
                    P.op("pe", lambda e: e.matmul(psS[:], lhsT=self.cb[:, 0, :], rhs=self.mrep[:, mi].rearrange("p h t -> p (h t)"),
                                                  start=True, stop=False), reads=[self.t_const], writes=[tS])
                    P.op("pe", lambda e: e.matmul(psS[:], lhsT=ckT[:, g, kb * 128:(kb + 1) * 128], rhs=q[qi][:, 4 * g:4 * g + 4, :],
                                                  start=False, stop=True), reads=[t_k, t_q[qi]], writes=[tS])
                    P.op("act", lambda e: e.activation(out=pT[pb][:], in_=psS[:], func=AF.Exp, scale=0.125),
                         reads=[tS], writes=[t_pT[pb]])

                def pv_fn():
                    P.op("pe", lambda e: e.matmul(self.ps[bO][0:64, :], lhsT=cv[:, kb, g, 0:64], rhs=pT[pb][:], start=(n_ == 0),
                                                  stop=(n_ == nkb - 1)), reads=[t_v, t_pT[pb]], writes=[self.pst[bO]])
                    P.op("pe", lambda e: e.matmul(self.ps[bD][0:64, :], lhsT=self.ones_bf[:, 0:64], rhs=pT[pb][:], start=(n_ == 0),
                                                  stop=False), reads=[self.t_const, t_pT[pb]], writes=[self.pst[bD]])
                    if n_ == nkb - 1:
                        P.op("pe", lambda e: e.matmul(self.ps[bD][0:64, :], lhsT=self.ones_f[0:1, 0:64],
                                                      rhs=skrow[0:1, 4 * g:4 * g + 4, :], start=False, stop=True),
                             reads=[self.t_const, t_sk], writes=[self.pst[bD]])
                        self.normalize_out2(self.ps[bO], self.ps[bD], self.pst[bO], self.pst[bD], 512, rc[r2], t_rc[r2],
                                            osb[r2], t_osb[r2], oT[ob][:, 4 * g:4 * g + 4, :].rearrange("p h t -> p (h t)"), t_oT[ob])
                        if g == 1:
                            P.op("sp", lambda e: e.dma_start(out=self.ocT_d[:, :, qs], in_=oT[ob][:]), reads=[t_oT[ob]], dma=True)
                return s_fn, pv_fn

            load_q(0)
            stages = []
            n = 0
            m = 0
            for qb in range(32):
                for g in range(2):
                    kbs = ([(qb - 1, 8)] if qb > 0 else []) + [(qb, 0)]
                    for n_, (kb, mi) in enumerate(kbs):
                        stages.append(make(n, m, qb, g, n_, len(kbs), kb, mi))
                        n += 1
                    m += 1
            self.run_pipe(stages)
            P.barrier()

    def phase_attnB(self, l):
        P = self.P
        with ExitStack() as es:
            bkT = self.sb(es, "bkT", [64, 12, L], BF16)
            bv = self.sb(es, "bv", [128, 32, 12, 65], BF16)
            t_k = Tok(); t_v = Tok()
            for g in range(3):
                P.op("sp", lambda e, g=g: e.dma_start(
                    out=bkT[:, 4 * g:4 * g + 4, :],
                    in_=self.zT_d[R_BK + 256 * g:R_BK + 256 * (g + 1), :].rearrange("(h d) t -> d h t", d=64)),
                    writes=[t_k], dma=True)
            for b4 in range(4):
                P.op("sp", lambda e, b4=b4: e.dma_start(
                    out=bv[:, 8 * b4:8 * b4 + 8],
                    in_=self.bv_d[1024 * b4:1024 * (b4 + 1)].rearrange("(b p) h e -> p b h e", p=128)),
                    writes=[t_v], dma=True)
            q = [self.sb(es, "bq%d" % i, [64, 12, 128], BF16) for i in range(3)]
            t_q = toks(3)
            pT = [self.sb(es, "pT%d" % i, [128, 4, 128], BF16) for i in range(3)]
            t_pT = toks(3)
            rc = [self.sb(es, "rc%d" % i, [64, 512], F32) for i in range(2)]
            t_rc = toks(2)
            osb = [self.sb(es, "osb%d" % i, [64, 512], F32) for i in range(2)]
            t_osb = toks(2)
            oT = [self.sb(es, "oT%d" % i, [64, 4, 128], BF16) for i in range(2)]
            t_oT = toks(2)

            def load_q(qb):
                qi = qb % 3
                qs = slice(qb * 128, (qb + 1) * 128)
                P.op("sp", lambda e: e.dma_start(out=q[qi][:], in_=self.zT_d[R_BQ:R_BQ + 768, qs].rearrange("(h d) t -> d h t", d=64)),
                     writes=[t_q[qi]], dma=True)

            def make(n, qb, n_, nit, g, kb, mi):
                qi, ob = qb % 3, qb % 2
                bS, pb = n % 4, n % 3
                bO, bD = 4 + qb % 2, 6 + qb % 2
                psS, tS = self.ps[bS], self.pst[bS]
                qs = slice(qb * 128, (qb + 1) * 128)
                last = (n_ == nit - 1)

                def s_fn():
                    if n_ == 0 and qb + 1 < 32:
                        load_q(qb + 1)
                    P.op("pe", lambda e: e.matmul(psS[:], lhsT=self.cb[:, 0, :], rhs=self.mrep[:, mi].rearrange("p h t -> p (h t)"),
                                                  start=True, stop=False), reads=[self.t_const], writes=[tS])
                    for j in range(4):
                        P.op("pe", lambda e, j=j: e.matmul(psS[:, j * 128:(j + 1) * 128], lhsT=bkT[:, 4 * g + j, kb * 128:(kb + 1) * 128],
                                                           rhs=q[qi][:, 4 * g + j, :], start=False, stop=(j == 3)),
                             reads=[t_k, t_q[qi]], writes=[tS])
                    P.op("act", lambda e: e.activation(out=pT[pb][:].rearrange("p h t -> p (h t)"), in_=psS[:], func=AF.Exp, scale=0.125),
                         reads=[tS], writes=[t_pT[pb]])

                def pv_fn():
                    if n_ == 0:
                        for bb in (bO, bD):
                            P.op("pe", lambda e, bb=bb: e.matmul(self.ps[bb][0:64, :], lhsT=self.zeros_bf[:, 0:64],
                                                               rhs=self.mrep[:, 0].rearrange("p h t -> p (h t)"), start=True, stop=False),
                                 reads=[self.t_const], writes=[self.pst[bb]])
                    for j in range(4):
                        P.op("pe", lambda e, j=j: e.matmul(self.ps[bO][0:64, j * 128:(j + 1) * 128], lhsT=bv[:, kb, 4 * g + j, 0:64],
                                                           rhs=pT[pb][:, j, :], start=False, stop=(last and j == 3)),
                             reads=[t_v, t_pT[pb]], writes=[self.pst[bO]])
                    P.op("pe", lambda e: e.matmul(self.ps[bD][0:64, :], lhsT=self.ones_bf[:, 0:64], rhs=pT[pb][:].rearrange("p h t -> p (h t)"),
                                                  start=False, stop=last), reads=[self.t_const, t_pT[pb]], writes=[self.pst[bD]])
                    if last:
                        self.normalize_out2(self.ps[bO], self.ps[bD], self.pst[bO], self.pst[bD], 512, rc[ob], t_rc[ob],
                                            osb[ob], t_osb[ob], oT[ob][:].rearrange("p h t -> p (h t)"), t_oT[ob])
                        P.op("sp", lambda e: e.dma_start(out=self.obT_d[:, :, qs], in_=oT[ob][:]), reads=[t_oT[ob]], dma=True)
                return s_fn, pv_fn

            load_q(0)
            stages = []
            n = 0
            for qb in range(32):
                items = []
                for g, Dl in enumerate((1, 4, 16)):
                    for o in range(Dl + 1):
                        kb = qb - o
                        if kb < 0:
                            break
                        if Dl == 1:
                            mi = 0 if o == 0 else 1
                        else:
                            base = 2 if Dl == 4 else 5
                            mi = base + (0 if o == 0 else (2 if o == Dl else 1))
                        items.append((g, kb, mi))
                for n_, (g, kb, mi) in enumerate(items):
                    stages.append(make(n, qb, n_, len(items), g, kb, mi))
                    n += 1
            self.run_pipe(stages)
            P.barrier()

    def phase_attnA(self, l):
        P = self.P
        with ExitStack() as es:
            kaT = self.sb(es, "kaT", [64, L], BF16)
            ikT = self.sb(es, "ikT", [64, L], BF16)
            av = self.sb(es, "av", [128, 32, 65], BF16)
            iw = self.sb(es, "iw", [128, 32, 8], F32)
            t_k = Tok(); t_ik = Tok(); t_v = Tok(); t_iw = Tok()
            P.op("sp", lambda e: e.dma_start(out=kaT[:], in_=self.zT_d[R_AKIK:R_AKIK + 64, :]), writes=[t_k], dma=True)
            P.op("sp", lambda e: e.dma_start(out=ikT[:], in_=self.zT_d[R_AKIK + 64:R_AKIK + 128, :]), writes=[t_ik], dma=True)
            P.op("sp", lambda e: e.dma_start(out=av[:], in_=self.av_d.rearrange("(b p) e -> p b e", p=128)), writes=[t_v], dma=True)
            P.op("sp", lambda e: e.dma_start(out=iw[:], in_=self.iw_d.rearrange("(b p) h -> p b h", p=128)), writes=[t_iw], dma=True)
            aq = [self.sb(es, "aq%d" % i, [64, 6, 128], BF16) for i in range(3)]
            iq = [self.sb(es, "iq%d" % i, [64, 8, 128], BF16) for i in range(3)]
            t_aq = toks(3); t_iq = toks(3)
            Dg = [self.sb(es, "Dg%d" % i, [128, 8, 128], BF16) for i in range(2)]
            t_Dg = toks(2)
            R = [self.sb(es, "R%d" % i, [128, 512], BF16) for i in range(16)]
            t_R = toks(16)
            score = [self.sb(es, "score%d" % i, [128, L], F32) for i in range(2)]
            t_sc = toks(2)
            junk = self.sb(es, "junk", [128, L], BF16)
            t_junk = Tok()
            negm = [self.sb(es, "negm%d" % i, [128, L], BF16) for i in range(2)]
            t_ng = toks(2)
            st = [self.sb(es, "st%d" % i, [128, 8], F32) for i in range(2)]
            thr = [self.sb(es, "thr%d" % i, [128, 2], F32) for i in range(2)]
            rtab = [self.sb(es, "rtab%d" % i, [128, N_BISECT], F32) for i in range(2)]
            t_st = toks(2)
            p2 = self.sb(es, "p2", [128, N_BISECT], F32)
            for i in range(N_BISECT):
                P.op("dve", lambda e, i=i: e.memset(p2[:, i:i + 1], 2.0 ** -(i + 1)), writes=[self.t_const])
            pT = [self.sb(es, "pT%d" % i, [128, 384], BF16) for i in range(4)]
            t_pT = toks(4)
            rc = [self.sb(es, "rc%d" % i, [64, 512], F32) for i in range(2)]
            t_rc = toks(2)
            osb = [self.sb(es, "osb%d" % i, [64, 512], F32) for i in range(2)]
            t_osb = toks(2)
            oT = [self.sb(es, "oT%d" % i, [64, 6, 128], BF16) for i in range(2)]
            t_oT = toks(2)
            ctr = {"ri": 0}
            SB = (3, 4, 7)

            def emit_idx(qb):
                qs = slice(qb * 128, (qb + 1) * 128)
                qi = qb % 3
                b2 = qb % 2
                nk = (qb + 1) * 128
                P.op("sp", lambda e: e.dma_start(out=aq[qi][:], in_=self.zT_d[R_AQ:R_AQ + 384, qs].rearrange("(h d) t -> d h t", d=64)),
                     writes=[t_aq[qi]], dma=True)
                ng, t_n = negm[b2], t_ng[b2]
                if qb < 2:
                    if qb == 1:
                        P.op("dve", lambda e: e.memset(ng[:, 0:128], 0.0), writes=[t_n])
                    P.op("dve", lambda e: e.tensor_copy(out=ng[:, qb * 128:(qb + 1) * 128], in_=self.cb[:, 1, :]),
                         reads=[self.t_const], writes=[t_n])
                    return
                P.op("sp", lambda e: e.dma_start(out=iq[qi][:], in_=self.zT_d[R_IQ:R_IQ + 512, qs].rearrange("(h d) t -> d h t", d=64)),
                     writes=[t_iq[qi]], dma=True)
                sc, t_s = score[b2], t_sc[b2]
                for h in range(8):
                    P.op("pool", lambda e, h=h: e.tensor_scalar(out=Dg[b2][:, h, :], in0=self.cb[:, 0, :], scalar1=iw[:, qb, h:h + 1],
                                                                scalar2=None, op0=ALU.mult),
                         reads=[self.t_const, t_iw], writes=[t_Dg[b2]])
                for c0 in range(0, nk, 512):
                    w = min(512, nk - c0)
                    lastc = (c0 + w == nk)
                    rbase = (ctr["ri"] % 2) * 8
                    ctr["ri"] += 1
                    for h in range(8):
                        bR = h % 2
                        rb = rbase + h
                        P.op("pe", lambda e, h=h, bR=bR, c0=c0, w=w: e.matmul(self.ps[bR][:, 0:w], lhsT=iq[qi][:, h, :], rhs=ikT[:, c0:c0 + w],
                                                                  start=True, stop=True),
                             reads=[t_iq[qi], t_ik], writes=[self.pst[bR]])
                        P.op("act", lambda e, bR=bR, rb=rb, w=w: e.activation(out=R[rb][:, 0:w], in_=self.ps[bR][:, 0:w], func=AF.Relu),
                             reads=[self.pst[bR]], writes=[t_R[rb]])
                    for h in range(8):
                        rb = rbase + h
                        P.op("pe", lambda e, h=h, rb=rb, w=w, lastc=lastc: e.matmul(self.ps[2][:, 0:w], lhsT=Dg[b2][:, h, :], rhs=R[rb][:, 0:w],
                                                                  start=(h == 0), stop=(h == 7 and not lastc)),
                             reads=[t_Dg[b2], t_R[rb]], writes=[self.pst[2]])
                    if lastc:
                        P.op("pe", lambda e, w=w: e.matmul(self.ps[2][:, w - 128:w], lhsT=self.cb[:, 0, :], rhs=self.cb[:, 1, :],
                                                           start=False, stop=True), reads=[self.t_const], writes=[self.pst[2]])
                    P.op("act", lambda e, c0=c0, w=w: e.activation(out=sc[:, c0:c0 + w], in_=self.ps[2][:, 0:w], func=AF.Copy),
                         reads=[self.pst[2]], writes=[t_s])
                S_, T_, RT = st[b2], thr[b2], rtab[b2]
                t_t = t_st[b2]
                P.op("dve", lambda e: e.tensor_reduce(out=S_[:, 0:1], in_=sc[:, 0:nk], axis=mybir.AxisListType.X, op=ALU.max),
                     reads=[t_s], writes=[t_t])
                P.op("dve", lambda e: e.tensor_reduce(out=S_[:, 1:2], in_=sc[:, 0:nk - 128], axis=mybir.AxisListType.X, op=ALU.min),
                     reads=[t_s], writes=[t_t])
                P.op("dve", lambda e: e.tensor_tensor(out=S_[:, 2:3], in0=S_[:, 0:1], in1=S_[:, 1:2], op=ALU.subtract),
                     reads=[t_t], writes=[t_t])
                P.op("dve", lambda e: e.tensor_scalar(out=RT[:], in0=p2[:], scalar1=S_[:, 2:3], scalar2=None, op0=ALU.mult),
                     reads=[t_t, self.t_const], writes=[t_t])
                P.op("dve", lambda e: e.scalar_tensor_tensor(out=T_[:, 0:1], in0=S_[:, 2:3], scalar=0.5, in1=S_[:, 1:2],
                                                             op0=ALU.mult, op1=ALU.add), reads=[t_t], writes=[t_t])
                cur = 0
                for i in range(N_BISECT):
                    P.op("dve", lambda e, cur=cur: e.tensor_scalar(
                        out=junk[:, 0:nk], in0=sc[:, 0:nk], scalar1=T_[:, cur:cur + 1], scalar2=None, op0=ALU.is_ge, op1=ALU.add,
                        accum_out=S_[:, 3:4]), reads=[t_s, t_t], writes=[t_t, t_junk])
                    P.op("dve", lambda e: e.tensor_scalar(out=S_[:, 4:5], in0=S_[:, 3:4], scalar1=255.5, scalar2=-0.5,
                                                          op0=ALU.is_ge, op1=ALU.add), reads=[t_t], writes=[t_t])
                    P.op("dve", lambda e, i=i, cur=cur: e.scalar_tensor_tensor(
                        out=T_[:, 1 - cur:2 - cur], in0=S_[:, 4:5], scalar=RT[:, i:i + 1], in1=T_[:, cur:cur + 1],
                        op0=ALU.mult, op1=ALU.add), reads=[t_t], writes=[t_t])
                    cur = 1 - cur
                P.op("dve", lambda e, cur=cur: e.tensor_scalar(out=ng[:, 0:nk], in0=sc[:, 0:nk], scalar1=T_[:, cur:cur + 1], scalar2=NEG,
                                                               op0=ALU.is_lt, op1=ALU.mult), reads=[t_s, t_t], writes=[t_n])

            def make(n, m, qb, half, kb):
                qi, ob, b2 = qb % 3, qb % 2, qb % 2
                ng, t_n = negm[b2], t_ng[b2]
                bS, pb = SB[n % 3], n % 4
                bO, bD, r2 = 5, 6, m % 2
                psS, tS = self.ps[bS], self.pst[bS]
                qs = slice(qb * 128, (qb + 1) * 128)

                def s_fn():
                    if half == 0 and kb == 0 and qb + 1 < 32:
                        emit_idx(qb + 1)
                    P.op("pe", lambda e: e.matmul(psS[:, 0:384], lhsT=ng[:, kb * 128:(kb + 1) * 128],
                                                  rhs=self.irep[:].rearrange("p h t -> p (h t)"), start=True, stop=False),
                         reads=[t_n, self.t_const], writes=[tS])
                    P.op("pe", lambda e: e.matmul(psS[:, 0:384], lhsT=kaT[:, kb * 128:(kb + 1) * 128],
                                                  rhs=aq[qi][:, 3 * half:3 * half + 3, :], start=False, stop=True),
                         reads=[t_k, t_aq[qi]], writes=[tS])
                    P.op("act", lambda e: e.activation(out=pT[pb][:], in_=psS[:, 0:384], func=AF.Exp, scale=0.125),
                         reads=[tS], writes=[t_pT[pb]])

                def pv_fn():
                    P.op("pe", lambda e: e.matmul(self.ps[bO][0:64, 0:384], lhsT=av[:, kb, 0:64], rhs=pT[pb][:], start=(kb == 0),
                                                  stop=(kb == qb)), reads=[t_v, t_pT[pb]], writes=[self.pst[bO]])
                    P.op("pe", lambda e: e.matmul(self.ps[bD][0:64, 0:384], lhsT=self.ones_bf[:, 0:64], rhs=pT[pb][:], start=(kb == 0),
                                                  stop=(kb == qb)), reads=[self.t_const, t_pT[pb]], writes=[self.pst[bD]])
                    if kb == qb:
                        self.normalize_out2(self.ps[bO], self.ps[bD], self.pst[bO], self.pst[bD], 384, rc[r2], t_rc[r2],
                                            osb[r2], t_osb[r2],
                                            oT[ob][:, 3 * half:3 * half + 3, :].rearrange("p h t -> p (h t)"), t_oT[ob])
                        if half == 1:
                            P.op("sp", lambda e: e.dma_start(out=self.oaT_d[:, :, qs], in_=oT[ob][:]), reads=[t_oT[ob]], dma=True)
                return s_fn, pv_fn

            emit_idx(0)
            stages = []
            n = 0
            m = 0
            for qb in range(32):
                for half in range(2):
                    for kb in range(qb + 1):
                        stages.append(make(n, m, qb, half, kb))
                        n += 1
                    m += 1
            self.run_pipe(stages)
            P.barrier()

    def phase_M(self, l):
        P = self.P
        TW = 256
        with ExitStack() as es:
            Wa = self.sb(es, "Wa", [64, 6, D], BF16)
            Wb = self.sb(es, "Wb", [64, 4, D], BF16)
            Wc = self.sb(es, "Wc", [64, 8, D], BF16)
            Wg = self.sb(es, "Wg", [128, 8, 3 * D], BF16)
            Wo = self.sb(es, "Wo", [128, 8, D], BF16)
            t_w = Tok()
            for (dst, src) in ((Wa, self.w_a[l]), (Wb, self.w_b[l]), (Wc, self.w_c[l])):
                P.op("pool", lambda e, dst=dst, src=src: e.dma_start(out=dst[:], in_=src.rearrange("(h d) m -> d h m", d=64)),
                     writes=[t_w], dma=True)
            for i in range(3):
                for hh in range(2):
                    c0 = C_G + i * D + hh * 512
                    self.load_w(Wg, self.w_in[l], c0, 512, i * D + hh * 512, t_w)
            for hh in range(2):
                self.load_w(Wo, self.w_o[l], hh * 512, 512, hh * 512, t_w)
            oa = [self.sb(es, "oa%d" % i, [64, 6, TW], BF16) for i in range(2)]
            ob_ = [self.sb(es, "ob%d" % i, [64, 4, TW], BF16) for i in range(2)]
            oc = [self.sb(es, "oc%d" % i, [64, 8, TW], BF16) for i in range(2)]
            uT = [self.sb(es, "uTm%d" % i, [128, 8, TW], BF16) for i in range(2)]
            hT = [self.sb(es, "hTm%d" % i, [128, 8, TW], F32) for i in range(2)]
            t_in = toks(2)
            t_h = toks(2)
            sig = [self.sb(es, "sig%d" % i, [128, TW], F32) for i in range(2)]
            t_sig = toks(2)
            mm_ = [self.sb(es, "mm%d" % i, [128, TW], F32) for i in range(3)]
            t_mm = toks(3)
            mg = [self.sb(es, "mg%d" % i, [128, 8, TW], BF16) for i in range(2)]
            t_mg = toks(2)
            branches = ((Wa, oa, 6), (Wb, ob_, 4), (Wc, oc, 8))
            k_ = 0
            for tc in range(L // TW):
                b = tc % 2
                tsl = slice(tc * TW, (tc + 1) * TW)
                P.op("sp", lambda e, b=b, tsl=tsl: e.dma_start(out=oa[b][:], in_=self.oaT_d[:, :, tsl]), writes=[t_in[b]], dma=True)
                P.op("sp", lambda e, b=b, tsl=tsl: e.dma_start(out=ob_[b][:], in_=self.obT_d[:, :, tsl]), writes=[t_in[b]], dma=True)
                P.op("sp", lambda e, b=b, tsl=tsl: e.dma_start(out=oc[b][:], in_=self.ocT_d[:, :, tsl]), writes=[t_in[b]], dma=True)
                P.op("sp", lambda e, b=b, tsl=tsl: e.dma_start(out=uT[b][:], in_=self.uT_d[:, :, tsl].rearrange("c p t -> p c t")),
                     writes=[t_in[b]], dma=True)
                P.op("sp", lambda e, b=b, tsl=tsl: e.dma_start(out=hT[b][:], in_=self.hT_d[:, :, tsl].rearrange("c p t -> p c t")),
                     writes=[t_h[b]], dma=True)
                for c in range(8):
                    cs = slice(c * 128, (c + 1) * 128)
                    for i, (Wi, oi, nh) in enumerate(branches):
                        bY = (k_ % 2) * 2
                        bG = (k_ % 2) * 2 + 1
                        sb_ = k_ % 2
                        k_ += 1
                        for h in range(nh):
                            P.op("pe", lambda e, Wi=Wi, oi=oi, h=h, cs=cs, b=b, bY=bY, nh=nh: e.matmul(
                                self.ps[bY][:, 0:TW], lhsT=Wi[:, h, cs], rhs=oi[b][:, h, :], start=(h == 0), stop=(h == nh - 1)),
                                reads=[t_w, t_in[b]], writes=[self.pst[bY]])
                        for k in range(8):
                            P.op("pe", lambda e, k=k, i=i, c=c, b=b, bG=bG: e.matmul(
                                self.ps[bG][:, 0:TW], lhsT=Wg[:, k, i * D + c * 128:i * D + (c + 1) * 128], rhs=uT[b][:, k, :],
                                start=(k == 0), stop=(k == 7)), reads=[t_w, t_in[b]], writes=[self.pst[bG]])
                        P.op("act", lambda e, bG=bG, sb_=sb_: e.activation(out=sig[sb_][:], in_=self.ps[bG][:, 0:TW], func=AF.Sigmoid),
                             reads=[self.pst[bG]], writes=[t_sig[sb_]])
                        P.op("dve", lambda e, bY=bY, sb_=sb_, i=i: e.tensor_tensor(out=mm_[i][:], in0=self.ps[bY][:, 0:TW], in1=sig[sb_][:], op=ALU.mult),
                             reads=[self.pst[bY], t_sig[sb_]], writes=[t_mm[i]])
                    P.op("pool", lambda e: e.tensor_tensor(out=mm_[0][:], in0=mm_[0][:], in1=mm_[1][:], op=ALU.add),
                         reads=[t_mm[0], t_mm[1]], writes=[t_mm[0]])
                    P.op("pool", lambda e, b=b, c=c: e.tensor_tensor(out=mg[b][:, c, :], in0=mm_[0][:], in1=mm_[2][:], op=ALU.add),
                         reads=[t_mm[0], t_mm[2]], writes=[t_mg[b]])
                for c2 in range(8):
                    bD = 4 + c2 % 2
                    for c in range(8):
                        P.op("pe", lambda e, c=c, c2=c2, b=b, bD=bD: e.matmul(
                            self.ps[bD][:, 0:TW], lhsT=Wo[:, c, c2 * 128:(c2 + 1) * 128], rhs=mg[b][:, c, :], start=(c == 0), stop=(c == 7)),
                            reads=[t_w, t_mg[b]], writes=[self.pst[bD]])
                    P.op("dve", lambda e, c2=c2, b=b, bD=bD: e.tensor_tensor(out=hT[b][:, c2, :], in0=self.ps[bD][:, 0:TW], in1=hT[b][:, c2, :], op=ALU.add),
                         reads=[self.pst[bD], t_h[b]], writes=[t_h[b]])
                P.op("sp", lambda e, b=b, tsl=tsl: e.dma_start(out=self.hT_d[:, :, tsl].rearrange("c p t -> p c t"), in_=hT[b][:]),
                     reads=[t_h[b]], dma=True)
            P.barrier()

    def phase_F(self, l):
        P = self.P
        for half in range(2):
            with ExitStack() as es:
                Wu = self.sb(es, "Wu", [128, 8, 2048], BF16)
                Wd = self.sb(es, "Wd", [128, 16, D], BF16)
                t_w = Tok()
                for q4 in range(4):
                    self.load_w(Wu, self.w_up[l], half * 2048 + q4 * 512, 512, q4 * 512, t_w)
                srcd = self.w_down[l][half * 2048:(half + 1) * 2048, :].rearrange("(kc p) m -> p kc m", p=128)
                for q2 in range(2):
                    P.op("pool", lambda e, q2=q2, srcd=srcd, Wd=Wd: e.dma_start(out=Wd[:, q2 * 8:(q2 + 1) * 8, :], in_=srcd[:, q2 * 8:(q2 + 1) * 8, :]),
                         writes=[t_w], dma=True)
                hT = [self.sb(es, "hTf%d" % i, [128, 8, 512], F32) for i in range(2)]
                t_h = toks(2)
                u2 = [self.sb(es, "u2%d" % i, [128, 8, 512], BF16) for i in range(2)]
                t_u = toks(2)
                sq = [self.sb(es, "sqf%d" % i, [128, 8, 512], BF16) for i in range(2)]
                t_sq = toks(2)
                rs = [self.sb(es, "rsf%d" % i, [128, 512], F32) for i in range(2)]
                t_rs = toks(2)
                hid = [self.sb(es, "hid%d" % i, [128, 16, 512], BF16) for i in range(2)]
                t_hid = toks(2)
                rl = [self.sb(es, "rl%d" % i, [128, 512], F32) for i in range(2)]
                t_rl = toks(2)
                k_ = 0
                for tc in range(8):
                    b = tc % 2
                    tsl = slice(tc * 512, (tc + 1) * 512)
                    P.op("sp", lambda e, b=b, tsl=tsl: e.dma_start(out=hT[b][:], in_=self.hT_d[:, :, tsl].rearrange("c p t -> p c t")),
                         writes=[t_h[b]], dma=True)
                    if half == 0:
                        self.norm_chunk(None, hT[b], t_h[b], lambda c: self.g_mlp[:, l, c:c + 1],
                                        lambda c, b=b: u2[b][:, c, :], t_u[b], sq[b], t_sq[b], rs[b], t_rs[b], 6 + b, l)
                        P.op("sp", lambda e, b=b, tsl=tsl: e.dma_start(out=self.uT_d[:, :, tsl].rearrange("c p t -> p c t"), in_=u2[b][:]),
                             reads=[t_u[b]], dma=True)
                    else:
                        P.op("sp", lambda e, b=b, tsl=tsl: e.dma_start(out=u2[b][:], in_=self.uT_d[:, :, tsl].rearrange("c p t -> p c t")),
                             writes=[t_u[b]], dma=True)
                    for f in range(16):
                        bU = k_ % 4
                        rb = k_ % 2
                        k_ += 1
                        for k in range(8):
                            P.op("pe", lambda e, k=k, f=f, b=b, bU=bU: e.matmul(
                                self.ps[bU][:], lhsT=Wu[:, k, f * 128:(f + 1) * 128], rhs=u2[b][:, k, :], start=(k == 0), stop=(k == 7)),
                                reads=[t_w, t_u[b]], writes=[self.pst[bU]])
                        P.op("act", lambda e, bU=bU, rb=rb: e.activation(out=rl[rb][:], in_=self.ps[bU][:], func=AF.Relu),
                             reads=[self.pst[bU]], writes=[t_rl[rb]])
                        eng = "dve" if f % 2 == 0 else "pool"
                        P.op(eng, lambda e, rb=rb, b=b, f=f: e.tensor_tensor(out=hid[b][:, f, :], in0=rl[rb][:], in1=rl[rb][:], op=ALU.mult),
                             reads=[t_rl[rb]], writes=[t_hid[b]])
                    for c2 in range(8):
                        bD = 4 + c2 % 2
                        for f in range(16):
                            P.op("pe", lambda e, f=f, c2=c2, b=b, bD=bD: e.matmul(
                                self.ps[bD][:], lhsT=Wd[:, f, c2 * 128:(c2 + 1) * 128], rhs=hid[b][:, f, :], start=(f == 0), stop=(f == 15)),
                                reads=[t_w, t_hid[b]], writes=[self.pst[bD]])
                        P.op("dve", lambda e, c2=c2, b=b, bD=bD: e.tensor_tensor(out=hT[b][:, c2, :], in0=self.ps[bD][:], in1=hT[b][:, c2, :], op=ALU.add),
                             reads=[self.pst[bD], t_h[b]], writes=[t_h[b]])
                    P.op("sp", lambda e, b=b, tsl=tsl: e.dma_start(out=self.hT_d[:, :, tsl].rearrange("c p t -> p c t"), in_=hT[b][:]),
                         reads=[t_h[b]], dma=True)
                if half == 1 and l == 0:
                    self.dump("d_hid", hid[0][:], [128, 16, 512], BF16, t_hid[0])
                    self.dump("d_wu", Wu[:], [128, 8, 2048], BF16, t_w)
                    self.dump("d_wd", Wd[:], [128, 16, D], BF16, t_w)
                    self.dump("d_u2", u2[0][:], [128, 8, 512], BF16, t_u[0])
                P.barrier()

    def phase_O(self):
        P = self.P
        with ExitStack() as es:
            hT = [self.sb(es, "hTo%d" % i, [128, 8, 512], F32) for i in range(2)]
            t_h = toks(2)
            y = [self.sb(es, "yo%d" % i, [128, 8, 512], F32) for i in range(2)]
            t_y = toks(2)
            sq = [self.sb(es, "sqo%d" % i, [128, 8, 512], BF16) for i in range(2)]
            t_sq = toks(2)
            rs = [self.sb(es, "rso%d" % i, [128, 512], F32) for i in range(2)]
            t_rs = toks(2)
            ot = [self.sb(es, "ot%d" % i, [128, D], F32) for i in range(3)]
            t_ot = toks(3)
            oi = 0
            for tc in range(8):
                b = tc % 2
                tsl = slice(tc * 512, (tc + 1) * 512)
                P.op("sp", lambda e, b=b, tsl=tsl: e.dma_start(out=hT[b][:], in_=self.hT_d[:, :, tsl].rearrange("c p t -> p c t")),
                     writes=[t_h[b]], dma=True)
                self.norm_chunk(None, hT[b], t_h[b], lambda c: self.g_fin[:, c:c + 1],
                                lambda c, b=b: y[b][:, c, :], t_y[b], sq[b], t_sq[b], rs[b], t_rs[b], 6 + b, 0)
                for j in range(4):
                    o3 = oi % 3
                    oi += 1
                    for hh in range(2):
                        pb = (2 * j + hh) % 4
                        for q in range(4):
                            c = hh * 4 + q
                            P.op("pe", lambda e, b=b, c=c, j=j, q=q, pb=pb: e.transpose(
                                out=self.ps[pb][:, q * 128:(q + 1) * 128], in_=y[b][:, c, j * 128:(j + 1) * 128], identity=self.ident_f),
                                reads=[t_y[b], self.t_const], writes=[self.pst[pb]])
                        if hh == 0:
                            P.op("dve", lambda e, o3=o3, pb=pb: e.tensor_copy(out=ot[o3][:, 0:512], in_=self.ps[pb][:]),
                                 reads=[self.pst[pb]], writes=[t_ot[o3]])
                        else:
                            P.op("act", lambda e, o3=o3, pb=pb: e.activation(out=ot[o3][:, 512:1024], in_=self.ps[pb][:], func=AF.Copy),
                                 reads=[self.pst[pb]], writes=[t_ot[o3]])
                    r0 = tc * 512 + j * 128
                    P.op("sp", lambda e, o3=o3, r0=r0: e.dma_start(out=self.out[r0:r0 + 128, :], in_=ot[o3][:]),
                         reads=[t_ot[o3]], dma=True)
            P.barrier()


def make_consts():
    p = np.arange(128)
    half = 32
    inv = (10000.0 ** (-(np.arange(half, dtype=np.float32)) / half)).astype(np.float32)
    cvec = np.zeros((128, 4), np.float32)
    cvec[:, 0] = inv[p % 32]
    cvec[:, 1] = np.where((p % 64) < 32, -1.0, 1.0)
    cmat = np.zeros((128, 5, 128), np.float32)
    cmat[:, 0, :] = np.eye(128, dtype=np.float32)
    r = np.arange(128)[:, None]
    c = np.arange(128)[None, :]
    cmat[:, 1, :] = np.where(c > r, NEG, 0.0)
    cmat[:, 2, :] = np.where(r > c, NEG, 0.0)
    cmat[:, 3, :] = np.where(r < c, NEG, 0.0)
    cmat[64:, 4, :] = 1.0
    cvec[:, 2] = (p >= 64).astype(np.float32)
    cvec[:, 3] = (p < 64).astype(np.float32)
    masks = np.zeros((128, 9, 128), np.float32)
    masks[:, 0, :] = np.where(r > c, NEG, 0.0)
    masks[:, 1, :] = np.where(r < c, NEG, 0.0)
    for base, dl in ((2, 4), (5, 16)):
        res = ((c - r) % dl) == 0
        masks[:, base + 0, :] = np.where(res & (r <= c), 0.0, NEG)
        masks[:, base + 1, :] = np.where(res, 0.0, NEG)
        masks[:, base + 2, :] = np.where(res & (c <= r), 0.0, NEG)
    masks[:, 8, :] = np.where(r <= c, NEG, 0.0)
    return cvec, cmat, masks


def build_inputs(inputs, b):
    cvec, cmat, masks = make_consts()
    m = {
        "x": np.ascontiguousarray(inputs["x"][b]),
        "pos": np.ascontiguousarray(inputs["positions"][b]).astype(np.int32),
        "cvec": cvec, "cmat": cmat, "masks": masks,
    }
    for k in ("attn_norm", "w_in", "idx_k_norm", "sinks", "w_a", "w_b", "w_c", "w_o", "mlp_norm", "w_up",
              "w_down", "final_norm"):
        m[k] = np.ascontiguousarray(np.asarray(inputs[k], dtype=np.float32))
    return m


def kernel(**inputs):
    bld = Builder()
    nc = bld.build()
    n = 8
    in_maps = [build_inputs(inputs, b) for b in range(n)]
    res = run_bass_kernel_spmd(nc, in_maps, core_ids=list(range(n)))
    return np.stack([r["out"] for r in res.results], axis=0)
```

```python
import math
import os
from contextlib import ExitStack

import numpy as np
import concourse.bass as bass
import concourse.mybir as mybir
from concourse.bass_utils import run_bass_kernel_spmd

F32 = mybir.dt.float32
BF16 = mybir.dt.bfloat16
I32 = mybir.dt.int32
AF = mybir.ActivationFunctionType
ALU = mybir.AluOpType

L = 4096
D = 1024
DEPTH = 2
NEG = -30000.0
EPS = 1e-6
N_BISECT = 14

C_AQ, C_AK, C_AV, C_IQ, C_IK, C_IW = 0, 384, 448, 512, 1024, 1088
C_BQ, C_BK, C_BV, C_CQ, C_CK, C_CV, C_G = 1096, 1864, 2632, 3400, 3912, 4040, 4168

R_AQ, R_AKIK, R_IQ, R_BQ, R_BK, R_CQ, R_CK = 0, 384, 512, 1024, 1792, 2560, 3072
N_ROPED = 3200


class Tok:
    __slots__ = ("w", "rs", "rd")

    def __init__(self):
        self.w = None
        self.rs = {}
        self.rd = []


def toks(n):
    return [Tok() for _ in range(n)]


class _Op:
    __slots__ = ("eng", "fn", "waits", "sem", "val", "inc", "is_dma")


class Prog:
    ENGS = ("pe", "act", "dve", "pool", "sp")

    def __init__(self, nc, ndma=36):
        self.nc = nc
        self.ops = {e: [] for e in self.ENGS}
        self.cnt = {e: 0 for e in self.ENGS}
        self.waited = {}
        self.ndma = ndma
        self.dma_uses = [0] * ndma
        self.dma_rr = 0
        self.nops = 0

    def _wait(self, X, sem, val):
        key = (X.eng, sem)
        if self.waited.get(key, 0) >= val:
            return
        self.waited[key] = val
        X.waits.append((sem, val))

    def op(self, eng, fn, reads=(), writes=(), dma=False):
        X = _Op()
        X.eng = eng
        X.fn = fn
        X.waits = []
        X.is_dma = dma
        deps = []
        for t in reads:
            if t.w is not None:
                deps.append((t.w, 0))
        for t in writes:
            if t.w is not None:
                deps.append((t.w, 1))
            for r in t.rs.values():
                deps.append((r, 1))
            for r in t.rd:
                deps.append((r, 1))
        if dma and eng == "pool" and not os.environ.get("NOUSEM"):
            self.n_usem = getattr(self, "n_usem", 0) + 1
            X.sem = ("u", self.n_usem - 1)
            X.val = 16
            X.inc = 16
        elif dma:
            j = self.dma_rr
            self.dma_rr = (j + 1) % self.ndma
            k = self.dma_uses[j]
            self.dma_uses[j] += 1
            X.sem = ("d", j)
            X.val = 16 * (k + 1)
            X.inc = 16
            if k > 0:
                self._wait(X, ("d", j), 16 * k)
        else:
            self.cnt[eng] += 1
            X.sem = ("e", eng)
            X.val = self.cnt[eng]
            X.inc = 1
        for d, hz in deps:
            if d is X:
                continue
            if (not d.is_dma) and (not dma) and d.eng == eng and hz == 1:
                continue
            self._wait(X, d.sem, d.val)
        for t in reads:
            if dma:
                t.rd.append(X)
            else:
                t.rs[eng] = X
        for t in writes:
            t.w = X
            t.rs = {}
            t.rd = []
        self.ops[eng].append(X)
        self.nops += 1
        return X

    def barrier(self):
        snap = dict(self.cnt)
        sd = list(self.dma_uses)
        nu = getattr(self, "n_usem", 0)
        for e in self.ENGS:
            X = _Op()
            X.eng = e
            X.fn = None
            X.waits = []
            X.is_dma = False
            X.sem = None
            X.val = 0
            X.inc = 0
            for e2 in self.ENGS:
                if e2 != e and snap[e2] > 0:
                    self._wait(X, ("e", e2), snap[e2])
            for j in range(self.ndma):
                if sd[j] > 0:
                    self._wait(X, ("d", j), 16 * sd[j])
            for j in range(nu):
                self._wait(X, ("u", j), 16)
            self.ops[e].append(X)

    def emit(self):
        nc = self.nc
        with ExitStack() as es:
            sems = {}
            for e in self.ENGS:
                sems[("e", e)] = es.enter_context(nc.semaphore("s_" + e))
            for j in range(self.ndma):
                sems[("d", j)] = es.enter_context(nc.semaphore("d_%d" % j))
            for j in range(getattr(self, "n_usem", 0)):
                sems[("u", j)] = es.enter_context(nc.semaphore("u_%d" % j))
            block = es.enter_context(nc.Block())

            def run(ename):
                def body(eng):
                    for X in self.ops[ename]:
                        for (s, v) in X.waits:
                            eng.wait_ge(sems[s], v)
                        if X.fn is None:
                            continue
                        ins = X.fn(eng)
                        ins.then_inc(sems[X.sem], X.inc)
                return body

            block.tensor(run("pe"))
            block.scalar(run("act"))
            block.vector(run("dve"))
            block.gpsimd(run("pool"))
            block.sync(run("sp"))


def sap(t, off, dims, npart=128, pstart=0):
    fs = 1
    for s in list(t.shape)[1:]:
        fs *= int(s)
    return bass.AP(t, pstart * fs + off, [[fs, npart]] + [list(d) for d in dims])


class Builder:
    def __init__(self, dbg=None, stop_after=None, skip=()):
        self.skip = skip
        self.dbg = dbg or ()
        self.stop_after = stop_after
        nc = bass.Bass("TRN2", target_bir_lowering=False)
        self.nc = nc
        self.P = Prog(nc)
        self.outs = []

        def din(name, shape, dt=F32):
            return nc.dram_tensor(name, list(shape), dt, kind="ExternalInput").ap()

        self.x = din("x", [L, D])
        self.pos = din("pos", [L], I32)
        self.attn_norm = din("attn_norm", [DEPTH, D])
        self.w_in = din("w_in", [DEPTH, D, 7240])
        self.idx_k_norm = din("idx_k_norm", [DEPTH, 64])
        self.sinks = din("sinks", [DEPTH, 8])
        self.w_a = din("w_a", [DEPTH, 384, D])
        self.w_b = din("w_b", [DEPTH, 256, D])
        self.w_c = din("w_c", [DEPTH, 512, D])
        self.w_o = din("w_o", [DEPTH, D, D])
        self.mlp_norm = din("mlp_norm", [DEPTH, D])
        self.w_up = din("w_up", [DEPTH, D, 4 * D])
        self.w_down = din("w_down", [DEPTH, 4 * D, D])
        self.final_norm = din("final_norm", [D])
        self.cvec = din("cvec", [128, 4])
        self.cmat = din("cmat", [128, 6, 128])
        self.masks = din("masks", [128, 9, 128])

        self.out = nc.dram_tensor("out", [L, D], F32, kind="ExternalOutput").ap()

        self.cos_d = self.scr("cos_d", [128, L], F32)
        self.sin_d = self.scr("sin_d", [128, L], F32)
        self.hT_d = self.scr("hT_d", [8, 128, L], F32)
        self.uT_d = self.scr("uT_d", [8, 128, L], BF16)
        self.zT_d = self.scr("zT_d", [N_ROPED, L], BF16)
        self.bv_d = self.scr("bv_d", [L, 12, 65], BF16)
        self.cv_d = self.scr("cv_d", [L, 2, 65], BF16)
        self.av_d = self.scr("av_d", [L, 65], BF16)
        self.iw_d = self.scr("iw_d", [L, 8], F32)
        self.oaT_d = self.scr("oaT_d", [384, L], BF16)
        self.obT_d = self.scr("obT_d", [256, L], BF16)
        self.ocT_d = self.scr("ocT_d", [512, L], BF16)

    def scr(self, name, shape, dt):
        kind = "ExternalOutput" if name in self.dbg else "Internal"
        t = self.nc.dram_tensor(name, list(shape), dt, kind=kind)
        if name in self.dbg:
            self.outs.append(name)
        return t.ap()

    def sb(self, es, name, shape, dt):
        self._sbn = getattr(self, "_sbn", 0) + 1
        return es.enter_context(self.nc.sbuf_tensor("%s_%d" % (name, self._sbn), list(shape), dt))

    def build(self):
        nc, P = self.nc, self.P
        with ExitStack() as es:
            self.ps = [es.enter_context(nc.psum_tensor("ps%d" % i, [128, 512], F32)) for i in range(8)]
            self.pst = toks(8)
            self.cvec_sb = self.sb(es, "cvec_sb", [128, 4], F32)
            self.cmat_sb = self.sb(es, "cmat_sb", [128, 6, 128], F32)
            self.ident_f = self.cmat_sb[:, 0, :]
            self.cb = self.sb(es, "cb", [128, 6, 128], BF16)
            self.ones_bf = self.sb(es, "ones_bf", [128, 128], BF16)
            self.ones_f = self.sb(es, "ones_f", [128, 128], F32)
            self.g_attn = self.sb(es, "g_attn", [128, DEPTH, 8], F32)
            self.g_mlp = self.sb(es, "g_mlp", [128, DEPTH, 8], F32)
            self.g_fin = self.sb(es, "g_fin", [128, 8], F32)
            self.gk = self.sb(es, "gk", [128, DEPTH, 2], F32)
            self.t_const = Tok()
            self.eps_t = self.sb(es, "eps_t", [128, 1], F32)
            P.op("dve", lambda e: e.memset(self.eps_t[:], EPS), writes=[self.t_const])
            self.phase_const()
            P.barrier()
            self.dump("d_gk", self.gk[:], [128, DEPTH, 2], F32, self.t_const)
            self.dump("d_gattn", self.g_attn[:], [128, DEPTH, 8], F32, self.t_const)
            if self.stop_after == "const":
                return self.finish()
            self.attn_consts(es)
            P.barrier()
            for l in range(DEPTH):
                self.phase_A(l)
                P.barrier()
                if self.stop_after in (("A", l), ("A1", l), ("A2a", l)):
                    return self.finish()
                for nm, fn in (("aC", self.phase_attnC), ("aB", self.phase_attnB), ("aA", self.phase_attnA),
                               ("M", self.phase_M), ("F", self.phase_F)):
                    if nm not in self.skip:
                        fn(l)
                    if self.stop_after == (nm, l):
                        return self.finish()
            self.phase_O()
            return self.finish()

    def dump(self, name, src_ap, shape, dt, tok):
        if name not in self.dbg:
            return
        t = self.nc.dram_tensor(name, list(shape), dt, kind="ExternalOutput").ap()
        self.outs.append(name)
        self.P.op("sp", lambda e: e.dma_start(out=t, in_=src_ap), reads=[tok], dma=True)

    def finish(self):
        self.P.barrier()
        self.P.emit()
        return self.nc

    def phase_const(self):
        nc, P = self.nc, self.P
        tc_ = self.t_const
        with ExitStack() as es:
            cosT = self.sb(es, "cosT", [128, L], F32)
            sinS = self.sb(es, "sinS", [128, L], F32)
            posi = self.sb(es, "posi", [128, L], I32)
            ang = self.sb(es, "ang", [128, L], F32)
            kk = self.sb(es, "kk", [128, L], F32)
            ki = self.sb(es, "ki", [128, L], I32)
            t1 = Tok(); t2 = Tok(); t3 = Tok(); t4 = Tok()
            P.op("sp", lambda e: e.dma_start(out=self.cvec_sb[:], in_=self.cvec), writes=[tc_], dma=True)
            P.op("sp", lambda e: e.dma_start(out=self.cmat_sb[:], in_=self.cmat), writes=[tc_], dma=True)
            P.op("sp", lambda e: e.dma_start(out=posi[:], in_=self.pos.partition_broadcast(128)), writes=[t1], dma=True)
            for (dst, src) in ((self.g_attn, self.attn_norm), (self.g_mlp, self.mlp_norm)):
                P.op("sp", lambda e, dst=dst, src=src: e.dma_start(
                    out=dst[:], in_=src.rearrange("l (c p) -> p l c", p=128),
                    allow_slow_non_contiguous=True), writes=[tc_], dma=True)
            P.op("sp", lambda e: e.dma_start(out=self.g_fin[:], in_=self.final_norm.rearrange("(c p) -> p c", p=128),
                                             allow_slow_non_contiguous=True), writes=[tc_], dma=True)
            P.op("dve", lambda e: e.memset(self.gk[:], 1.0), writes=[tc_])
            for l in range(DEPTH):
                src = self.idx_k_norm[l]
                P.op("sp", lambda e, l=l, src=src: e.dma_start(
                    out=self.gk[64:128, l, 0:1], in_=src.rearrange("(p o) -> p o", o=1),
                    allow_slow_non_contiguous=True), writes=[tc_], dma=True)
                P.op("sp", lambda e, l=l, src=src: e.dma_start(
                    out=self.gk[64:96, l, 1:2], in_=src[32:64].rearrange("(p o) -> p o", o=1),
                    allow_slow_non_contiguous=True), writes=[tc_], dma=True)
                P.op("sp", lambda e, l=l, src=src: e.dma_start(
                    out=self.gk[96:128, l, 1:2], in_=src[0:32].rearrange("(p o) -> p o", o=1),
                    allow_slow_non_contiguous=True), writes=[tc_], dma=True)
            P.op("dve", lambda e: e.memset(self.ones_bf[:], 1.0), writes=[tc_])
            P.op("dve", lambda e: e.memset(self.ones_f[:], 1.0), writes=[tc_])
            P.op("dve", lambda e: e.tensor_copy(out=self.cb[:], in_=self.cmat_sb[:]), reads=[tc_], writes=[tc_])
            P.op("dve", lambda e: e.tensor_copy(out=ang[:], in_=posi[:]), reads=[t1], writes=[t2])
            P.op("dve", lambda e: e.tensor_scalar(out=ang[:], in0=ang[:], scalar1=self.cvec_sb[:, 0:1], scalar2=None,
                                                  op0=ALU.mult), reads=[t2, tc_], writes=[t2])
            P.op("dve", lambda e: e.tensor_scalar(out=kk[:], in0=ang[:], scalar1=1.0 / (2 * math.pi), scalar2=0.5,
                                                  op0=ALU.mult, op1=ALU.add), reads=[t2], writes=[t3])
            P.op("dve", lambda e: e.tensor_copy(out=ki[:], in_=kk[:]), reads=[t3], writes=[t4])
            P.op("dve", lambda e: e.tensor_copy(out=kk[:], in_=ki[:]), reads=[t4], writes=[t3])
            C1 = 6.28125
            C2 = 2 * math.pi - C1
            P.op("dve", lambda e: e.scalar_tensor_tensor(out=ang[:], in0=kk[:], scalar=-C1, in1=ang[:],
                                                         op0=ALU.mult, op1=ALU.add), reads=[t3, t2], writes=[t2])
            P.op("dve", lambda e: e.scalar_tensor_tensor(out=ang[:], in0=kk[:], scalar=-C2, in1=ang[:],
                                                         op0=ALU.mult, op1=ALU.add), reads=[t3, t2], writes=[t2])
            P.op("dve", lambda e: e.tensor_scalar(out=kk[:], in0=ang[:], scalar1=-math.pi, scalar2=2 * math.pi,
                                                  op0=ALU.is_lt, op1=ALU.mult), reads=[t2], writes=[t3])
            P.op("dve", lambda e: e.tensor_tensor(out=ang[:], in0=ang[:], in1=kk[:], op=ALU.add),
                 reads=[t2, t3], writes=[t2])
            P.op("dve", lambda e: e.tensor_scalar(out=kk[:], in0=ang[:], scalar1=math.pi, scalar2=-2 * math.pi,
                                                  op0=ALU.is_gt, op1=ALU.mult), reads=[t2], writes=[t3])
            P.op("dve", lambda e: e.tensor_tensor(out=ang[:], in0=ang[:], in1=kk[:], op=ALU.add),
                 reads=[t2, t3], writes=[t2])
            P.op("dve", lambda e: e.tensor_scalar(out=ang[:], in0=ang[:], scalar1=-3.1415925, scalar2=3.1415925,
                                                  op0=ALU.max, op1=ALU.min), reads=[t2], writes=[t2])
            P.op("act", lambda e: e.activation(out=sinS[:], in_=ang[:], func=AF.Sin), reads=[t2], writes=[tc_])
            P.op("dve", lambda e: e.tensor_scalar(out=sinS[:], in0=sinS[:], scalar1=self.cvec_sb[:, 1:2],
                                                  scalar2=None, op0=ALU.mult), reads=[tc_], writes=[tc_])
            P.op("dve", lambda e: e.tensor_scalar(out=kk[:], in0=ang[:], scalar1=-1.0, scalar2=None,
                                                  op0=ALU.mult), reads=[t2], writes=[t3])
            P.op("dve", lambda e: e.tensor_tensor(out=kk[:], in0=kk[:], in1=ang[:], op=ALU.max),
                 reads=[t2, t3], writes=[t3])
            P.op("dve", lambda e: e.tensor_scalar(out=kk[:], in0=kk[:], scalar1=-1.0, scalar2=math.pi / 2,
                                                  op0=ALU.mult, op1=ALU.add), reads=[t3], writes=[t3])
            P.op("act", lambda e: e.activation(out=cosT[:], in_=kk[:], func=AF.Sin), reads=[t3], writes=[tc_])
            P.op("sp", lambda e: e.dma_start(out=self.cos_d, in_=cosT[:]), reads=[tc_], dma=True)
            P.op("sp", lambda e: e.dma_start(out=self.sin_d, in_=sinS[:]), reads=[tc_], dma=True)
            P.barrier()

    def load_w(self, dst, l_w_ap, col0, ncols, dcol0, tok):
        src = l_w_ap[:, col0:col0 + ncols].rearrange("(kc p) c -> p kc c", p=128)
        self.P.op("pool", lambda e: e.dma_start(out=dst[:, :, dcol0:dcol0 + ncols], in_=src),
                  writes=[tok], dma=True)

    def norm_chunk(self, es_names, hT, t_h, gcol, uT_out_fn, t_u, sq, t_sq, rs, t_rs, psb, l_tag):
        P = self.P
        P.op("act", lambda e: e.activation(out=sq[:], in_=hT[:], func=AF.Square), reads=[t_h], writes=[t_sq])
        for c in range(8):
            P.op("pe", lambda e, c=c: e.matmul(self.ps[psb][:], lhsT=self.ones_bf[:], rhs=sq[:, c, :],
                                               start=(c == 0), stop=(c == 7)),
                 reads=[t_sq, self.t_const], writes=[self.pst[psb]])
        P.op("act", lambda e: e.activation(out=rs[:], in_=self.ps[psb][:], func=AF.Ln, scale=1.0 / D, bias=self.eps_t[:, 0:1]),
             reads=[self.pst[psb], self.t_const], writes=[t_rs])
        P.op("act", lambda e: e.activation(out=rs[:], in_=rs[:], func=AF.Exp, scale=-0.5), reads=[t_rs], writes=[t_rs])
        for c in range(8):
            P.op("dve", lambda e, c=c: e.scalar_tensor_tensor(out=uT_out_fn(c), in0=hT[:, c, :], scalar=gcol(c),
                                                              in1=rs[:], op0=ALU.mult, op1=ALU.mult),
                 reads=[t_h, t_rs, self.t_const], writes=[t_u])

    def phase_A(self, l):
        nc, P = self.nc, self.P
        w_in = self.w_in[l]
        with ExitStack() as es:
            uT = self.sb(es, "uT", [128, 8, L], BF16)
            t_uT = toks(8)
            with ExitStack() as es1:
                hT = [self.sb(es1, "hT%d" % i, [128, 8, 512], F32) for i in range(2)]
                t_h = toks(2)
                sq = [self.sb(es1, "sq%d" % i, [128, 8, 512], BF16) for i in range(2)]
                t_sq = toks(2)
                rs = [self.sb(es1, "rs%d" % i, [128, 512], F32) for i in range(2)]
                t_rs = toks(2)
                if l == 0:
                    xt = [self.sb(es1, "xt%d" % i, [128, D], F32) for i in range(3)]
                    t_x = toks(3)
                xi = 0
                for tc in range(8):
                    b = tc % 2
                    if l == 0:
                        for j in range(4):
                            ti = tc * 4 + j
                            xb = xi % 3
                            xi += 1
                            P.op("sp", lambda e, xb=xb, ti=ti: e.dma_start(out=xt[xb][:], in_=self.x[ti * 128:(ti + 1) * 128, :]),
                                 writes=[t_x[xb]], dma=True)
                            for half in range(2):
                                pb = (2 * j + half) % 4
                                for q in range(4):
                                    c = half * 4 + q
                                    P.op("pe", lambda e, xb=xb, c=c, pb=pb, q=q: e.transpose(
                                        out=self.ps[pb][:, q * 128:(q + 1) * 128], in_=xt[xb][:, c * 128:(c + 1) * 128],
                                        identity=self.ident_f), reads=[t_x[xb], self.t_const], writes=[self.pst[pb]])
                                eng = "dve" if half == 0 else "act"
                                if eng == "dve":
                                    P.op("dve", lambda e, b=b, half=half, j=j, pb=pb: e.tensor_copy(
                                        out=hT[b][:, half * 4:half * 4 + 4, j * 128:(j + 1) * 128],
                                        in_=self.ps[pb][:].rearrange("p (q t) -> p q t", q=4)),
                                        reads=[self.pst[pb]], writes=[t_h[b]])
                                else:
                                    P.op("act", lambda e, b=b, half=half, j=j, pb=pb: e.activation(
                                        out=hT[b][:, half * 4:half * 4 + 4, j * 128:(j + 1) * 128],
                                        in_=self.ps[pb][:].rearrange("p (q t) -> p q t", q=4), func=AF.Copy),
                                        reads=[self.pst[pb]], writes=[t_h[b]])
                        P.op("sp", lambda e, b=b, tc=tc: e.dma_start(
                            out=self.hT_d[:, :, tc * 512:(tc + 1) * 512].rearrange("c p t -> p c t"), in_=hT[b][:]),
                            reads=[t_h[b]], dma=True)
                    else:
                        P.op("sp", lambda e, b=b, tc=tc: e.dma_start(
                            out=hT[b][:], in_=self.hT_d[:, :, tc * 512:(tc + 1) * 512].rearrange("c p t -> p c t")),
                            writes=[t_h[b]], dma=True)
                    self.norm_chunk(None, hT[b], t_h[b], lambda c: self.g_attn[:, l, c:c + 1],
                                    lambda c, tc=tc: uT[:, c, tc * 512:(tc + 1) * 512], t_uT[tc],
                                    sq[b], t_sq[b], rs[b], t_rs[b], 4 + b, l)
                    P.op("sp", lambda e, tc=tc: e.dma_start(
                        out=self.uT_d[:, :, tc * 512:(tc + 1) * 512].rearrange("c p t -> p c t"),
                        in_=uT[:, :, tc * 512:(tc + 1) * 512]), reads=[t_uT[tc]], dma=True)
                P.barrier()
            if self.stop_after == ("A1", l):
                return
            with ExitStack() as es2:
                cosT = self.sb(es2, "cosT", [128, L], F32)
                sinS = self.sb(es2, "sinS", [128, L], F32)
                P.op("sp", lambda e: e.dma_start(out=cosT[:], in_=self.cos_d), writes=[self.t_const], dma=True)
                P.op("sp", lambda e: e.dma_start(out=sinS[:], in_=self.sin_d), writes=[self.t_const], dma=True)
                W = [self.sb(es2, "W%d" % i, [128, 8, 512], BF16) for i in range(2)]
                Ws = [self.sb(es2, "Ws%d" % i, [128, 8, 512], BF16) for i in range(2)]
                t_W = toks(2)
                t_Ws = toks(2)
                r1 = [self.sb(es2, "r1_%d" % i, [128, 512], F32) for i in range(2)]
                r2 = [self.sb(es2, "r2_%d" % i, [128, 512], F32) for i in range(2)]
                t_r1 = toks(2)
                t_r2 = toks(2)
                ro = [self.sb(es2, "ro%d" % i, [128, 512], BF16) for i in range(3)]
                t_ro = toks(3)
                sqk = self.sb(es2, "sqk", [128, 512], BF16)
                t_sqk = Tok()
                fk = self.sb(es2, "fk", [128, 512], F32)
                t_fk = Tok()
                groups = [
                    (R_AQ, [(C_AQ, 384), (C_AK, 64), (C_IK, 64)]),
                    (R_IQ, [(C_IQ, 512)]),
                    (R_BQ, [(C_BQ, 512)]),
                    (R_BQ + 512, [(C_BQ + 512, 256), (C_BK, 256)]),
                    (R_BK + 256, [(C_BK + 256, 512)]),
                    (R_CQ, [(C_CQ, 512)]),
                    (R_CK, [(C_CK, 128)]),
                ]
                rr = 0
                ri = 0
                for gi, (row0, pieces) in enumerate(groups):
                    wb = gi % 2
                    dc = 0
                    for (c0, ncol) in pieces:
                        self.load_w(W[wb], w_in, c0, ncol, dc, t_W[wb])
                        dc += ncol
                    ncols = dc
                    nh = ncols // 64
                    wv = W[wb][:, :, 0:ncols].rearrange("p k (h two d) -> p k h two d", two=2, d=32)
                    wsv = Ws[wb][:, :, 0:ncols].rearrange("p k (h two d) -> p k h two d", two=2, d=32)
                    for k in range(8):
                        P.op("act", lambda e, k=k, wv=wv, wsv=wsv: e.activation(out=wsv[:, k, :, 0, :], in_=wv[:, k, :, 1, :], func=AF.Copy),
                             reads=[t_W[wb]], writes=[t_Ws[wb]])
                        P.op("pool", lambda e, k=k, wv=wv, wsv=wsv: e.tensor_copy(out=wsv[:, k, :, 1, :], in_=wv[:, k, :, 0, :]),
                             reads=[t_W[wb]], writes=[t_Ws[wb]])
                    for tc in range(8):
                        tsl = slice(tc * 512, (tc + 1) * 512)
                        for j in range(ncols // 128):
                            row = row0 + j * 128
                            is_kik = (row == R_AKIK)
                            pa, pb_ = 0 + (rr % 2) * 2, 1 + (rr % 2) * 2
                            rb = rr % 2
                            rr += 1
                            for k in range(8):
                                P.op("pe", lambda e, k=k, j=j, pa=pa, wb=wb, tsl=tsl: e.matmul(
                                    self.ps[pa][:], lhsT=W[wb][:, k, j * 128:(j + 1) * 128], rhs=uT[:, k, tsl],
                                    start=(k == 0), stop=(k == 7)), reads=[t_W[wb], t_uT[tc]], writes=[self.pst[pa]])
                            for k in range(8):
                                P.op("pe", lambda e, k=k, j=j, pb_=pb_, wb=wb, tsl=tsl: e.matmul(
                                    self.ps[pb_][:], lhsT=Ws[wb][:, k, j * 128:(j + 1) * 128], rhs=uT[:, k, tsl],
                                    start=(k == 0), stop=(k == 7)), reads=[t_Ws[wb], t_uT[tc]], writes=[self.pst[pb_]])
                            ob = ri % 3
                            ri += 1
                            if not is_kik:
                                P.op("dve", lambda e, pa=pa, rb=rb, tsl=tsl: e.tensor_tensor(
                                    out=r1[rb][:], in0=self.ps[pa][:], in1=cosT[:, tsl], op=ALU.mult),
                                    reads=[self.pst[pa], self.t_const], writes=[t_r1[rb]])
                                P.op("dve", lambda e, pb_=pb_, rb=rb, tsl=tsl: e.tensor_tensor(
                                    out=r2[rb][:], in0=self.ps[pb_][:], in1=sinS[:, tsl], op=ALU.mult),
                                    reads=[self.pst[pb_], self.t_const], writes=[t_r2[rb]])
                                P.op("pool", lambda e, rb=rb, ob=ob: e.tensor_tensor(
                                    out=ro[ob][:], in0=r1[rb][:], in1=r2[rb][:], op=ALU.add),
                                    reads=[t_r1[rb], t_r2[rb]], writes=[t_ro[ob]])
                            else:
                                P.op("act", lambda e, pa=pa: e.activation(out=sqk[:], in_=self.ps[pa][:], func=AF.Square),
                                     reads=[self.pst[pa]], writes=[t_sqk])
                                P.op("pe", lambda e: e.matmul(self.ps[6][:], lhsT=self.cb[:, 4, :], rhs=sqk[:],
                                                              start=True, stop=True),
                                     reads=[t_sqk, self.t_const], writes=[self.pst[6]])
                                P.op("act", lambda e: e.activation(out=fk[:], in_=self.ps[6][:], func=AF.Ln,
                                                                   scale=1.0 / 64, bias=self.eps_t[:, 0:1]),
                                     reads=[self.pst[6], self.t_const], writes=[t_fk])
                                P.op("act", lambda e: e.activation(out=fk[:], in_=fk[:], func=AF.Exp, scale=-0.5),
                                     reads=[t_fk], writes=[t_fk])
                                P.op("dve", lambda e: e.tensor_scalar(out=fk[:], in0=fk[:], scalar1=self.cvec_sb[:, 2:3],
                                                                      scalar2=self.cvec_sb[:, 3:4], op0=ALU.mult, op1=ALU.add),
                                     reads=[t_fk, self.t_const], writes=[t_fk])
                                P.op("dve", lambda e, pa=pa, rb=rb, tsl=tsl: e.scalar_tensor_tensor(
                                    out=r1[rb][:], in0=self.ps[pa][:], scalar=self.gk[:, l, 0:1], in1=cosT[:, tsl],
                                    op0=ALU.mult, op1=ALU.mult),
                                    reads=[self.pst[pa], self.t_const], writes=[t_r1[rb]])
                                P.op("dve", lambda e, pb_=pb_, rb=rb, tsl=tsl: e.scalar_tensor_tensor(
                                    out=r2[rb][:], in0=self.ps[pb_][:], scalar=self.gk[:, l, 1:2], in1=sinS[:, tsl],
                                    op0=ALU.mult, op1=ALU.mult),
                                    reads=[self.pst[pb_], self.t_const], writes=[t_r2[rb]])
                                P.op("pool", lambda e, rb=rb: e.tensor_tensor(
                                    out=r1[rb][:], in0=r1[rb][:], in1=r2[rb][:], op=ALU.add),
                                    reads=[t_r1[rb], t_r2[rb]], writes=[t_r1[rb]])
                                P.op("pool", lambda e, rb=rb, ob=ob: e.tensor_tensor(
                                    out=ro[ob][:], in0=r1[rb][:], in1=fk[:], op=ALU.mult),
                                    reads=[t_r1[rb], t_fk], writes=[t_ro[ob]])
                            P.op("sp", lambda e, ob=ob, row=row, tsl=tsl: e.dma_start(
                                out=self.zT_d[row:row + 128, tsl], in_=ro[ob][:]), reads=[t_ro[ob]], dma=True)
                P.barrier()
            if self.stop_after == ("A2a", l):
                return
            with ExitStack() as es3:
                WV = [self.sb(es3, "WV%d" % i, [128, 8, 512], BF16) for i in range(2)]
                t_WV = toks(2)
                self.load_w(WV[0], w_in, C_BV, 512, 0, t_WV[0])
                self.load_w(WV[1], w_in, C_BV + 512, 256, 0, t_WV[1])
                self.load_w(WV[1], w_in, C_CV, 128, 256, t_WV[1])
                self.load_w(WV[1], w_in, C_AV, 64, 384, t_WV[1])
                self.load_w(WV[1], w_in, C_IW, 8, 448, t_WV[1])
                bvs = [self.sb(es3, "bvs%d" % i, [128, 12, 65], BF16) for i in range(2)]
                cvs = [self.sb(es3, "cvs%d" % i, [128, 2, 65], BF16) for i in range(2)]
                avs = [self.sb(es3, "avs%d" % i, [128, 65], BF16) for i in range(2)]
                iws = [self.sb(es3, "iws%d" % i, [128, 8], F32) for i in range(2)]
                t_st = toks(2)
                for i in range(2):
                    P.op("dve", lambda e, i=i: e.memset(bvs[i][:], 1.0), writes=[t_st[i]])
                    P.op("dve", lambda e, i=i: e.memset(cvs[i][:], 1.0), writes=[t_st[i]])
                    P.op("dve", lambda e, i=i: e.memset(avs[i][:], 1.0), writes=[t_st[i]])
                for ti in range(32):
                    b = ti % 2
                    tcs = ti // 4
                    tk = slice(ti * 128, (ti + 1) * 128)
                    for k in range(8):
                        P.op("pe", lambda e, k=k, tk=tk, b=b: e.matmul(self.ps[b * 2][:], lhsT=uT[:, k, tk], rhs=WV[0][:, k, :],
                                                                     start=(k == 0), stop=(k == 7)),
                             reads=[t_uT[tcs], t_WV[0]], writes=[self.pst[b * 2]])
                    for k in range(8):
                        P.op("pe", lambda e, k=k, tk=tk, b=b: e.matmul(self.ps[b * 2 + 1][:, 0:456], lhsT=uT[:, k, tk], rhs=WV[1][:, k, 0:456],
                                                                     start=(k == 0), stop=(k == 7)),
                             reads=[t_uT[tcs], t_WV[1]], writes=[self.pst[b * 2 + 1]])
                    p0, p1 = self.ps[b * 2], self.ps[b * 2 + 1]
                    P.op("dve", lambda e, b=b, p0=p0: e.tensor_copy(out=bvs[b][:, 0:8, 0:64], in_=p0[:].rearrange("p (h d) -> p h d", d=64)),
                         reads=[self.pst[b * 2]], writes=[t_st[b]])
                    P.op("act", lambda e, b=b, p1=p1: e.activation(out=bvs[b][:, 8:12, 0:64], in_=p1[:, 0:256].rearrange("p (h d) -> p h d", d=64), func=AF.Copy),
                         reads=[self.pst[b * 2 + 1]], writes=[t_st[b]])
                    P.op("dve", lambda e, b=b, p1=p1: e.tensor_copy(out=cvs[b][:, :, 0:64], in_=p1[:, 256:384].rearrange("p (h d) -> p h d", d=64)),
                         reads=[self.pst[b * 2 + 1]], writes=[t_st[b]])
                    P.op("act", lambda e, b=b, p1=p1: e.activation(out=avs[b][:, 0:64], in_=p1[:, 384:448], func=AF.Copy),
                         reads=[self.pst[b * 2 + 1]], writes=[t_st[b]])
                    P.op("dve", lambda e, b=b, p1=p1: e.tensor_copy(out=iws[b][:], in_=p1[:, 448:456]),
                         reads=[self.pst[b * 2 + 1]], writes=[t_st[b]])
                    P.op("sp", lambda e, b=b, tk=tk: e.dma_start(out=self.bv_d[tk], in_=bvs[b][:]), reads=[t_st[b]], dma=True)
                    P.op("sp", lambda e, b=b, tk=tk: e.dma_start(out=self.cv_d[tk], in_=cvs[b][:]), reads=[t_st[b]], dma=True)
                    P.op("sp", lambda e, b=b, tk=tk: e.dma_start(out=self.av_d[tk], in_=avs[b][:]), reads=[t_st[b]], dma=True)
                    P.op("sp", lambda e, b=b, tk=tk: e.dma_start(out=self.iw_d[tk], in_=iws[b][:]), reads=[t_st[b]], dma=True)
                P.barrier()


    def attn_consts(self, es):
        P = self.P
        self.mrep = self.sb(es, "mrep", [128, 9, 4, 128], BF16)
        self.irep = self.sb(es, "irep", [128, 3, 128], BF16)
        self.zeros_bf = self.sb(es, "zeros_bf", [128, 64], BF16)
        mf = self.sb(es, "masks_f", [128, 9, 128], F32)
        t = Tok()
        P.op("sp", lambda e: e.dma_start(out=mf[:], in_=self.masks), writes=[t], dma=True)
        P.op("dve", lambda e: e.tensor_copy(out=self.mrep[:], in_=sap(mf, 0, [[128, 9], [0, 4], [1, 128]])),
             reads=[t], writes=[self.t_const])
        P.op("dve", lambda e: e.tensor_copy(out=self.irep[:], in_=sap(self.cmat_sb, 0, [[0, 3], [1, 128]])),
             reads=[self.t_const], writes=[self.t_const])
        P.op("dve", lambda e: e.memset(self.zeros_bf[:], 0.0), writes=[self.t_const])

    def normalize_out(self, psO, psD, tO, tD, n, rc, t_rc, out_ap, t_out):
        P = self.P
        P.op("act", lambda e: e.activation(out=rc[0:64, 0:n], in_=psD[0:64, 0:n], func=AF.Ln), reads=[tD], writes=[t_rc])
        P.op("act", lambda e: e.activation(out=rc[0:64, 0:n], in_=rc[0:64, 0:n], func=AF.Exp, scale=-1.0),
             reads=[t_rc], writes=[t_rc])
        P.op("dve", lambda e: e.tensor_tensor(out=out_ap, in0=psO[0:64, 0:n], in1=rc[0:64, 0:n], op=ALU.mult),
             reads=[tO, t_rc], writes=[t_out])


    def run_pipe(self, stages):
        pending = None
        for (s_fn, pv_fn) in stages:
            s_fn()
            if pending is not None:
                pending()
            pending = pv_fn
        if pending is not None:
            pending()

    def normalize_out2(self, psO, psD, tO, tD, n, rc, t_rc, osb, t_osb, out_ap, t_out):
        P = self.P
        P.op("act", lambda e: e.activation(out=rc[0:64, 0:n], in_=psD[0:64, 0:n], func=AF.Ln), reads=[tD], writes=[t_rc])
        P.op("act", lambda e: e.activation(out=rc[0:64, 0:n], in_=rc[0:64, 0:n], func=AF.Exp, scale=-1.0),
             reads=[t_rc], writes=[t_rc])
        P.op("act", lambda e: e.activation(out=osb[0:64, 0:n], in_=psO[0:64, 0:n], func=AF.Copy), reads=[tO], writes=[t_osb])
        P.op("pool", lambda e: e.tensor_tensor(out=out_ap, in0=osb[0:64, 0:n], in1=rc[0:64, 0:n], op=ALU.mult),
             reads=[t_osb, t_rc], writes=[t_out])

    def phase_attnC(self, l):
        P = self.P
        with ExitStack() as es:
            ckT = self.sb(es, "ckT", [64, 2, L], BF16)
            cv = self.sb(es, "cv", [128, 32, 2, 65], BF16)
            t_k = Tok(); t_v = Tok()
            P.op("sp", lambda e: e.dma_start(out=ckT[:], in_=self.zT_d[R_CK:R_CK + 128, :].rearrange("(g d) t -> d g t", d=64)),
                 writes=[t_k], dma=True)
            P.op("sp", lambda e: e.dma_start(out=cv[:], in_=self.cv_d.rearrange("(b p) g e -> p b g e", p=128)),
                 writes=[t_v], dma=True)
            sk = self.sb(es, "sk", [1, 8], F32)
            skrow = self.sb(es, "skrow", [1, 8, 128], F32)
            t_sk = Tok()
            P.op("sp", lambda e: e.dma_start(out=sk[:], in_=self.sinks[l:l + 1, :]), writes=[t_sk], dma=True)
            P.op("act", lambda e: e.activation(out=sk[:], in_=sk[:], func=AF.Exp), reads=[t_sk], writes=[t_sk])
            P.op("dve", lambda e: e.tensor_copy(out=skrow[:], in_=sap(sk, 0, [[1, 8], [0, 128]], npart=1)),
                 reads=[t_sk], writes=[t_sk])
            q = [self.sb(es, "cq%d" % i, [64, 8, 128], BF16) for i in range(3)]
            t_q = toks(3)
            pT = [self.sb(es, "pT%d" % i, [128, 512], BF16) for i in range(3)]
            t_pT = toks(3)
            rc = [self.sb(es, "rc%d" % i, [64, 512], F32) for i in range(2)]
            t_rc = toks(2)
            osb = [self.sb(es, "osb%d" % i, [64, 512], F32) for i in range(2)]
            t_osb = toks(2)
            oT = [self.sb(es, "oT%d" % i, [64, 8, 128], BF16) for i in range(2)]
            t_oT = toks(2)
            SB = (0, 1, 6, 7)

            def load_q(qb):
                qi = qb % 3
                qs = slice(qb * 128, (qb + 1) * 128)
                P.op("sp", lambda e: e.dma_start(out=q[qi][:], in_=self.zT_d[R_CQ:R_CQ + 512, qs].rearrange("(h d) t -> d h t", d=64)),
                     writes=[t_q[qi]], dma=True)

            def make(n, m, qb, g, n_, nkb, kb, mi):
                qi, ob = qb % 3, qb % 2
                bS, pb = SB[n % 4], n % 3
                bO, bD, r2 = 2 + m % 2, 4 + m % 2, m % 2
                psS, tS = self.ps[bS], self.pst[bS]
                qs = slice(qb * 128, (qb + 1) * 128)

                def s_fn():
                    if g == 0 and n_ == 0 and qb + 1 < 32:
                        load_q(qb + 1)
                    P.op("pe", lambda e: e.matmul(psS[:], lhsT=self.cb[:, 0, :], rhs=self.mrep[:, mi].rearrange("p h t -> p (h t)"),
                                                  start=True, stop=False), reads=[self.t_const], writes=[tS])
                    P.op("pe", lambda e: e.matmul(psS[:], lhsT=ckT[:, g, kb * 128:(kb + 1) * 128], rhs=q[qi][:, 4 * g:4 * g + 4, :],
                                                  start=False, stop=True), reads=[t_k, t_q[qi]], writes=[tS])
                    P.op("act", lambda e: e.activation(out=pT[pb][:], in_=psS[:], func=AF.Exp, scale=0.125),
                         reads=[tS], writes=[t_pT[pb]])

                def pv_fn():
                    P.op("pe", lambda e: e.matmul(self.ps[bO][0:64, :], lhsT=cv[:, kb, g, 0:64], rhs=pT[pb][:], start=(n_ == 0),
                                                  stop=(n_ == nkb - 1)), reads=[t_v, t_pT[pb]], writes=[self.pst[bO]])
                    P.op("pe", lambda e: e.matmul(self.ps[bD][0:64, :], lhsT=self.ones_bf[:, 0:64], rhs=pT[pb][:], start=(n_ == 0),
                                                  stop=False), reads=[self.t_const, t_pT[pb]], writes=[self.pst[bD]])
                    if n_ == nkb - 1:
                        P.op("pe", lambda e: e.matmul(self.ps[bD][0:64, :], lhsT=self.ones_f[0:1, 0:64],
                                                      rhs=skrow[0:1, 4 * g:4 * g + 4, :], start=False, stop=True),
                             reads=[self.t_const, t_sk], writes=[self.pst[bD]])
                        self.normalize_out2(self.ps[bO], self.ps[bD], self.pst[bO], self.pst[bD], 512, rc[r2], t_rc[r2],
                                            osb[r2], t_osb[r2], oT[ob][:, 4 * g:4 * g + 4, :].rearrange("p h t -> p (h t)"), t_oT[ob])
                        if g == 1:
                            P.op("sp", lambda e: e.dma_start(out=self.ocT_d[:, qs].rearrange("(h d) t -> d h t", d=64), in_=oT[ob][:]), reads=[t_oT[ob]], dma=True)
                return s_fn, pv_fn

            load_q(0)
            stages = []
            n = 0
            m = 0
            for qb in range(32):
                for g in range(2):
                    kbs = ([(qb - 1, 8)] if qb > 0 else []) + [(qb, 0)]
                    for n_, (kb, mi) in enumerate(kbs):
                        stages.append(make(n, m, qb, g, n_, len(kbs), kb, mi))
                        n += 1
                    m += 1
            self.run_pipe(stages)
            P.barrier()

    def phase_attnB(self, l):
        P = self.P
        with ExitStack() as es:
            bkT = self.sb(es, "bkT", [64, 12, L], BF16)
            bv = self.sb(es, "bv", [128, 32, 12, 65], BF16)
            t_k = Tok(); t_v = Tok()
            for g in range(3):
                P.op("sp", lambda e, g=g: e.dma_start(
                    out=bkT[:, 4 * g:4 * g + 4, :],
                    in_=self.zT_d[R_BK + 256 * g:R_BK + 256 * (g + 1), :].rearrange("(h d) t -> d h t", d=64)),
                    writes=[t_k], dma=True)
            for b4 in range(4):
                P.op("sp", lambda e, b4=b4: e.dma_start(
                    out=bv[:, 8 * b4:8 * b4 + 8],
                    in_=self.bv_d[1024 * b4:1024 * (b4 + 1)].rearrange("(b p) h e -> p b h e", p=128)),
                    writes=[t_v], dma=True)
            q = [self.sb(es, "bq%d" % i, [64, 12, 128], BF16) for i in range(3)]
            t_q = toks(3)
            pT = [self.sb(es, "pT%d" % i, [128, 4, 128], BF16) for i in range(3)]
            t_pT = toks(3)
            rc = [self.sb(es, "rc%d" % i, [64, 512], F32) for i in range(2)]
            t_rc = toks(2)
            osb = [self.sb(es, "osb%d" % i, [64, 512], F32) for i in range(2)]
            t_osb = toks(2)
            oT = [self.sb(es, "oT%d" % i, [64, 4, 128], BF16) for i in range(2)]
            t_oT = toks(2)

            def load_q(qb):
                qi = qb % 3
                qs = slice(qb * 128, (qb + 1) * 128)
                P.op("sp", lambda e: e.dma_start(out=q[qi][:], in_=self.zT_d[R_BQ:R_BQ + 768, qs].rearrange("(h d) t -> d h t", d=64)),
                     writes=[t_q[qi]], dma=True)

            def make(n, qb, n_, nit, g, kb, mi):
                qi, ob = qb % 3, qb % 2
                bS, pb = n % 4, n % 3
                bO, bD = 4 + qb % 2, 6 + qb % 2
                psS, tS = self.ps[bS], self.pst[bS]
                qs = slice(qb * 128, (qb + 1) * 128)
                last = (n_ == nit - 1)

                def s_fn():
                    if n_ == 0 and qb + 1 < 32:
                        load_q(qb + 1)
                    P.op("pe", lambda e: e.matmul(psS[:], lhsT=self.cb[:, 0, :], rhs=self.mrep[:, mi].rearrange("p h t -> p (h t)"),
                                                  start=True, stop=False), reads=[self.t_const], writes=[tS])
                    for j in range(4):
                        P.op("pe", lambda e, j=j: e.matmul(psS[:, j * 128:(j + 1) * 128], lhsT=bkT[:, 4 * g + j, kb * 128:(kb + 1) * 128],
                                                           rhs=q[qi][:, 4 * g + j, :], start=False, stop=(j == 3)),
                             reads=[t_k, t_q[qi]], writes=[tS])
                    P.op("act", lambda e: e.activation(out=pT[pb][:].rearrange("p h t -> p (h t)"), in_=psS[:], func=AF.Exp, scale=0.125),
                         reads=[tS], writes=[t_pT[pb]])

                def pv_fn():
                    if n_ == 0:
                        for bb in (bO, bD):
                            P.op("pe", lambda e, bb=bb: e.matmul(self.ps[bb][0:64, :], lhsT=self.zeros_bf[:, 0:64],
                                                               rhs=self.mrep[:, 0].rearrange("p h t -> p (h t)"), start=True, stop=False),
                                 reads=[self.t_const], writes=[self.pst[bb]])
                    for j in range(4):
                        P.op("pe", lambda e, j=j: e.matmul(self.ps[bO][0:64, j * 128:(j + 1) * 128], lhsT=bv[:, kb, 4 * g + j, 0:64],
                                                           rhs=pT[pb][:, j, :], start=False, stop=(last and j == 3)),
                             reads=[t_v, t_pT[pb]], writes=[self.pst[bO]])
                    P.op("pe", lambda e: e.matmul(self.ps[bD][0:64, :], lhsT=self.ones_bf[:, 0:64], rhs=pT[pb][:].rearrange("p h t -> p (h t)"),
                                                  start=False, stop=last), reads=[self.t_const, t_pT[pb]], writes=[self.pst[bD]])
                    if last:
                        self.normalize_out2(self.ps[bO], self.ps[bD], self.pst[bO], self.pst[bD], 512, rc[ob], t_rc[ob],
                                            osb[ob], t_osb[ob], oT[ob][:].rearrange("p h t -> p (h t)"), t_oT[ob])
                        P.op("sp", lambda e: e.dma_start(out=self.obT_d[:, qs].rearrange("(h d) t -> d h t", d=64), in_=oT[ob][:]), reads=[t_oT[ob]], dma=True)
                return s_fn, pv_fn

            load_q(0)
            stages = []
            n = 0
            for qb in range(32):
                items = []
                for g, Dl in enumerate((1, 4, 16)):
                    for o in range(Dl + 1):
                        kb = qb - o
                        if kb < 0:
                            break
                        if Dl == 1:
                            mi = 0 if o == 0 else 1
                        else:
                            base = 2 if Dl == 4 else 5
                            mi = base + (0 if o == 0 else (2 if o == Dl else 1))
                        items.append((g, kb, mi))
                for n_, (g, kb, mi) in enumerate(items):
                    stages.append(make(n, qb, n_, len(items), g, kb, mi))
                    n += 1
            self.run_pipe(stages)
            P.barrier()

    def phase_attnA(self, l):
        P = self.P
        with ExitStack() as es:
            kaT = self.sb(es, "kaT", [64, L], BF16)
            ikT = self.sb(es, "ikT", [64, L], BF16)
            av = self.sb(es, "av", [128, 32, 65], BF16)
            iw = self.sb(es, "iw", [128, 32, 8], F32)
            t_k = Tok(); t_ik = Tok(); t_v = Tok(); t_iw = Tok()
            P.op("sp", lambda e: e.dma_start(out=kaT[:], in_=self.zT_d[R_AKIK:R_AKIK + 64, :]), writes=[t_k], dma=True)
            P.op("sp", lambda e: e.dma_start(out=ikT[:], in_=self.zT_d[R_AKIK + 64:R_AKIK + 128, :]), writes=[t_ik], dma=True)
            P.op("sp", lambda e: e.dma_start(out=av[:], in_=self.av_d.rearrange("(b p) e -> p b e", p=128)), writes=[t_v], dma=True)
            P.op("sp", lambda e: e.dma_start(out=iw[:], in_=self.iw_d.rearrange("(b p) h -> p b h", p=128)), writes=[t_iw], dma=True)
            aq = [self.sb(es, "aq%d" % i, [64, 6, 128], BF16) for i in range(3)]
            iq = [self.sb(es, "iq%d" % i, [64, 8, 128], BF16) for i in range(3)]
            t_aq = toks(3); t_iq = toks(3)
            Dg = [self.sb(es, "Dg%d" % i, [128, 8, 128], BF16) for i in range(2)]
            t_Dg = toks(2)
            R = [self.sb(es, "R%d" % i, [128, 512], BF16) for i in range(16)]
            t_R = toks(16)
            score = [self.sb(es, "score%d" % i, [128, L], F32) for i in range(2)]
            t_sc = toks(2)
            junk = self.sb(es, "junk", [128, L], BF16)
            t_junk = Tok()
            negm = [self.sb(es, "negm%d" % i, [128, L], BF16) for i in range(2)]
            t_ng = toks(2)
            st = [self.sb(es, "st%d" % i, [128, 8], F32) for i in range(2)]
            thr = [self.sb(es, "thr%d" % i, [128, 2], F32) for i in range(2)]
            rtab = [self.sb(es, "rtab%d" % i, [128, N_BISECT], F32) for i in range(2)]
            t_st = toks(2)
            p2 = self.sb(es, "p2", [128, N_BISECT], F32)
            for i in range(N_BISECT):
                P.op("dve", lambda e, i=i: e.memset(p2[:, i:i + 1], 2.0 ** -(i + 1)), writes=[self.t_const])
            pT = [self.sb(es, "pT%d" % i, [128, 384], BF16) for i in range(4)]
            t_pT = toks(4)
            rc = [self.sb(es, "rc%d" % i, [64, 512], F32) for i in range(2)]
            t_rc = toks(2)
            osb = [self.sb(es, "osb%d" % i, [64, 512], F32) for i in range(2)]
            t_osb = toks(2)
            oT = [self.sb(es, "oT%d" % i, [64, 6, 128], BF16) for i in range(2)]
            t_oT = toks(2)
            ctr = {"ri": 0}
            SB = (3, 4, 7)

            def emit_idx(qb):
                qs = slice(qb * 128, (qb + 1) * 128)
                qi = qb % 3
                b2 = qb % 2
                nk = (qb + 1) * 128
                P.op("sp", lambda e: e.dma_start(out=aq[qi][:], in_=self.zT_d[R_AQ:R_AQ + 384, qs].rearrange("(h d) t -> d h t", d=64)),
                     writes=[t_aq[qi]], dma=True)
                ng, t_n = negm[b2], t_ng[b2]
                if qb < 2:
                    if qb == 1:
                        P.op("dve", lambda e: e.memset(ng[:, 0:128], 0.0), writes=[t_n])
                    P.op("dve", lambda e: e.tensor_copy(out=ng[:, qb * 128:(qb + 1) * 128], in_=self.cb[:, 1, :]),
                         reads=[self.t_const], writes=[t_n])
                    return
                P.op("sp", lambda e: e.dma_start(out=iq[qi][:], in_=self.zT_d[R_IQ:R_IQ + 512, qs].rearrange("(h d) t -> d h t", d=64)),
                     writes=[t_iq[qi]], dma=True)
                sc, t_s = score[b2], t_sc[b2]
                for h in range(8):
                    P.op("pool", lambda e, h=h: e.tensor_scalar(out=Dg[b2][:, h, :], in0=self.cb[:, 0, :], scalar1=iw[:, qb, h:h + 1],
                                                                scalar2=None, op0=ALU.mult),
                         reads=[self.t_const, t_iw], writes=[t_Dg[b2]])
                for c0 in range(0, nk, 512):
                    w = min(512, nk - c0)
                    lastc = (c0 + w == nk)
                    rbase = (ctr["ri"] % 2) * 8
                    ctr["ri"] += 1
                    for h in range(8):
                        bR = h % 2
                        rb = rbase + h
                        P.op("pe", lambda e, h=h, bR=bR, c0=c0, w=w: e.matmul(self.ps[bR][:, 0:w], lhsT=iq[qi][:, h, :], rhs=ikT[:, c0:c0 + w],
                                                                  start=True, stop=True),
                             reads=[t_iq[qi], t_ik], writes=[self.pst[bR]])
                        P.op("act", lambda e, bR=bR, rb=rb, w=w: e.activation(out=R[rb][:, 0:w], in_=self.ps[bR][:, 0:w], func=AF.Relu),
                             reads=[self.pst[bR]], writes=[t_R[rb]])
                    for h in range(8):
                        rb = rbase + h
                        P.op("pe", lambda e, h=h, rb=rb, w=w, lastc=lastc: e.matmul(self.ps[2][:, 0:w], lhsT=Dg[b2][:, h, :], rhs=R[rb][:, 0:w],
                                                                  start=(h == 0), stop=(h == 7 and not lastc)),
                             reads=[t_Dg[b2], t_R[rb]], writes=[self.pst[2]])
                    if lastc:
                        P.op("pe", lambda e, w=w: e.matmul(self.ps[2][:, w - 128:w], lhsT=self.cb[:, 0, :], rhs=self.cb[:, 1, :],
                                                           start=False, stop=True), reads=[self.t_const], writes=[self.pst[2]])
                    P.op("act", lambda e, c0=c0, w=w: e.activation(out=sc[:, c0:c0 + w], in_=self.ps[2][:, 0:w], func=AF.Copy),
                         reads=[self.pst[2]], writes=[t_s])
                S_, T_, RT = st[b2], thr[b2], rtab[b2]
                t_t = t_st[b2]
                P.op("dve", lambda e: e.tensor_reduce(out=S_[:, 0:1], in_=sc[:, 0:nk], axis=mybir.AxisListType.X, op=ALU.max),
                     reads=[t_s], writes=[t_t])
                P.op("dve", lambda e: e.tensor_reduce(out=S_[:, 1:2], in_=sc[:, 0:nk - 128], axis=mybir.AxisListType.X, op=ALU.min),
                     reads=[t_s], writes=[t_t])
                P.op("dve", lambda e: e.tensor_tensor(out=S_[:, 2:3], in0=S_[:, 0:1], in1=S_[:, 1:2], op=ALU.subtract),
                     reads=[t_t], writes=[t_t])
                P.op("dve", lambda e: e.tensor_scalar(out=RT[:], in0=p2[:], scalar1=S_[:, 2:3], scalar2=None, op0=ALU.mult),
                     reads=[t_t, self.t_const], writes=[t_t])
                P.op("dve", lambda e: e.scalar_tensor_tensor(out=T_[:, 0:1], in0=S_[:, 2:3], scalar=0.5, in1=S_[:, 1:2],
                                                             op0=ALU.mult, op1=ALU.add), reads=[t_t], writes=[t_t])
                cur = 0
                for i in range(N_BISECT):
                    P.op("dve", lambda e, cur=cur: e.tensor_scalar(
                        out=junk[:, 0:nk], in0=sc[:, 0:nk], scalar1=T_[:, cur:cur + 1], scalar2=None, op0=ALU.is_ge, op1=ALU.add,
                        accum_out=S_[:, 3:4]), reads=[t_s, t_t], writes=[t_t, t_junk])
                    P.op("dve", lambda e: e.tensor_scalar(out=S_[:, 4:5], in0=S_[:, 3:4], scalar1=255.5, scalar2=-0.5,
                                                          op0=ALU.is_ge, op1=ALU.add), reads=[t_t], writes=[t_t])
                    P.op("dve", lambda e, i=i, cur=cur: e.scalar_tensor_tensor(
                        out=T_[:, 1 - cur:2 - cur], in0=S_[:, 4:5], scalar=RT[:, i:i + 1], in1=T_[:, cur:cur + 1],
                        op0=ALU.mult, op1=ALU.add), reads=[t_t], writes=[t_t])
                    cur = 1 - cur
                P.op("dve", lambda e, cur=cur: e.tensor_scalar(out=ng[:, 0:nk], in0=sc[:, 0:nk], scalar1=T_[:, cur:cur + 1], scalar2=NEG,
                                                               op0=ALU.is_lt, op1=ALU.mult), reads=[t_s, t_t], writes=[t_n])

            def make(n, m, qb, half, kb):
                qi, ob, b2 = qb % 3, qb % 2, qb % 2
                ng, t_n = negm[b2], t_ng[b2]
                bS, pb = SB[n % 3], n % 4
                bO, bD, r2 = 5, 6, m % 2
                psS, tS = self.ps[bS], self.pst[bS]
                qs = slice(qb * 128, (qb + 1) * 128)

                def s_fn():
                    if half == 0 and kb == 0 and qb + 1 < 32:
                        emit_idx(qb + 1)
                    P.op("pe", lambda e: e.matmul(psS[:, 0:384], lhsT=ng[:, kb * 128:(kb + 1) * 128],
                                                  rhs=self.irep[:].rearrange("p h t -> p (h t)"), start=True, stop=False),
                         reads=[t_n, self.t_const], writes=[tS])
                    P.op("pe", lambda e: e.matmul(psS[:, 0:384], lhsT=kaT[:, kb * 128:(kb + 1) * 128],
                                                  rhs=aq[qi][:, 3 * half:3 * half + 3, :], start=False, stop=True),
                         reads=[t_k, t_aq[qi]], writes=[tS])
                    P.op("act", lambda e: e.activation(out=pT[pb][:], in_=psS[:, 0:384], func=AF.Exp, scale=0.125),
                         reads=[tS], writes=[t_pT[pb]])

                def pv_fn():
                    P.op("pe", lambda e: e.matmul(self.ps[bO][0:64, 0:384], lhsT=av[:, kb, 0:64], rhs=pT[pb][:], start=(kb == 0),
                                                  stop=(kb == qb)), reads=[t_v, t_pT[pb]], writes=[self.pst[bO]])
                    P.op("pe", lambda e: e.matmul(self.ps[bD][0:64, 0:384], lhsT=self.ones_bf[:, 0:64], rhs=pT[pb][:], start=(kb == 0),
                                                  stop=(kb == qb)), reads=[self.t_const, t_pT[pb]], writes=[self.pst[bD]])
                    if kb == qb:
                        self.normalize_out2(self.ps[bO], self.ps[bD], self.pst[bO], self.pst[bD], 384, rc[r2], t_rc[r2],
                                            osb[r2], t_osb[r2],
                                            oT[ob][:, 3 * half:3 * half + 3, :].rearrange("p h t -> p (h t)"), t_oT[ob])
                        if half == 1:
                            P.op("sp", lambda e: e.dma_start(out=self.oaT_d[:, qs].rearrange("(h d) t -> d h t", d=64), in_=oT[ob][:]), reads=[t_oT[ob]], dma=True)
                return s_fn, pv_fn

            emit_idx(0)
            stages = []
            n = 0
            m = 0
            for qb in range(32):
                for half in range(2):
                    for kb in range(qb + 1):
                        stages.append(make(n, m, qb, half, kb))
                        n += 1
                    m += 1
            self.run_pipe(stages)
            P.barrier()

    def phase_M(self, l):
        P = self.P
        TW = 256
        with ExitStack() as es:
            Wa = self.sb(es, "Wa", [128, 3, D], BF16)
            Wb = self.sb(es, "Wb", [128, 2, D], BF16)
            Wc = self.sb(es, "Wc", [128, 4, D], BF16)
            Wg = self.sb(es, "Wg", [128, 8, 3 * D], BF16)
            Wo = self.sb(es, "Wo", [128, 8, D], BF16)
            t_wa, t_wb, t_wc, t_wo = Tok(), Tok(), Tok(), Tok()
            t_wg = toks(3)
            for (dst, src, tk) in ((Wa, self.w_a[l], t_wa), (Wb, self.w_b[l], t_wb), (Wc, self.w_c[l], t_wc)):
                P.op("pool", lambda e, dst=dst, src=src: e.dma_start(out=dst[:], in_=src.rearrange("(c p) m -> p c m", p=128)),
                     writes=[tk], dma=True)
            for i in range(3):
                self.load_w(Wg, self.w_in[l], C_G + i * D, D, i * D, t_wg[i])
            self.load_w(Wo, self.w_o[l], 0, D, 0, t_wo)
            oa = [self.sb(es, "oa%d" % i, [128, 3, TW], BF16) for i in range(2)]
            ob_ = [self.sb(es, "ob%d" % i, [128, 2, TW], BF16) for i in range(2)]
            oc = [self.sb(es, "oc%d" % i, [128, 4, TW], BF16) for i in range(2)]
            uT = [self.sb(es, "uTm%d" % i, [128, 8, TW], BF16) for i in range(2)]
            hT = [self.sb(es, "hTm%d" % i, [128, 8, TW], F32) for i in range(2)]
            t_in = toks(2)
            t_h = toks(2)
            sig = [self.sb(es, "sig%d" % i, [128, TW], F32) for i in range(2)]
            t_sig = toks(2)
            mm_ = [self.sb(es, "mm%d" % i, [128, TW], F32) for i in range(3)]
            t_mm = toks(3)
            mg = [self.sb(es, "mg%d" % i, [128, 8, TW], BF16) for i in range(2)]
            t_mg = toks(2)
            branches = ((Wa, oa, 3, t_wa), (Wb, ob_, 2, t_wb), (Wc, oc, 4, t_wc))
            k_ = 0
            for tc in range(L // TW):
                b = tc % 2
                tsl = slice(tc * TW, (tc + 1) * TW)
                P.op("sp", lambda e, b=b, tsl=tsl: e.dma_start(out=oa[b][:], in_=self.oaT_d[:, tsl].rearrange("(c p) t -> p c t", p=128)), writes=[t_in[b]], dma=True)
                P.op("sp", lambda e, b=b, tsl=tsl: e.dma_start(out=ob_[b][:], in_=self.obT_d[:, tsl].rearrange("(c p) t -> p c t", p=128)), writes=[t_in[b]], dma=True)
                P.op("sp", lambda e, b=b, tsl=tsl: e.dma_start(out=oc[b][:], in_=self.ocT_d[:, tsl].rearrange("(c p) t -> p c t", p=128)), writes=[t_in[b]], dma=True)
                P.op("sp", lambda e, b=b, tsl=tsl: e.dma_start(out=uT[b][:], in_=self.uT_d[:, :, tsl].rearrange("c p t -> p c t")),
                     writes=[t_in[b]], dma=True)
                P.op("sp", lambda e, b=b, tsl=tsl: e.dma_start(out=hT[b][:], in_=self.hT_d[:, :, tsl].rearrange("c p t -> p c t")),
                     writes=[t_h[b]], dma=True)
                for c in range(8):
                    cs = slice(c * 128, (c + 1) * 128)
                    for i, (Wi, oi, nh, t_wi) in enumerate(branches):
                        bY = (k_ % 2) * 2
                        bG = (k_ % 2) * 2 + 1
                        sb_ = k_ % 2
                        k_ += 1
                        for h in range(nh):
                            P.op("pe", lambda e, Wi=Wi, oi=oi, h=h, cs=cs, b=b, bY=bY, nh=nh: e.matmul(
                                self.ps[bY][:, 0:TW], lhsT=Wi[:, h, cs], rhs=oi[b][:, h, :], start=(h == 0), stop=(h == nh - 1)),
                                reads=[t_wi, t_in[b]], writes=[self.pst[bY]])
                        for k in range(8):
                            P.op("pe", lambda e, k=k, i=i, c=c, b=b, bG=bG: e.matmul(
                                self.ps[bG][:, 0:TW], lhsT=Wg[:, k, i * D + c * 128:i * D + (c + 1) * 128], rhs=uT[b][:, k, :],
                                start=(k == 0), stop=(k == 7)), reads=[t_wg[i], t_in[b]], writes=[self.pst[bG]])
                        P.op("act", lambda e, bG=bG, sb_=sb_: e.activation(out=sig[sb_][:], in_=self.ps[bG][:, 0:TW], func=AF.Sigmoid),
                             reads=[self.pst[bG]], writes=[t_sig[sb_]])
                        P.op("dve", lambda e, bY=bY, sb_=sb_, i=i: e.tensor_tensor(out=mm_[i][:], in0=self.ps[bY][:, 0:TW], in1=sig[sb_][:], op=ALU.mult),
                             reads=[self.pst[bY], t_sig[sb_]], writes=[t_mm[i]])
                    P.op("pool", lambda e: e.tensor_tensor(out=mm_[0][:], in0=mm_[0][:], in1=mm_[1][:], op=ALU.add),
                         reads=[t_mm[0], t_mm[1]], writes=[t_mm[0]])
                    P.op("pool", lambda e, b=b, c=c: e.tensor_tensor(out=mg[b][:, c, :], in0=mm_[0][:], in1=mm_[2][:], op=ALU.add),
                         reads=[t_mm[0], t_mm[2]], writes=[t_mg[b]])
                for c2 in range(8):
                    bD = 4 + c2 % 2
                    for c in range(8):
                        P.op("pe", lambda e, c=c, c2=c2, b=b, bD=bD: e.matmul(
                            self.ps[bD][:, 0:TW], lhsT=Wo[:, c, c2 * 128:(c2 + 1) * 128], rhs=mg[b][:, c, :], start=(c == 0), stop=(c == 7)),
                            reads=[t_wo, t_mg[b]], writes=[self.pst[bD]])
                    P.op("dve", lambda e, c2=c2, b=b, bD=bD: e.tensor_tensor(out=hT[b][:, c2, :], in0=self.ps[bD][:, 0:TW], in1=hT[b][:, c2, :], op=ALU.add),
                         reads=[self.pst[bD], t_h[b]], writes=[t_h[b]])
                P.op("sp", lambda e, b=b, tsl=tsl: e.dma_start(out=self.hT_d[:, :, tsl].rearrange("c p t -> p c t"), in_=hT[b][:]),
                     reads=[t_h[b]], dma=True)
            P.barrier()

    def phase_F(self, l):
        P = self.P
        for half in range(2):
            with ExitStack() as es:
                Wu = self.sb(es, "Wu", [128, 8, 2048], BF16)
                Wd = self.sb(es, "Wd", [128, 16, D], BF16)
                t_w = Tok()
                t_wd = Tok()
                self.load_w(Wu, self.w_up[l], half * 2048, 2048, 0, t_w)
                srcd = self.w_down[l][half * 2048:(half + 1) * 2048, :].rearrange("(kc p) m -> p kc m", p=128)
                P.op("pool", lambda e, srcd=srcd, Wd=Wd: e.dma_start(out=Wd[:], in_=srcd), writes=[t_wd], dma=True)
                hT = [self.sb(es, "hTf%d" % i, [128, 8, 512], F32) for i in range(2)]
                t_h = toks(2)
                u2 = [self.sb(es, "u2%d" % i, [128, 8, 512], BF16) for i in range(2)]
                t_u = toks(2)
                sq = [self.sb(es, "sqf%d" % i, [128, 8, 512], BF16) for i in range(2)]
                t_sq = toks(2)
                rs = [self.sb(es, "rsf%d" % i, [128, 512], F32) for i in range(2)]
                t_rs = toks(2)
                hid = [self.sb(es, "hid%d" % i, [128, 16, 512], BF16) for i in range(2)]
                t_hid = toks(2)
                rl = [self.sb(es, "rl%d" % i, [128, 512], F32) for i in range(2)]
                t_rl = toks(2)
                k_ = 0
                for tc in range(8):
                    b = tc % 2
                    tsl = slice(tc * 512, (tc + 1) * 512)
                    P.op("sp", lambda e, b=b, tsl=tsl: e.dma_start(out=hT[b][:], in_=self.hT_d[:, :, tsl].rearrange("c p t -> p c t")),
                         writes=[t_h[b]], dma=True)
                    if half == 0:
                        self.norm_chunk(None, hT[b], t_h[b], lambda c: self.g_mlp[:, l, c:c + 1],
                                        lambda c, b=b: u2[b][:, c, :], t_u[b], sq[b], t_sq[b], rs[b], t_rs[b], 6 + b, l)
                        P.op("sp", lambda e, b=b, tsl=tsl: e.dma_start(out=self.uT_d[:, :, tsl].rearrange("c p t -> p c t"), in_=u2[b][:]),
                             reads=[t_u[b]], dma=True)
                    else:
                        P.op("sp", lambda e, b=b, tsl=tsl: e.dma_start(out=u2[b][:], in_=self.uT_d[:, :, tsl].rearrange("c p t -> p c t")),
                             writes=[t_u[b]], dma=True)
                    for f in range(16):
                        bU = k_ % 4
                        rb = k_ % 2
                        k_ += 1
                        for k in range(8):
                            P.op("pe", lambda e, k=k, f=f, b=b, bU=bU: e.matmul(
                                self.ps[bU][:], lhsT=Wu[:, k, f * 128:(f + 1) * 128], rhs=u2[b][:, k, :], start=(k == 0), stop=(k == 7)),
                                reads=[t_w, t_u[b]], writes=[self.pst[bU]])
                        P.op("act", lambda e, bU=bU, rb=rb: e.activation(out=rl[rb][:], in_=self.ps[bU][:], func=AF.Relu),
                             reads=[self.pst[bU]], writes=[t_rl[rb]])
                        eng = "dve" if f % 2 == 0 else "pool"
                        P.op(eng, lambda e, rb=rb, b=b, f=f: e.tensor_tensor(out=hid[b][:, f, :], in0=rl[rb][:], in1=rl[rb][:], op=ALU.mult),
                             reads=[t_rl[rb]], writes=[t_hid[b]])
                    for c2 in range(8):
                        bD = 4 + c2 % 2
                        for f in range(16):
                            P.op("pe", lambda e, f=f, c2=c2, b=b, bD=bD: e.matmul(
                                self.ps[bD][:], lhsT=Wd[:, f, c2 * 128:(c2 + 1) * 128], rhs=hid[b][:, f, :], start=(f == 0), stop=(f == 15)),
                                reads=[t_wd, t_hid[b]], writes=[self.pst[bD]])
                        P.op("dve", lambda e, c2=c2, b=b, bD=bD: e.tensor_tensor(out=hT[b][:, c2, :], in0=self.ps[bD][:], in1=hT[b][:, c2, :], op=ALU.add),
                             reads=[self.pst[bD], t_h[b]], writes=[t_h[b]])
                    P.op("sp", lambda e, b=b, tsl=tsl: e.dma_start(out=self.hT_d[:, :, tsl].rearrange("c p t -> p c t"), in_=hT[b][:]),
                         reads=[t_h[b]], dma=True)
                if half == 1 and l == 0:
                    self.dump("d_hid", hid[0][:], [128, 16, 512], BF16, t_hid[0])
                    self.dump("d_wu", Wu[:], [128, 8, 2048], BF16, t_w)
                    self.dump("d_wd", Wd[:], [128, 16, D], BF16, t_w)
                    self.dump("d_u2", u2[0][:], [128, 8, 512], BF16, t_u[0])
                P.barrier()

    def phase_O(self):
        P = self.P
        with ExitStack() as es:
            hT = [self.sb(es, "hTo%d" % i, [128, 8, 512], F32) for i in range(2)]
            t_h = toks(2)
            y = [self.sb(es, "yo%d" % i, [128, 8, 512], F32) for i in range(2)]
            t_y = toks(2)
            sq = [self.sb(es, "sqo%d" % i, [128, 8, 512], BF16) for i in range(2)]
            t_sq = toks(2)
            rs = [self.sb(es, "rso%d" % i, [128, 512], F32) for i in range(2)]
            t_rs = toks(2)
            ot = [self.sb(es, "ot%d" % i, [128, D], F32) for i in range(3)]
            t_ot = toks(3)
            oi = 0
            for tc in range(8):
                b = tc % 2
                tsl = slice(tc * 512, (tc + 1) * 512)
                P.op("sp", lambda e, b=b, tsl=tsl: e.dma_start(out=hT[b][:], in_=self.hT_d[:, :, tsl].rearrange("c p t -> p c t")),
                     writes=[t_h[b]], dma=True)
                self.norm_chunk(None, hT[b], t_h[b], lambda c: self.g_fin[:, c:c + 1],
                                lambda c, b=b: y[b][:, c, :], t_y[b], sq[b], t_sq[b], rs[b], t_rs[b], 6 + b, 0)
                for j in range(4):
                    o3 = oi % 3
                    oi += 1
                    for hh in range(2):
                        pb = (2 * j + hh) % 4
                        for q in range(4):
                            c = hh * 4 + q
                            P.op("pe", lambda e, b=b, c=c, j=j, q=q, pb=pb: e.transpose(
                                out=self.ps[pb][:, q * 128:(q + 1) * 128], in_=y[b][:, c, j * 128:(j + 1) * 128], identity=self.ident_f),
                                reads=[t_y[b], self.t_const], writes=[self.pst[pb]])
                        if hh == 0:
                            P.op("dve", lambda e, o3=o3, pb=pb: e.tensor_copy(out=ot[o3][:, 0:512], in_=self.ps[pb][:]),
                                 reads=[self.pst[pb]], writes=[t_ot[o3]])
                        else:
                            P.op("act", lambda e, o3=o3, pb=pb: e.activation(out=ot[o3][:, 512:1024], in_=self.ps[pb][:], func=AF.Copy),
                                 reads=[self.pst[pb]], writes=[t_ot[o3]])
                    r0 = tc * 512 + j * 128
                    P.op("sp", lambda e, o3=o3, r0=r0: e.dma_start(out=self.out[r0:r0 + 128, :], in_=ot[o3][:]),
                         reads=[t_ot[o3]], dma=True)
            P.barrier()


def make_consts():
    p = np.arange(128)
    half = 32
    inv = (10000.0 ** (-(np.arange(half, dtype=np.float32)) / half)).astype(np.float32)
    cvec = np.zeros((128, 4), np.float32)
    cvec[:, 0] = inv[p % 32]
    cvec[:, 1] = np.where((p % 64) < 32, -1.0, 1.0)
    cmat = np.zeros((128, 6, 128), np.float32)
    cmat[:, 0, :] = np.eye(128, dtype=np.float32)
    r = np.arange(128)[:, None]
    c = np.arange(128)[None, :]
    cmat[:, 1, :] = np.where(c > r, NEG, 0.0)
    cmat[:, 2, :] = np.where(r > c, NEG, 0.0)
    cmat[:, 3, :] = np.where(r < c, NEG, 0.0)
    partner = np.where((p % 64) < 32, p + 32, p - 32)
    cmat[partner, 5, p] = 1.0
    cmat[64:, 4, :] = 1.0
    cvec[:, 2] = (p >= 64).astype(np.float32)
    cvec[:, 3] = (p < 64).astype(np.float32)
    masks = np.zeros((128, 9, 128), np.float32)
    masks[:, 0, :] = np.where(r > c, NEG, 0.0)
    masks[:, 1, :] = np.where(r < c, NEG, 0.0)
    for base, dl in ((2, 4), (5, 16)):
        res = ((c - r) % dl) == 0
        masks[:, base + 0, :] = np.where(res & (r <= c), 0.0, NEG)
        masks[:, base + 1, :] = np.where(res, 0.0, NEG)
        masks[:, base + 2, :] = np.where(res & (c <= r), 0.0, NEG)
    masks[:, 8, :] = np.where(r <= c, NEG, 0.0)
    return cvec, cmat, masks


def build_inputs(inputs, b):
    cvec, cmat, masks = make_consts()
    m = {
        "x": np.ascontiguousarray(inputs["x"][b]),
        "pos": np.ascontiguousarray(inputs["positions"][b]).astype(np.int32),
        "cvec": cvec, "cmat": cmat, "masks": masks,
    }
    for k in ("attn_norm", "w_in", "idx_k_norm", "sinks", "w_a", "w_b", "w_c", "w_o", "mlp_norm", "w_up",
              "w_down", "final_norm"):
        m[k] = np.ascontiguousarray(np.asarray(inputs[k], dtype=np.float32))
    return m


def kernel(**inputs):
    bld = Builder()
    nc = bld.build()
    n = 8
    in_maps = [build_inputs(inputs, b) for b in range(n)]
    res = run_bass_kernel_spmd(nc, in_maps, core_ids=list(range(n)))
    return np.stack([r["out"] for r in res.results], axis=0)
```

```python
import math
import os
from contextlib import ExitStack

import numpy as np
import concourse.bass as bass
import concourse.mybir as mybir
from concourse.bass_utils import run_bass_kernel_spmd

F32 = mybir.dt.float32
BF16 = mybir.dt.bfloat16
I32 = mybir.dt.int32
AF = mybir.ActivationFunctionType
ALU = mybir.AluOpType

L = 4096
D = 1024
DEPTH = 2
NEG = -30000.0
EPS = 1e-6
N_BISECT = 14

C_AQ, C_AK, C_AV, C_IQ, C_IK, C_IW = 0, 384, 448, 512, 1024, 1088
C_BQ, C_BK, C_BV, C_CQ, C_CK, C_CV, C_G = 1096, 1864, 2632, 3400, 3912, 4040, 4168

R_AQ, R_AKIK, R_IQ, R_BQ, R_BK, R_CQ, R_CK = 0, 384, 512, 1024, 1792, 2560, 3072
N_ROPED = 3200


class Tok:
    __slots__ = ("w", "rs", "rd")

    def __init__(self):
        self.w = None
        self.rs = {}
        self.rd = []


def toks(n):
    return [Tok() for _ in range(n)]


class _Op:
    __slots__ = ("eng", "fn", "waits", "sem", "val", "inc", "is_dma")


class Prog:
    ENGS = ("pe", "act", "dve", "pool", "sp")

    def __init__(self, nc, ndma=36):
        self.nc = nc
        self.ops = {e: [] for e in self.ENGS}
        self.cnt = {e: 0 for e in self.ENGS}
        self.waited = {}
        self.ndma = ndma
        self.dma_uses = [0] * ndma
        self.dma_rr = 0
        self.nops = 0

    def _wait(self, X, sem, val):
        key = (X.eng, sem)
        if self.waited.get(key, 0) >= val:
            return
        self.waited[key] = val
        X.waits.append((sem, val))

    def op(self, eng, fn, reads=(), writes=(), dma=False):
        X = _Op()
        X.eng = eng
        X.fn = fn
        X.waits = []
        X.is_dma = dma
        deps = []
        for t in reads:
            if t.w is not None:
                deps.append((t.w, 0))
        for t in writes:
            if t.w is not None:
                deps.append((t.w, 1))
            for r in t.rs.values():
                deps.append((r, 1))
            for r in t.rd:
                deps.append((r, 1))
        if dma and eng == "pool" and not os.environ.get("NOUSEM"):
            self.n_usem = getattr(self, "n_usem", 0) + 1
            X.sem = ("u", self.n_usem - 1)
            X.val = 16
            X.inc = 16
        elif dma:
            j = self.dma_rr
            self.dma_rr = (j + 1) % self.ndma
            k = self.dma_uses[j]
            self.dma_uses[j] += 1
            X.sem = ("d", j)
            X.val = 16 * (k + 1)
            X.inc = 16
            if k > 0:
                self._wait(X, ("d", j), 16 * k)
        else:
            self.cnt[eng] += 1
            X.sem = ("e", eng)
            X.val = self.cnt[eng]
            X.inc = 1
        for d, hz in deps:
            if d is X:
                continue
            if (not d.is_dma) and (not dma) and d.eng == eng and hz == 1:
                continue
            self._wait(X, d.sem, d.val)
        for t in reads:
            if dma:
                t.rd.append(X)
            else:
                t.rs[eng] = X
        for t in writes:
            t.w = X
            t.rs = {}
            t.rd = []
        self.ops[eng].append(X)
        self.nops += 1
        return X

    def barrier(self):
        snap = dict(self.cnt)
        sd = list(self.dma_uses)
        nu = getattr(self, "n_usem", 0)
        for e in self.ENGS:
            X = _Op()
            X.eng = e
            X.fn = None
            X.waits = []
            X.is_dma = False
            X.sem = None
            X.val = 0
            X.inc = 0
            for e2 in self.ENGS:
                if e2 != e and snap[e2] > 0:
                    self._wait(X, ("e", e2), snap[e2])
            for j in range(self.ndma):
                if sd[j] > 0:
                    self._wait(X, ("d", j), 16 * sd[j])
            for j in range(nu):
                self._wait(X, ("u", j), 16)
            self.ops[e].append(X)

    def emit(self):
        nc = self.nc
        with ExitStack() as es:
            sems = {}
            for e in self.ENGS:
                sems[("e", e)] = es.enter_context(nc.semaphore("s_" + e))
            for j in range(self.ndma):
                sems[("d", j)] = es.enter_context(nc.semaphore("d_%d" % j))
            for j in range(getattr(self, "n_usem", 0)):
                sems[("u", j)] = es.enter_context(nc.semaphore("u_%d" % j))
            block = es.enter_context(nc.Block())

            def run(ename):
                def body(eng):
                    for X in self.ops[ename]:
                        for (s, v) in X.waits:
                            eng.wait_ge(sems[s], v)
                        if X.fn is None:
                            continue
                        ins = X.fn(eng)
                        ins.then_inc(sems[X.sem], X.inc)
                return body

            block.tensor(run("pe"))
            block.scalar(run("act"))
            block.vector(run("dve"))
            block.gpsimd(run("pool"))
            block.sync(run("sp"))


def sap(t, off, dims, npart=128, pstart=0):
    fs = 1
    for s in list(t.shape)[1:]:
        fs *= int(s)
    return bass.AP(t, pstart * fs + off, [[fs, npart]] + [list(d) for d in dims])


class Builder:
    def __init__(self, dbg=None, stop_after=None, skip=()):
        self.skip = skip
        self.dbg = dbg or ()
        self.stop_after = stop_after
        nc = bass.Bass("TRN2", target_bir_lowering=False)
        self.nc = nc
        self.P = Prog(nc)
        self.outs = []

        def din(name, shape, dt=F32):
            return nc.dram_tensor(name, list(shape), dt, kind="ExternalInput").ap()

        self.x = din("x", [L, D])
        self.pos = din("pos", [L], I32)
        self.attn_norm = din("attn_norm", [DEPTH, D])
        self.w_in = din("w_in", [DEPTH, D, 7240])
        self.idx_k_norm = din("idx_k_norm", [DEPTH, 64])
        self.sinks = din("sinks", [DEPTH, 8])
        self.w_a = din("w_a", [DEPTH, 384, D])
        self.w_b = din("w_b", [DEPTH, 256, D])
        self.w_c = din("w_c", [DEPTH, 512, D])
        self.w_o = din("w_o", [DEPTH, D, D])
        self.mlp_norm = din("mlp_norm", [DEPTH, D])
        self.w_up = din("w_up", [DEPTH, D, 4 * D])
        self.w_down = din("w_down", [DEPTH, 4 * D, D])
        self.final_norm = din("final_norm", [D])
        self.cvec = din("cvec", [128, 4])
        self.cmat = din("cmat", [128, 6, 128])
        self.masks = din("masks", [128, 9, 128])

        self.out = nc.dram_tensor("out", [L, D], F32, kind="ExternalOutput").ap()

        self.cos_d = self.scr("cos_d", [128, L], F32)
        self.sin_d = self.scr("sin_d", [128, L], F32)
        self.hT_d = self.scr("hT_d", [8, 128, L], F32)
        self.uT_d = self.scr("uT_d", [8, 128, L], BF16)
        self.zT_d = self.scr("zT_d", [N_ROPED, L], BF16)
        self.bv_d = self.scr("bv_d", [L, 12, 65], BF16)
        self.cv_d = self.scr("cv_d", [L, 2, 65], BF16)
        self.av_d = self.scr("av_d", [L, 65], BF16)
        self.iw_d = self.scr("iw_d", [L, 8], F32)
        self.oaT_d = self.scr("oaT_d", [384, L], BF16)
        self.obT_d = self.scr("obT_d", [256, L], BF16)
        self.ocT_d = self.scr("ocT_d", [512, L], BF16)

    def scr(self, name, shape, dt):
        kind = "ExternalOutput" if name in self.dbg else "Internal"
        t = self.nc.dram_tensor(name, list(shape), dt, kind=kind)
        if name in self.dbg:
            self.outs.append(name)
        return t.ap()

    def sb(self, es, name, shape, dt):
        self._sbn = getattr(self, "_sbn", 0) + 1
        return es.enter_context(self.nc.sbuf_tensor("%s_%d" % (name, self._sbn), list(shape), dt))

    def build(self):
        nc, P = self.nc, self.P
        with ExitStack() as es:
            self.ps = [es.enter_context(nc.psum_tensor("ps%d" % i, [128, 512], F32)) for i in range(8)]
            self.pst = toks(8)
            self.cvec_sb = self.sb(es, "cvec_sb", [128, 4], F32)
            self.cmat_sb = self.sb(es, "cmat_sb", [128, 6, 128], F32)
            self.ident_f = self.cmat_sb[:, 0, :]
            self.cb = self.sb(es, "cb", [128, 6, 128], BF16)
            self.ones_bf = self.sb(es, "ones_bf", [128, 128], BF16)
            self.ones_f = self.sb(es, "ones_f", [128, 128], F32)
            self.g_attn = self.sb(es, "g_attn", [128, DEPTH, 8], F32)
            self.g_mlp = self.sb(es, "g_mlp", [128, DEPTH, 8], F32)
            self.g_fin = self.sb(es, "g_fin", [128, 8], F32)
            self.gk = self.sb(es, "gk", [128, DEPTH, 2], F32)
            self.t_const = Tok()
            self.eps_t = self.sb(es, "eps_t", [128, 1], F32)
            P.op("dve", lambda e: e.memset(self.eps_t[:], EPS), writes=[self.t_const])
            self.phase_const()
            P.barrier()
            self.dump("d_gk", self.gk[:], [128, DEPTH, 2], F32, self.t_const)
            self.dump("d_gattn", self.g_attn[:], [128, DEPTH, 8], F32, self.t_const)
            if self.stop_after == "const":
                return self.finish()
            self.attn_consts(es)
            P.barrier()
            for l in range(DEPTH):
                self.phase_A(l)
                P.barrier()
                if self.stop_after in (("A", l), ("A1", l), ("A2a", l)):
                    return self.finish()
                for nm, fn in (("aC", self.phase_attnC), ("aB", self.phase_attnB), ("aA", self.phase_attnA),
                               ("M", self.phase_M), ("F", self.phase_F)):
                    if nm not in self.skip:
                        fn(l)
                    if self.stop_after == (nm, l):
                        return self.finish()
            self.phase_O()
            return self.finish()

    def dump(self, name, src_ap, shape, dt, tok):
        if name not in self.dbg:
            return
        t = self.nc.dram_tensor(name, list(shape), dt, kind="ExternalOutput").ap()
        self.outs.append(name)
        self.P.op("sp", lambda e: e.dma_start(out=t, in_=src_ap), reads=[tok], dma=True)

    def finish(self):
        self.P.barrier()
        self.P.emit()
        return self.nc

    def phase_const(self):
        nc, P = self.nc, self.P
        tc_ = self.t_const
        with ExitStack() as es:
            cosT = self.sb(es, "cosT", [128, L], F32)
            sinS = self.sb(es, "sinS", [128, L], F32)
            posi = self.sb(es, "posi", [128, L], I32)
            ang = self.sb(es, "ang", [128, L], F32)
            kk = self.sb(es, "kk", [128, L], F32)
            ki = self.sb(es, "ki", [128, L], I32)
            t1 = Tok(); t2 = Tok(); t3 = Tok(); t4 = Tok()
            P.op("sp", lambda e: e.dma_start(out=self.cvec_sb[:], in_=self.cvec), writes=[tc_], dma=True)
            P.op("sp", lambda e: e.dma_start(out=self.cmat_sb[:], in_=self.cmat), writes=[tc_], dma=True)
            P.op("sp", lambda e: e.dma_start(out=posi[:], in_=self.pos.partition_broadcast(128)), writes=[t1], dma=True)
            for (dst, src) in ((self.g_attn, self.attn_norm), (self.g_mlp, self.mlp_norm)):
                P.op("sp", lambda e, dst=dst, src=src: e.dma_start(
                    out=dst[:], in_=src.rearrange("l (c p) -> p l c", p=128),
                    allow_slow_non_contiguous=True), writes=[tc_], dma=True)
            P.op("sp", lambda e: e.dma_start(out=self.g_fin[:], in_=self.final_norm.rearrange("(c p) -> p c", p=128),
                                             allow_slow_non_contiguous=True), writes=[tc_], dma=True)
            P.op("dve", lambda e: e.memset(self.gk[:], 1.0), writes=[tc_])
            for l in range(DEPTH):
                src = self.idx_k_norm[l]
                P.op("sp", lambda e, l=l, src=src: e.dma_start(
                    out=self.gk[64:128, l, 0:1], in_=src.rearrange("(p o) -> p o", o=1),
                    allow_slow_non_contiguous=True), writes=[tc_], dma=True)
                P.op("sp", lambda e, l=l, src=src: e.dma_start(
                    out=self.gk[64:96, l, 1:2], in_=src[32:64].rearrange("(p o) -> p o", o=1),
                    allow_slow_non_contiguous=True), writes=[tc_], dma=True)
                P.op("sp", lambda e, l=l, src=src: e.dma_start(
                    out=self.gk[96:128, l, 1:2], in_=src[0:32].rearrange("(p o) -> p o", o=1),
                    allow_slow_non_contiguous=True), writes=[tc_], dma=True)
            P.op("dve", lambda e: e.memset(self.ones_bf[:], 1.0), writes=[tc_])
            P.op("dve", lambda e: e.memset(self.ones_f[:], 1.0), writes=[tc_])
            P.op("dve", lambda e: e.tensor_copy(out=self.cb[:], in_=self.cmat_sb[:]), reads=[tc_], writes=[tc_])
            P.op("dve", lambda e: e.tensor_copy(out=ang[:], in_=posi[:]), reads=[t1], writes=[t2])
            P.op("dve", lambda e: e.tensor_scalar(out=ang[:], in0=ang[:], scalar1=self.cvec_sb[:, 0:1], scalar2=None,
                                                  op0=ALU.mult), reads=[t2, tc_], writes=[t2])
            P.op("dve", lambda e: e.tensor_scalar(out=kk[:], in0=ang[:], scalar1=1.0 / (2 * math.pi), scalar2=0.5,
                                                  op0=ALU.mult, op1=ALU.add), reads=[t2], writes=[t3])
            P.op("dve", lambda e: e.tensor_copy(out=ki[:], in_=kk[:]), reads=[t3], writes=[t4])
            P.op("dve", lambda e: e.tensor_copy(out=kk[:], in_=ki[:]), reads=[t4], writes=[t3])
            C1 = 6.28125
            C2 = 2 * math.pi - C1
            P.op("dve", lambda e: e.scalar_tensor_tensor(out=ang[:], in0=kk[:], scalar=-C1, in1=ang[:],
                                                         op0=ALU.mult, op1=ALU.add), reads=[t3, t2], writes=[t2])
            P.op("dve", lambda e: e.scalar_tensor_tensor(out=ang[:], in0=kk[:], scalar=-C2, in1=ang[:],
                                                         op0=ALU.mult, op1=ALU.add), reads=[t3, t2], writes=[t2])
            P.op("dve", lambda e: e.tensor_scalar(out=kk[:], in0=ang[:], scalar1=-math.pi, scalar2=2 * math.pi,
                                                  op0=ALU.is_lt, op1=ALU.mult), reads=[t2], writes=[t3])
            P.op("dve", lambda e: e.tensor_tensor(out=ang[:], in0=ang[:], in1=kk[:], op=ALU.add),
                 reads=[t2, t3], writes=[t2])
            P.op("dve", lambda e: e.tensor_scalar(out=kk[:], in0=ang[:], scalar1=math.pi, scalar2=-2 * math.pi,
                                                  op0=ALU.is_gt, op1=ALU.mult), reads=[t2], writes=[t3])
            P.op("dve", lambda e: e.tensor_tensor(out=ang[:], in0=ang[:], in1=kk[:], op=ALU.add),
                 reads=[t2, t3], writes=[t2])
            P.op("dve", lambda e: e.tensor_scalar(out=ang[:], in0=ang[:], scalar1=-3.1415925, scalar2=3.1415925,
                                                  op0=ALU.max, op1=ALU.min), reads=[t2], writes=[t2])
            P.op("act", lambda e: e.activation(out=sinS[:], in_=ang[:], func=AF.Sin), reads=[t2], writes=[tc_])
            P.op("dve", lambda e: e.tensor_scalar(out=sinS[:], in0=sinS[:], scalar1=self.cvec_sb[:, 1:2],
                                                  scalar2=None, op0=ALU.mult), reads=[tc_], writes=[tc_])
            P.op("dve", lambda e: e.tensor_scalar(out=kk[:], in0=ang[:], scalar1=-1.0, scalar2=None,
                                                  op0=ALU.mult), reads=[t2], writes=[t3])
            P.op("dve", lambda e: e.tensor_tensor(out=kk[:], in0=kk[:], in1=ang[:], op=ALU.max),
                 reads=[t2, t3], writes=[t3])
            P.op("dve", lambda e: e.tensor_scalar(out=kk[:], in0=kk[:], scalar1=-1.0, scalar2=math.pi / 2,
                                                  op0=ALU.mult, op1=ALU.add), reads=[t3], writes=[t3])
            P.op("act", lambda e: e.activation(out=cosT[:], in_=kk[:], func=AF.Sin), reads=[t3], writes=[tc_])
            P.op("sp", lambda e: e.dma_start(out=self.cos_d, in_=cosT[:]), reads=[tc_], dma=True)
            P.op("sp", lambda e: e.dma_start(out=self.sin_d, in_=sinS[:]), reads=[tc_], dma=True)
            P.barrier()

    def load_w(self, dst, l_w_ap, col0, ncols, dcol0, tok):
        src = l_w_ap[:, col0:col0 + ncols].rearrange("(kc p) c -> p kc c", p=128)
        self.P.op("pool", lambda e: e.dma_start(out=dst[:, :, dcol0:dcol0 + ncols], in_=src),
                  writes=[tok], dma=True)

    def norm_chunk(self, es_names, hT, t_h, gcol, uT_out_fn, t_u, sq, t_sq, rs, t_rs, psb, l_tag):
        P = self.P
        P.op("act", lambda e: e.activation(out=sq[:], in_=hT[:], func=AF.Square), reads=[t_h], writes=[t_sq])
        for c in range(8):
            P.op("pe", lambda e, c=c: e.matmul(self.ps[psb][:], lhsT=self.ones_bf[:], rhs=sq[:, c, :],
                                               start=(c == 0), stop=(c == 7)),
                 reads=[t_sq, self.t_const], writes=[self.pst[psb]])
        P.op("act", lambda e: e.activation(out=rs[:], in_=self.ps[psb][:], func=AF.Ln, scale=1.0 / D, bias=self.eps_t[:, 0:1]),
             reads=[self.pst[psb], self.t_const], writes=[t_rs])
        P.op("act", lambda e: e.activation(out=rs[:], in_=rs[:], func=AF.Exp, scale=-0.5), reads=[t_rs], writes=[t_rs])
        for c in range(8):
            P.op("dve", lambda e, c=c: e.scalar_tensor_tensor(out=uT_out_fn(c), in0=hT[:, c, :], scalar=gcol(c),
                                                              in1=rs[:], op0=ALU.mult, op1=ALU.mult),
                 reads=[t_h, t_rs, self.t_const], writes=[t_u])

    def phase_A(self, l):
        nc, P = self.nc, self.P
        w_in = self.w_in[l]
        with ExitStack() as es:
            uT = self.sb(es, "uT", [128, 8, L], BF16)
            t_uT = toks(8)
            with ExitStack() as es1:
                hT = [self.sb(es1, "hT%d" % i, [128, 8, 512], F32) for i in range(2)]
                t_h = toks(2)
                sq = [self.sb(es1, "sq%d" % i, [128, 8, 512], BF16) for i in range(2)]
                t_sq = toks(2)
                rs = [self.sb(es1, "rs%d" % i, [128, 512], F32) for i in range(2)]
                t_rs = toks(2)
                if l == 0:
                    xt = [self.sb(es1, "xt%d" % i, [128, D], F32) for i in range(3)]
                    t_x = toks(3)
                xi = 0
                for tc in range(8):
                    b = tc % 2
                    if l == 0:
                        for j in range(4):
                            ti = tc * 4 + j
                            xb = xi % 3
                            xi += 1
                            P.op("sp", lambda e, xb=xb, ti=ti: e.dma_start(out=xt[xb][:], in_=self.x[ti * 128:(ti + 1) * 128, :]),
                                 writes=[t_x[xb]], dma=True)
                            for half in range(2):
                                pb = (2 * j + half) % 4
                                for q in range(4):
                                    c = half * 4 + q
                                    P.op("pe", lambda e, xb=xb, c=c, pb=pb, q=q: e.transpose(
                                        out=self.ps[pb][:, q * 128:(q + 1) * 128], in_=xt[xb][:, c * 128:(c + 1) * 128],
                                        identity=self.ident_f), reads=[t_x[xb], self.t_const], writes=[self.pst[pb]])
                                eng = "dve" if half == 0 else "act"
                                if eng == "dve":
                                    P.op("dve", lambda e, b=b, half=half, j=j, pb=pb: e.tensor_copy(
                                        out=hT[b][:, half * 4:half * 4 + 4, j * 128:(j + 1) * 128],
                                        in_=self.ps[pb][:].rearrange("p (q t) -> p q t", q=4)),
                                        reads=[self.pst[pb]], writes=[t_h[b]])
                                else:
                                    P.op("act", lambda e, b=b, half=half, j=j, pb=pb: e.activation(
                                        out=hT[b][:, half * 4:half * 4 + 4, j * 128:(j + 1) * 128],
                                        in_=self.ps[pb][:].rearrange("p (q t) -> p q t", q=4), func=AF.Copy),
                                        reads=[self.pst[pb]], writes=[t_h[b]])
                        P.op("sp", lambda e, b=b, tc=tc: e.dma_start(
                            out=self.hT_d[:, :, tc * 512:(tc + 1) * 512].rearrange("c p t -> p c t"), in_=hT[b][:]),
                            reads=[t_h[b]], dma=True)
                    else:
                        P.op("sp", lambda e, b=b, tc=tc: e.dma_start(
                            out=hT[b][:], in_=self.hT_d[:, :, tc * 512:(tc + 1) * 512].rearrange("c p t -> p c t")),
                            writes=[t_h[b]], dma=True)
                    self.norm_chunk(None, hT[b], t_h[b], lambda c: self.g_attn[:, l, c:c + 1],
                                    lambda c, tc=tc: uT[:, c, tc * 512:(tc + 1) * 512], t_uT[tc],
                                    sq[b], t_sq[b], rs[b], t_rs[b], 4 + b, l)
                    P.op("sp", lambda e, tc=tc: e.dma_start(
                        out=self.uT_d[:, :, tc * 512:(tc + 1) * 512].rearrange("c p t -> p c t"),
                        in_=uT[:, :, tc * 512:(tc + 1) * 512]), reads=[t_uT[tc]], dma=True)
                P.barrier()
            if self.stop_after == ("A1", l):
                return
            with ExitStack() as es2:
                cosT = self.sb(es2, "cosT", [128, L], F32)
                sinS = self.sb(es2, "sinS", [128, L], F32)
                P.op("sp", lambda e: e.dma_start(out=cosT[:], in_=self.cos_d), writes=[self.t_const], dma=True)
                P.op("sp", lambda e: e.dma_start(out=sinS[:], in_=self.sin_d), writes=[self.t_const], dma=True)
                W = [self.sb(es2, "W%d" % i, [128, 8, 512], BF16) for i in range(2)]
                Ws = [self.sb(es2, "Ws%d" % i, [128, 8, 512], BF16) for i in range(2)]
                t_W = toks(2)
                t_Ws = toks(2)
                r1 = [self.sb(es2, "r1_%d" % i, [128, 512], F32) for i in range(2)]
                r2 = [self.sb(es2, "r2_%d" % i, [128, 512], F32) for i in range(2)]
                t_r1 = toks(2)
                t_r2 = toks(2)
                ro = [self.sb(es2, "ro%d" % i, [128, 512], BF16) for i in range(3)]
                t_ro = toks(3)
                sqk = self.sb(es2, "sqk", [128, 512], BF16)
                t_sqk = Tok()
                fk = self.sb(es2, "fk", [128, 512], F32)
                t_fk = Tok()
                groups = [
                    (R_AQ, [(C_AQ, 384), (C_AK, 64), (C_IK, 64)]),
                    (R_IQ, [(C_IQ, 512)]),
                    (R_BQ, [(C_BQ, 512)]),
                    (R_BQ + 512, [(C_BQ + 512, 256), (C_BK, 256)]),
                    (R_BK + 256, [(C_BK + 256, 512)]),
                    (R_CQ, [(C_CQ, 512)]),
                    (R_CK, [(C_CK, 128)]),
                ]
                rr = 0
                ri = 0
                for gi, (row0, pieces) in enumerate(groups):
                    wb = gi % 2
                    dc = 0
                    for (c0, ncol) in pieces:
                        self.load_w(W[wb], w_in, c0, ncol, dc, t_W[wb])
                        dc += ncol
                    ncols = dc
                    nh = ncols // 64
                    wv = W[wb][:, :, 0:ncols].rearrange("p k (h two d) -> p k h two d", two=2, d=32)
                    wsv = Ws[wb][:, :, 0:ncols].rearrange("p k (h two d) -> p k h two d", two=2, d=32)
                    for k in range(8):
                        P.op("act", lambda e, k=k, wv=wv, wsv=wsv: e.activation(out=wsv[:, k, :, 0, :], in_=wv[:, k, :, 1, :], func=AF.Copy),
                             reads=[t_W[wb]], writes=[t_Ws[wb]])
                        P.op("pool", lambda e, k=k, wv=wv, wsv=wsv: e.tensor_copy(out=wsv[:, k, :, 1, :], in_=wv[:, k, :, 0, :]),
                             reads=[t_W[wb]], writes=[t_Ws[wb]])
                    for tc in range(8):
                        tsl = slice(tc * 512, (tc + 1) * 512)
                        for j in range(ncols // 128):
                            row = row0 + j * 128
                            is_kik = (row == R_AKIK)
                            pa, pb_ = 0 + (rr % 2) * 2, 1 + (rr % 2) * 2
                            rb = rr % 2
                            rr += 1
                            for k in range(8):
                                P.op("pe", lambda e, k=k, j=j, pa=pa, wb=wb, tsl=tsl: e.matmul(
                                    self.ps[pa][:], lhsT=W[wb][:, k, j * 128:(j + 1) * 128], rhs=uT[:, k, tsl],
                                    start=(k == 0), stop=(k == 7)), reads=[t_W[wb], t_uT[tc]], writes=[self.pst[pa]])
                            for k in range(8):
                                P.op("pe", lambda e, k=k, j=j, pb_=pb_, wb=wb, tsl=tsl: e.matmul(
                                    self.ps[pb_][:], lhsT=Ws[wb][:, k, j * 128:(j + 1) * 128], rhs=uT[:, k, tsl],
                                    start=(k == 0), stop=(k == 7)), reads=[t_Ws[wb], t_uT[tc]], writes=[self.pst[pb_]])
                            ob = ri % 3
                            ri += 1
                            if not is_kik:
                                P.op("dve", lambda e, pa=pa, rb=rb, tsl=tsl: e.tensor_tensor(
                                    out=r1[rb][:], in0=self.ps[pa][:], in1=cosT[:, tsl], op=ALU.mult),
                                    reads=[self.pst[pa], self.t_const], writes=[t_r1[rb]])
                                P.op("dve", lambda e, pb_=pb_, rb=rb, tsl=tsl: e.tensor_tensor(
                                    out=r2[rb][:], in0=self.ps[pb_][:], in1=sinS[:, tsl], op=ALU.mult),
                                    reads=[self.pst[pb_], self.t_const], writes=[t_r2[rb]])
                                P.op("pool", lambda e, rb=rb, ob=ob: e.tensor_tensor(
                                    out=ro[ob][:], in0=r1[rb][:], in1=r2[rb][:], op=ALU.add),
                                    reads=[t_r1[rb], t_r2[rb]], writes=[t_ro[ob]])
                            else:
                                P.op("act", lambda e, pa=pa: e.activation(out=sqk[:], in_=self.ps[pa][:], func=AF.Square),
                                     reads=[self.pst[pa]], writes=[t_sqk])
                                P.op("pe", lambda e: e.matmul(self.ps[6][:], lhsT=self.cb[:, 4, :], rhs=sqk[:],
                                                              start=True, stop=True),
                                     reads=[t_sqk, self.t_const], writes=[self.pst[6]])
                                P.op("act", lambda e: e.activation(out=fk[:], in_=self.ps[6][:], func=AF.Ln,
                                                                   scale=1.0 / 64, bias=self.eps_t[:, 0:1]),
                                     reads=[self.pst[6], self.t_const], writes=[t_fk])
                                P.op("act", lambda e: e.activation(out=fk[:], in_=fk[:], func=AF.Exp, scale=-0.5),
                                     reads=[t_fk], writes=[t_fk])
                                P.op("dve", lambda e: e.tensor_scalar(out=fk[:], in0=fk[:], scalar1=self.cvec_sb[:, 2:3],
                                                                      scalar2=self.cvec_sb[:, 3:4], op0=ALU.mult, op1=ALU.add),
                                     reads=[t_fk, self.t_const], writes=[t_fk])
                                P.op("dve", lambda e, pa=pa, rb=rb, tsl=tsl: e.scalar_tensor_tensor(
                                    out=r1[rb][:], in0=self.ps[pa][:], scalar=self.gk[:, l, 0:1], in1=cosT[:, tsl],
                                    op0=ALU.mult, op1=ALU.mult),
                                    reads=[self.pst[pa], self.t_const], writes=[t_r1[rb]])
                                P.op("dve", lambda e, pb_=pb_, rb=rb, tsl=tsl: e.scalar_tensor_tensor(
                                    out=r2[rb][:], in0=self.ps[pb_][:], scalar=self.gk[:, l, 1:2], in1=sinS[:, tsl],
                                    op0=ALU.mult, op1=ALU.mult),
                                    reads=[self.pst[pb_], self.t_const], writes=[t_r2[rb]])
                                P.op("pool", lambda e, rb=rb: e.tensor_tensor(
                                    out=r1[rb][:], in0=r1[rb][:], in1=r2[rb][:], op=ALU.add),
                                    reads=[t_r1[rb], t_r2[rb]], writes=[t_r1[rb]])
                                P.op("pool", lambda e, rb=rb, ob=ob: e.tensor_tensor(
                                    out=ro[ob][:], in0=r1[rb][:], in1=fk[:], op=ALU.mult),
                                    reads=[t_r1[rb], t_fk], writes=[t_ro[ob]])
                            P.op("sp", lambda e, ob=ob, row=row, tsl=tsl: e.dma_start(
                                out=self.zT_d[row:row + 128, tsl], in_=ro[ob][:]), reads=[t_ro[ob]], dma=True)
                P.barrier()
            if self.stop_after == ("A2a", l):
                return
            with ExitStack() as es3:
                WV = [self.sb(es3, "WV%d" % i, [128, 8, 512], BF16) for i in range(2)]
                t_WV = toks(2)
                self.load_w(WV[0], w_in, C_BV, 512, 0, t_WV[0])
                self.load_w(WV[1], w_in, C_BV + 512, 256, 0, t_WV[1])
                self.load_w(WV[1], w_in, C_CV, 128, 256, t_WV[1])
                self.load_w(WV[1], w_in, C_AV, 64, 384, t_WV[1])
                self.load_w(WV[1], w_in, C_IW, 8, 448, t_WV[1])
                bvs = [self.sb(es3, "bvs%d" % i, [128, 12, 65], BF16) for i in range(2)]
                cvs = [self.sb(es3, "cvs%d" % i, [128, 2, 65], BF16) for i in range(2)]
                avs = [self.sb(es3, "avs%d" % i, [128, 65], BF16) for i in range(2)]
                iws = [self.sb(es3, "iws%d" % i, [128, 8], F32) for i in range(2)]
                t_st = toks(2)
                for i in range(2):
                    P.op("dve", lambda e, i=i: e.memset(bvs[i][:], 1.0), writes=[t_st[i]])
                    P.op("dve", lambda e, i=i: e.memset(cvs[i][:], 1.0), writes=[t_st[i]])
                    P.op("dve", lambda e, i=i: e.memset(avs[i][:], 1.0), writes=[t_st[i]])
                for ti in range(32):
                    b = ti % 2
                    tcs = ti // 4
                    tk = slice(ti * 128, (ti + 1) * 128)
                    for k in range(8):
                        P.op("pe", lambda e, k=k, tk=tk, b=b: e.matmul(self.ps[b * 2][:], lhsT=uT[:, k, tk], rhs=WV[0][:, k, :],
                                                                     start=(k == 0), stop=(k == 7)),
                             reads=[t_uT[tcs], t_WV[0]], writes=[self.pst[b * 2]])
                    for k in range(8):
                        P.op("pe", lambda e, k=k, tk=tk, b=b: e.matmul(self.ps[b * 2 + 1][:, 0:456], lhsT=uT[:, k, tk], rhs=WV[1][:, k, 0:456],
                                                                     start=(k == 0), stop=(k == 7)),
                             reads=[t_uT[tcs], t_WV[1]], writes=[self.pst[b * 2 + 1]])
                    p0, p1 = self.ps[b * 2], self.ps[b * 2 + 1]
                    P.op("dve", lambda e, b=b, p0=p0: e.tensor_copy(out=bvs[b][:, 0:8, 0:64], in_=p0[:].rearrange("p (h d) -> p h d", d=64)),
                         reads=[self.pst[b * 2]], writes=[t_st[b]])
                    P.op("act", lambda e, b=b, p1=p1: e.activation(out=bvs[b][:, 8:12, 0:64], in_=p1[:, 0:256].rearrange("p (h d) -> p h d", d=64), func=AF.Copy),
                         reads=[self.pst[b * 2 + 1]], writes=[t_st[b]])
                    P.op("dve", lambda e, b=b, p1=p1: e.tensor_copy(out=cvs[b][:, :, 0:64], in_=p1[:, 256:384].rearrange("p (h d) -> p h d", d=64)),
                         reads=[self.pst[b * 2 + 1]], writes=[t_st[b]])
                    P.op("act", lambda e, b=b, p1=p1: e.activation(out=avs[b][:, 0:64], in_=p1[:, 384:448], func=AF.Copy),
                         reads=[self.pst[b * 2 + 1]], writes=[t_st[b]])
                    P.op("dve", lambda e, b=b, p1=p1: e.tensor_copy(out=iws[b][:], in_=p1[:, 448:456]),
                         reads=[self.pst[b * 2 + 1]], writes=[t_st[b]])
                    P.op("sp", lambda e, b=b, tk=tk: e.dma_start(out=self.bv_d[tk], in_=bvs[b][:]), reads=[t_st[b]], dma=True)
                    P.op("sp", lambda e, b=b, tk=tk: e.dma_start(out=self.cv_d[tk], in_=cvs[b][:]), reads=[t_st[b]], dma=True)
                    P.op("sp", lambda e, b=b, tk=tk: e.dma_start(out=self.av_d[tk], in_=avs[b][:]), reads=[t_st[b]], dma=True)
                    P.op("sp", lambda e, b=b, tk=tk: e.dma_start(out=self.iw_d[tk], in_=iws[b][:]), reads=[t_st[b]], dma=True)
                P.barrier()


    def attn_consts(self, es):
        P = self.P
        self.mrep = self.sb(es, "mrep", [128, 9, 4, 128], BF16)
        self.irep = self.sb(es, "irep", [128, 3, 128], BF16)
        self.zeros_bf = self.sb(es, "zeros_bf", [128, 64], BF16)
        mf = self.sb(es, "masks_f", [128, 9, 128], F32)
        t = Tok()
        P.op("sp", lambda e: e.dma_start(out=mf[:], in_=self.masks), writes=[t], dma=True)
        P.op("dve", lambda e: e.tensor_copy(out=self.mrep[:], in_=sap(mf, 0, [[128, 9], [0, 4], [1, 128]])),
             reads=[t], writes=[self.t_const])
        P.op("dve", lambda e: e.tensor_copy(out=self.irep[:], in_=sap(self.cmat_sb, 0, [[0, 3], [1, 128]])),
             reads=[self.t_const], writes=[self.t_const])
        P.op("dve", lambda e: e.memset(self.zeros_bf[:], 0.0), writes=[self.t_const])

    def normalize_out(self, psO, psD, tO, tD, n, rc, t_rc, out_ap, t_out):
        P = self.P
        P.op("act", lambda e: e.activation(out=rc[0:64, 0:n], in_=psD[0:64, 0:n], func=AF.Ln), reads=[tD], writes=[t_rc])
        P.op("act", lambda e: e.activation(out=rc[0:64, 0:n], in_=rc[0:64, 0:n], func=AF.Exp, scale=-1.0),
             reads=[t_rc], writes=[t_rc])
        P.op("dve", lambda e: e.tensor_tensor(out=out_ap, in0=psO[0:64, 0:n], in1=rc[0:64, 0:n], op=ALU.mult),
             reads=[tO, t_rc], writes=[t_out])


    def run_pipe(self, stages):
        pending = None
        for (s_fn, pv_fn) in stages:
            s_fn()
            if pending is not None:
                pending()
            pending = pv_fn
        if pending is not None:
            pending()

    def normalize_out2(self, psO, psD, tO, tD, n, rc, t_rc, osb, t_osb, out_ap, t_out):
        P = self.P
        P.op("act", lambda e: e.activation(out=rc[0:64, 0:n], in_=psD[0:64, 0:n], func=AF.Ln), reads=[tD], writes=[t_rc])
        P.op("act", lambda e: e.activation(out=rc[0:64, 0:n], in_=rc[0:64, 0:n], func=AF.Exp, scale=-1.0),
             reads=[t_rc], writes=[t_rc])
        P.op("act", lambda e: e.activation(out=osb[0:64, 0:n], in_=psO[0:64, 0:n], func=AF.Copy), reads=[tO], writes=[t_osb])
        P.op("pool", lambda e: e.tensor_tensor(out=out_ap, in0=osb[0:64, 0:n], in1=rc[0:64, 0:n], op=ALU.mult),
             reads=[t_osb, t_rc], writes=[t_out])

    def phase_attnC(self, l):
        P = self.P
        with ExitStack() as es:
            ckT = self.sb(es, "ckT", [64, 2, L], BF16)
            cv = self.sb(es, "cv", [128, 32, 2, 65], BF16)
            t_k = Tok(); t_v = Tok()
            P.op("sp", lambda e: e.dma_start(out=ckT[:], in_=self.zT_d[R_CK:R_CK + 128, :].rearrange("(g d) t -> d g t", d=64)),
                 writes=[t_k], dma=True)
            P.op("sp", lambda e: e.dma_start(out=cv[:], in_=self.cv_d.rearrange("(b p) g e -> p b g e", p=128)),
                 writes=[t_v], dma=True)
            sk = self.sb(es, "sk", [1, 8], F32)
            skrow = self.sb(es, "skrow", [1, 8, 128], F32)
            t_sk = Tok()
            P.op("sp", lambda e: e.dma_start(out=sk[:], in_=self.sinks[l:l + 1, :]), writes=[t_sk], dma=True)
            P.op("act", lambda e: e.activation(out=sk[:], in_=sk[:], func=AF.Exp), reads=[t_sk], writes=[t_sk])
            P.op("dve", lambda e: e.tensor_copy(out=skrow[:], in_=sap(sk, 0, [[1, 8], [0, 128]], npart=1)),
                 reads=[t_sk], writes=[t_sk])
            q = [self.sb(es, "cq%d" % i, [64, 8, 128], BF16) for i in range(3)]
            t_q = toks(3)
            pT = [self.sb(es, "pT%d" % i, [128, 512], BF16) for i in range(3)]
            t_pT = toks(3)
            rc = [self.sb(es, "rc%d" % i, [64, 512], F32) for i in range(2)]
            t_rc = toks(2)
            osb = [self.sb(es, "osb%d" % i, [64, 512], F32) for i in range(2)]
            t_osb = toks(2)
            oT = [self.sb(es, "oT%d" % i, [64, 8, 128], BF16) for i in range(2)]
            t_oT = toks(2)
            SB = (0, 1, 6, 7)

            def load_q(qb):
                qi = qb % 3
                qs = slice(qb * 128, (qb + 1) * 128)
                P.op("sp", lambda e: e.dma_start(out=q[qi][:], in_=self.zT_d[R_CQ:R_CQ + 512, qs].rearrange("(h d) t -> d h t", d=64)),
                     writes=[t_q[qi]], dma=True)

            def make(n, m, qb, g, n_, nkb, kb, mi):
                qi, ob = qb % 3, qb % 2
                bS, pb = SB[n % 4], n % 3
                bO, bD, r2 = 2 + m % 2, 4 + m % 2, m % 2
                psS, tS = self.ps[bS], self.pst[bS]
                qs = slice(qb * 128, (qb + 1) * 128)

                def s_fn():
                    if g == 0 and n_ == 0 and qb + 1 < 32:
                        load_q(qb + 1)
                    P.op("pe", lambda e: e.matmul(psS[:], lhsT=self.cb[:, 0, :], rhs=self.mrep[:, mi].rearrange("p h t -> p (h t)"),
                                                  start=True, stop=False), reads=[self.t_const], writes=[tS])
                    P.op("pe", lambda e: e.matmul(psS[:], lhsT=ckT[:, g, kb * 128:(kb + 1) * 128], rhs=q[qi][:, 4 * g:4 * g + 4, :],
                                                  start=False, stop=True), reads=[t_k, t_q[qi]], writes=[tS])
                    P.op("act", lambda e: e.activation(out=pT[pb][:], in_=psS[:], func=AF.Exp, scale=0.125),
                         reads=[tS], writes=[t_pT[pb]])

                def pv_fn():
                    P.op("pe", lambda e: e.matmul(self.ps[bO][0:64, :], lhsT=cv[:, kb, g, 0:64], rhs=pT[pb][:], start=(n_ == 0),
                                                  stop=(n_ == nkb - 1)), reads=[t_v, t_pT[pb]], writes=[self.pst[bO]])
                    P.op("pe", lambda e: e.matmul(self.ps[bD][0:64, :], lhsT=self.ones_bf[:, 0:64], rhs=pT[pb][:], start=(n_ == 0),
                                                  stop=False), reads=[self.t_const, t_pT[pb]], writes=[self.pst[bD]])
                    if n_ == nkb - 1:
                        P.op("pe", lambda e: e.matmul(self.ps[bD][0:64, :], lhsT=self.ones_f[0:1, 0:64],
                                                      rhs=skrow[0:1, 4 * g:4 * g + 4, :], start=False, stop=True),
                             reads=[self.t_const, t_sk], writes=[self.pst[bD]])
                        self.normalize_out2(self.ps[bO], self.ps[bD], self.pst[bO], self.pst[bD], 512, rc[r2], t_rc[r2],
                                            osb[r2], t_osb[r2], oT[ob][:, 4 * g:4 * g + 4, :].rearrange("p h t -> p (h t)"), t_oT[ob])
                        if g == 1:
                            P.op("sp", lambda e: e.dma_start(out=self.ocT_d[:, qs].rearrange("(h d) t -> d h t", d=64), in_=oT[ob][:]), reads=[t_oT[ob]], dma=True)
                return s_fn, pv_fn

            load_q(0)
            stages = []
            n = 0
            m = 0
            for qb in range(32):
                for g in range(2):
                    kbs = ([(qb - 1, 8)] if qb > 0 else []) + [(qb, 0)]
                    for n_, (kb, mi) in enumerate(kbs):
                        stages.append(make(n, m, qb, g, n_, len(kbs), kb, mi))
                        n += 1
                    m += 1
            self.run_pipe(stages)
            P.barrier()

    def phase_attnB(self, l):
        P = self.P
        with ExitStack() as es:
            bkT = self.sb(es, "bkT", [64, 12, L], BF16)
            bv = self.sb(es, "bv", [128, 32, 12, 65], BF16)
            t_k = Tok(); t_v = Tok()
            for g in range(3):
                P.op("sp", lambda e, g=g: e.dma_start(
                    out=bkT[:, 4 * g:4 * g + 4, :],
                    in_=self.zT_d[R_BK + 256 * g:R_BK + 256 * (g + 1), :].rearrange("(h d) t -> d h t", d=64)),
                    writes=[t_k], dma=True)
            for b4 in range(4):
                P.op("sp", lambda e, b4=b4: e.dma_start(
                    out=bv[:, 8 * b4:8 * b4 + 8],
                    in_=self.bv_d[1024 * b4:1024 * (b4 + 1)].rearrange("(b p) h e -> p b h e", p=128)),
                    writes=[t_v], dma=True)
            q = [self.sb(es, "bq%d" % i, [64, 12, 128], BF16) for i in range(3)]
            t_q = toks(3)
            pT = [self.sb(es, "pT%d" % i, [128, 4, 128], BF16) for i in range(3)]
            t_pT = toks(3)
            rc = [self.sb(es, "rc%d" % i, [64, 512], F32) for i in range(2)]
            t_rc = toks(2)
            osb = [self.sb(es, "osb%d" % i, [64, 512], F32) for i in range(2)]
            t_osb = toks(2)
            oT = [self.sb(es, "oT%d" % i, [64, 4, 128], BF16) for i in range(2)]
            t_oT = toks(2)

            def load_q(qb):
                qi = qb % 3
                qs = slice(qb * 128, (qb + 1) * 128)
                P.op("sp", lambda e: e.dma_start(out=q[qi][:], in_=self.zT_d[R_BQ:R_BQ + 768, qs].rearrange("(h d) t -> d h t", d=64)),
                     writes=[t_q[qi]], dma=True)

            def make(n, qb, n_, nit, g, kb, mi):
                qi, ob = qb % 3, qb % 2
                bS, pb = n % 4, n % 3
                bO, bD = 4 + qb % 2, 6 + qb % 2
                psS, tS = self.ps[bS], self.pst[bS]
                qs = slice(qb * 128, (qb + 1) * 128)
                last = (n_ == nit - 1)

                def s_fn():
                    if n_ == 0 and qb + 1 < 32:
                        load_q(qb + 1)
                    P.op("pe", lambda e: e.matmul(psS[:], lhsT=self.cb[:, 0, :], rhs=self.mrep[:, mi].rearrange("p h t -> p (h t)"),
                                                  start=True, stop=False), reads=[self.t_const], writes=[tS])
                    for j in range(4):
                        P.op("pe", lambda e, j=j: e.matmul(psS[:, j * 128:(j + 1) * 128], lhsT=bkT[:, 4 * g + j, kb * 128:(kb + 1) * 128],
                                                           rhs=q[qi][:, 4 * g + j, :], start=False, stop=(j == 3)),
                             reads=[t_k, t_q[qi]], writes=[tS])
                    P.op("act", lambda e: e.activation(out=pT[pb][:].rearrange("p h t -> p (h t)"), in_=psS[:], func=AF.Exp, scale=0.125),
                         reads=[tS], writes=[t_pT[pb]])

                def pv_fn():
                    if n_ == 0:
                        for bb in (bO, bD):
                            P.op("pe", lambda e, bb=bb: e.matmul(self.ps[bb][0:64, :], lhsT=self.zeros_bf[:, 0:64],
                                                               rhs=self.mrep[:, 0].rearrange("p h t -> p (h t)"), start=True, stop=False),
                                 reads=[self.t_const], writes=[self.pst[bb]])
                    for j in range(4):
                        P.op("pe", lambda e, j=j: e.matmul(self.ps[bO][0:64, j * 128:(j + 1) * 128], lhsT=bv[:, kb, 4 * g + j, 0:64],
                                                           rhs=pT[pb][:, j, :], start=False, stop=(last and j == 3)),
                             reads=[t_v, t_pT[pb]], writes=[self.pst[bO]])
                    P.op("pe", lambda e: e.matmul(self.ps[bD][0:64, :], lhsT=self.ones_bf[:, 0:64], rhs=pT[pb][:].rearrange("p h t -> p (h t)"),
                                                  start=False, stop=last), reads=[self.t_const, t_pT[pb]], writes=[self.pst[bD]])
                    if last:
                        self.normalize_out2(self.ps[bO], self.ps[bD], self.pst[bO], self.pst[bD], 512, rc[ob], t_rc[ob],
                                            osb[ob], t_osb[ob], oT[ob][:].rearrange("p h t -> p (h t)"), t_oT[ob])
                        P.op("sp", lambda e: e.dma_start(out=self.obT_d[:, qs].rearrange("(h d) t -> d h t", d=64), in_=oT[ob][:]), reads=[t_oT[ob]], dma=True)
                return s_fn, pv_fn

            load_q(0)
            stages = []
            n = 0
            for qb in range(32):
                items = []
                for g, Dl in enumerate((1, 4, 16)):
                    for o in range(Dl + 1):
                        kb = qb - o
                        if kb < 0:
                            break
                        if Dl == 1:
                            mi = 0 if o == 0 else 1
                        else:
                            base = 2 if Dl == 4 else 5
                            mi = base + (0 if o == 0 else (2 if o == Dl else 1))
                        items.append((g, kb, mi))
                for n_, (g, kb, mi) in enumerate(items):
                    stages.append(make(n, qb, n_, len(items), g, kb, mi))
                    n += 1
            self.run_pipe(stages)
            P.barrier()

    def phase_attnA(self, l):
        P = self.P
        with ExitStack() as es:
            kaT = self.sb(es, "kaT", [64, L], BF16)
            ikT = self.sb(es, "ikT", [64, L], BF16)
            av = self.sb(es, "av", [128, 32, 65], BF16)
            iw = self.sb(es, "iw", [128, 32, 8], F32)
            t_k = Tok(); t_ik = Tok(); t_v = Tok(); t_iw = Tok()
            P.op("sp", lambda e: e.dma_start(out=kaT[:], in_=self.zT_d[R_AKIK:R_AKIK + 64, :]), writes=[t_k], dma=True)
            P.op("sp", lambda e: e.dma_start(out=ikT[:], in_=self.zT_d[R_AKIK + 64:R_AKIK + 128, :]), writes=[t_ik], dma=True)
            P.op("sp", lambda e: e.dma_start(out=av[:], in_=self.av_d.rearrange("(b p) e -> p b e", p=128)), writes=[t_v], dma=True)
            P.op("sp", lambda e: e.dma_start(out=iw[:], in_=self.iw_d.rearrange("(b p) h -> p b h", p=128)), writes=[t_iw], dma=True)
            aq = [self.sb(es, "aq%d" % i, [64, 6, 128], BF16) for i in range(4)]
            iq = [self.sb(es, "iq%d" % i, [64, 8, 128], BF16) for i in range(4)]
            t_aq = toks(4); t_iq = toks(4)
            Dg = [self.sb(es, "Dg%d" % i, [128, 8, 128], BF16) for i in range(2)]
            t_Dg = toks(2)
            R = [self.sb(es, "R%d" % i, [128, 512], BF16) for i in range(16)]
            t_R = toks(16)
            score = [self.sb(es, "score%d" % i, [128, L], F32) for i in range(2)]
            t_sc = toks(2)
            junk = [self.sb(es, "junk%d" % i, [128, L], BF16) for i in range(2)]
            t_junk = toks(2)
            negm = [self.sb(es, "negm%d" % i, [128, L], BF16) for i in range(4)]
            t_ng = toks(4)
            st = [self.sb(es, "st%d" % i, [128, 8], F32) for i in range(2)]
            thr = [self.sb(es, "thr%d" % i, [128, 2], F32) for i in range(2)]
            rtab = [self.sb(es, "rtab%d" % i, [128, N_BISECT], F32) for i in range(2)]
            t_st = toks(2)
            p2 = self.sb(es, "p2", [128, N_BISECT], F32)
            for i in range(N_BISECT):
                P.op("dve", lambda e, i=i: e.memset(p2[:, i:i + 1], 2.0 ** -(i + 1)), writes=[self.t_const])
            pT = [self.sb(es, "pT%d" % i, [128, 384], BF16) for i in range(4)]
            t_pT = toks(4)
            rc = [self.sb(es, "rc%d" % i, [64, 512], F32) for i in range(2)]
            t_rc = toks(2)
            osb = [self.sb(es, "osb%d" % i, [64, 512], F32) for i in range(2)]
            t_osb = toks(2)
            oT = [self.sb(es, "oT%d" % i, [64, 6, 128], BF16) for i in range(2)]
            t_oT = toks(2)
            ctr = {"ri": 0}
            SB = (3, 4, 7)

            def emit_scores(qb):
                qs = slice(qb * 128, (qb + 1) * 128)
                qi = qb % 4
                b2 = qb % 2
                nk = (qb + 1) * 128
                P.op("sp", lambda e: e.dma_start(out=aq[qi][:], in_=self.zT_d[R_AQ:R_AQ + 384, qs].rearrange("(h d) t -> d h t", d=64)),
                     writes=[t_aq[qi]], dma=True)
                ng, t_n = negm[qb % 4], t_ng[qb % 4]
                if qb < 2:
                    if qb == 1:
                        P.op("dve", lambda e: e.memset(ng[:, 0:128], 0.0), writes=[t_n])
                    P.op("dve", lambda e: e.tensor_copy(out=ng[:, qb * 128:(qb + 1) * 128], in_=self.cb[:, 1, :]),
                         reads=[self.t_const], writes=[t_n])
                    return
                P.op("sp", lambda e: e.dma_start(out=iq[qi][:], in_=self.zT_d[R_IQ:R_IQ + 512, qs].rearrange("(h d) t -> d h t", d=64)),
                     writes=[t_iq[qi]], dma=True)
                sc, t_s = score[b2], t_sc[b2]
                for h in range(8):
                    P.op("pool", lambda e, h=h: e.tensor_scalar(out=Dg[b2][:, h, :], in0=self.cb[:, 0, :], scalar1=iw[:, qb, h:h + 1],
                                                                scalar2=None, op0=ALU.mult),
                         reads=[self.t_const, t_iw], writes=[t_Dg[b2]])
                for c0 in range(0, nk, 512):
                    w = min(512, nk - c0)
                    lastc = (c0 + w == nk)
                    rbase = (ctr["ri"] % 2) * 8
                    ctr["ri"] += 1
                    for h in range(8):
                        bR = h % 2
                        rb = rbase + h
                        P.op("pe", lambda e, h=h, bR=bR, c0=c0, w=w: e.matmul(self.ps[bR][:, 0:w], lhsT=iq[qi][:, h, :], rhs=ikT[:, c0:c0 + w],
                                                                  start=True, stop=True),
                             reads=[t_iq[qi], t_ik], writes=[self.pst[bR]])
                        P.op("act", lambda e, bR=bR, rb=rb, w=w: e.activation(out=R[rb][:, 0:w], in_=self.ps[bR][:, 0:w], func=AF.Relu),
                             reads=[self.pst[bR]], writes=[t_R[rb]])
                    for h in range(8):
                        rb = rbase + h
                        P.op("pe", lambda e, h=h, rb=rb, w=w, lastc=lastc: e.matmul(self.ps[2][:, 0:w], lhsT=Dg[b2][:, h, :], rhs=R[rb][:, 0:w],
                                                                  start=(h == 0), stop=(h == 7 and not lastc)),
                             reads=[t_Dg[b2], t_R[rb]], writes=[self.pst[2]])
                    if lastc:
                        P.op("pe", lambda e, w=w: e.matmul(self.ps[2][:, w - 128:w], lhsT=self.cb[:, 0, :], rhs=self.cb[:, 1, :],
                                                           start=False, stop=True), reads=[self.t_const], writes=[self.pst[2]])
                    P.op("act", lambda e, c0=c0, w=w: e.activation(out=sc[:, c0:c0 + w], in_=self.ps[2][:, 0:w], func=AF.Copy),
                         reads=[self.pst[2]], writes=[t_s])

            def emit_bisect(qbs):
                ch = []
                for qb in qbs:
                    b2 = qb % 2
                    ch.append(dict(nk=(qb + 1) * 128, sc=score[b2], t_s=t_sc[b2], S=st[b2], T=thr[b2], RT=rtab[b2], t_t=t_st[b2],
                                   jk=junk[b2], t_j=t_junk[b2], ng=negm[qb % 4], t_n=t_ng[qb % 4]))
                for c in ch:
                    P.op("dve", lambda e, c=c: e.tensor_reduce(out=c["S"][:, 0:1], in_=c["sc"][:, 0:c["nk"]], axis=mybir.AxisListType.X, op=ALU.max),
                         reads=[c["t_s"]], writes=[c["t_t"]])
                for c in ch:
                    P.op("dve", lambda e, c=c: e.tensor_reduce(out=c["S"][:, 1:2], in_=c["sc"][:, 0:c["nk"] - 128], axis=mybir.AxisListType.X, op=ALU.min),
                         reads=[c["t_s"]], writes=[c["t_t"]])
                for c in ch:
                    P.op("dve", lambda e, c=c: e.tensor_tensor(out=c["S"][:, 2:3], in0=c["S"][:, 0:1], in1=c["S"][:, 1:2], op=ALU.subtract),
                         reads=[c["t_t"]], writes=[c["t_t"]])
                for c in ch:
                    P.op("dve", lambda e, c=c: e.tensor_scalar(out=c["RT"][:], in0=p2[:], scalar1=c["S"][:, 2:3], scalar2=None, op0=ALU.mult),
                         reads=[c["t_t"], self.t_const], writes=[c["t_t"]])
                for c in ch:
                    P.op("dve", lambda e, c=c: e.scalar_tensor_tensor(out=c["T"][:, 0:1], in0=c["S"][:, 2:3], scalar=0.5, in1=c["S"][:, 1:2],
                                                                      op0=ALU.mult, op1=ALU.add), reads=[c["t_t"]], writes=[c["t_t"]])
                cur = 0
                for i in range(N_BISECT):
                    for c in ch:
                        P.op("dve", lambda e, c=c, cur=cur: e.tensor_scalar(
                            out=c["jk"][:, 0:c["nk"]], in0=c["sc"][:, 0:c["nk"]], scalar1=c["T"][:, cur:cur + 1], scalar2=None,
                            op0=ALU.is_ge, op1=ALU.add, accum_out=c["S"][:, 3:4]), reads=[c["t_s"], c["t_t"]], writes=[c["t_t"], c["t_j"]])
                    for c in ch:
                        P.op("dve", lambda e, c=c: e.tensor_scalar(out=c["S"][:, 4:5], in0=c["S"][:, 3:4], scalar1=255.5, scalar2=-0.5,
                                                                   op0=ALU.is_ge, op1=ALU.add), reads=[c["t_t"]], writes=[c["t_t"]])
                    for c in ch:
                        P.op("dve", lambda e, c=c, i=i, cur=cur: e.scalar_tensor_tensor(
                            out=c["T"][:, 1 - cur:2 - cur], in0=c["S"][:, 4:5], scalar=c["RT"][:, i:i + 1], in1=c["T"][:, cur:cur + 1],
                            op0=ALU.mult, op1=ALU.add), reads=[c["t_t"]], writes=[c["t_t"]])
                    cur = 1 - cur
                for c in ch:
                    P.op("dve", lambda e, c=c, cur=cur: e.tensor_scalar(out=c["ng"][:, 0:c["nk"]], in0=c["sc"][:, 0:c["nk"]],
                                                                        scalar1=c["T"][:, cur:cur + 1], scalar2=NEG,
                                                                        op0=ALU.is_lt, op1=ALU.mult), reads=[c["t_s"], c["t_t"]], writes=[c["t_n"]])

            def emit_idx_pair(a0):
                qbs = [qb for qb in (a0, a0 + 1) if qb < 32]
                for qb in qbs:
                    emit_scores(qb)
                real = [qb for qb in qbs if qb >= 2]
                if real:
                    emit_bisect(real)

            def make(n, m, qb, half, kb):
                qi, ob = qb % 4, qb % 2
                ng, t_n = negm[qb % 4], t_ng[qb % 4]
                bS, pb = SB[n % 3], n % 4
                bO, bD, r2 = 5, 6, m % 2
                psS, tS = self.ps[bS], self.pst[bS]
                qs = slice(qb * 128, (qb + 1) * 128)

                def s_fn():
                    if half == 0 and kb == 0 and qb % 2 == 0 and qb + 2 < 32:
                        emit_idx_pair(qb + 2)
                    P.op("pe", lambda e: e.matmul(psS[:, 0:384], lhsT=ng[:, kb * 128:(kb + 1) * 128],
                                                  rhs=self.irep[:].rearrange("p h t -> p (h t)"), start=True, stop=False),
                         reads=[t_n, self.t_const], writes=[tS])
                    P.op("pe", lambda e: e.matmul(psS[:, 0:384], lhsT=kaT[:, kb * 128:(kb + 1) * 128],
                                                  rhs=aq[qi][:, 3 * half:3 * half + 3, :], start=False, stop=True),
                         reads=[t_k, t_aq[qi]], writes=[tS])
                    P.op("act", lambda e: e.activation(out=pT[pb][:], in_=psS[:, 0:384], func=AF.Exp, scale=0.125),
                         reads=[tS], writes=[t_pT[pb]])

                def pv_fn():
                    P.op("pe", lambda e: e.matmul(self.ps[bO][0:64, 0:384], lhsT=av[:, kb, 0:64], rhs=pT[pb][:], start=(kb == 0),
                                                  stop=(kb == qb)), reads=[t_v, t_pT[pb]], writes=[self.pst[bO]])
                    P.op("pe", lambda e: e.matmul(self.ps[bD][0:64, 0:384], lhsT=self.ones_bf[:, 0:64], rhs=pT[pb][:], start=(kb == 0),
                                                  stop=(kb == qb)), reads=[self.t_const, t_pT[pb]], writes=[self.pst[bD]])
                    if kb == qb:
                        self.normalize_out2(self.ps[bO], self.ps[bD], self.pst[bO], self.pst[bD], 384, rc[r2], t_rc[r2],
                                            osb[r2], t_osb[r2],
                                            oT[ob][:, 3 * half:3 * half + 3, :].rearrange("p h t -> p (h t)"), t_oT[ob])
                        if half == 1:
                            P.op("sp", lambda e: e.dma_start(out=self.oaT_d[:, qs].rearrange("(h d) t -> d h t", d=64), in_=oT[ob][:]), reads=[t_oT[ob]], dma=True)
                return s_fn, pv_fn

            emit_idx_pair(0)
            stages = []
            n = 0
            m = 0
            for qb in range(32):
                for half in range(2):
                    for kb in range(qb + 1):
                        stages.append(make(n, m, qb, half, kb))
                        n += 1
                    m += 1
            self.run_pipe(stages)
            P.barrier()

    def phase_M(self, l):
        P = self.P
        TW = 256
        with ExitStack() as es:
            Wa = self.sb(es, "Wa", [128, 3, D], BF16)
            Wb = self.sb(es, "Wb", [128, 2, D], BF16)
            Wc = self.sb(es, "Wc", [128, 4, D], BF16)
            Wg = self.sb(es, "Wg", [128, 8, 3 * D], BF16)
            Wo = self.sb(es, "Wo", [128, 8, D], BF16)
            t_wa, t_wb, t_wc, t_wo = Tok(), Tok(), Tok(), Tok()
            t_wg = toks(3)
            for (dst, src, tk) in ((Wa, self.w_a[l], t_wa), (Wb, self.w_b[l], t_wb), (Wc, self.w_c[l], t_wc)):
                P.op("pool", lambda e, dst=dst, src=src: e.dma_start(out=dst[:], in_=src.rearrange("(c p) m -> p c m", p=128)),
                     writes=[tk], dma=True)
            for i in range(3):
                self.load_w(Wg, self.w_in[l], C_G + i * D, D, i * D, t_wg[i])
            self.load_w(Wo, self.w_o[l], 0, D, 0, t_wo)
            oa = [self.sb(es, "oa%d" % i, [128, 3, TW], BF16) for i in range(2)]
            ob_ = [self.sb(es, "ob%d" % i, [128, 2, TW], BF16) for i in range(2)]
            oc = [self.sb(es, "oc%d" % i, [128, 4, TW], BF16) for i in range(2)]
            uT = [self.sb(es, "uTm%d" % i, [128, 8, TW], BF16) for i in range(2)]
            hT = [self.sb(es, "hTm%d" % i, [128, 8, TW], F32) for i in range(2)]
            t_in = toks(2)
            t_h = toks(2)
            sig = [self.sb(es, "sig%d" % i, [128, TW], F32) for i in range(2)]
            t_sig = toks(2)
            mm_ = [self.sb(es, "mm%d" % i, [128, TW], F32) for i in range(3)]
            t_mm = toks(3)
            mg = [self.sb(es, "mg%d" % i, [128, 8, TW], BF16) for i in range(2)]
            t_mg = toks(2)
            branches = ((Wa, oa, 3, t_wa), (Wb, ob_, 2, t_wb), (Wc, oc, 4, t_wc))
            k_ = 0
            for tc in range(L // TW):
                b = tc % 2
                tsl = slice(tc * TW, (tc + 1) * TW)
                P.op("sp", lambda e, b=b, tsl=tsl: e.dma_start(out=oa[b][:], in_=self.oaT_d[:, tsl].rearrange("(c p) t -> p c t", p=128)), writes=[t_in[b]], dma=True)
                P.op("sp", lambda e, b=b, tsl=tsl: e.dma_start(out=ob_[b][:], in_=self.obT_d[:, tsl].rearrange("(c p) t -> p c t", p=128)), writes=[t_in[b]], dma=True)
                P.op("sp", lambda e, b=b, tsl=tsl: e.dma_start(out=oc[b][:], in_=self.ocT_d[:, tsl].rearrange("(c p) t -> p c t", p=128)), writes=[t_in[b]], dma=True)
                P.op("sp", lambda e, b=b, tsl=tsl: e.dma_start(out=uT[b][:], in_=self.uT_d[:, :, tsl].rearrange("c p t -> p c t")),
                     writes=[t_in[b]], dma=True)
                P.op("sp", lambda e, b=b, tsl=tsl: e.dma_start(out=hT[b][:], in_=self.hT_d[:, :, tsl].rearrange("c p t -> p c t")),
                     writes=[t_h[b]], dma=True)
                for c in range(8):
                    cs = slice(c * 128, (c + 1) * 128)
                    for i, (Wi, oi, nh, t_wi) in enumerate(branches):
                        bY = (k_ % 2) * 2
                        bG = (k_ % 2) * 2 + 1
                        sb_ = k_ % 2
                        k_ += 1
                        for h in range(nh):
                            P.op("pe", lambda e, Wi=Wi, oi=oi, h=h, cs=cs, b=b, bY=bY, nh=nh: e.matmul(
                                self.ps[bY][:, 0:TW], lhsT=Wi[:, h, cs], rhs=oi[b][:, h, :], start=(h == 0), stop=(h == nh - 1)),
                                reads=[t_wi, t_in[b]], writes=[self.pst[bY]])
                        for k in range(8):
                            P.op("pe", lambda e, k=k, i=i, c=c, b=b, bG=bG: e.matmul(
                                self.ps[bG][:, 0:TW], lhsT=Wg[:, k, i * D + c * 128:i * D + (c + 1) * 128], rhs=uT[b][:, k, :],
                                start=(k == 0), stop=(k == 7)), reads=[t_wg[i], t_in[b]], writes=[self.pst[bG]])
                        P.op("act", lambda e, bG=bG, sb_=sb_: e.activation(out=sig[sb_][:], in_=self.ps[bG][:, 0:TW], func=AF.Sigmoid),
                             reads=[self.pst[bG]], writes=[t_sig[sb_]])
                        P.op("dve", lambda e, bY=bY, sb_=sb_, i=i: e.tensor_tensor(out=mm_[i][:], in0=self.ps[bY][:, 0:TW], in1=sig[sb_][:], op=ALU.mult),
                             reads=[self.pst[bY], t_sig[sb_]], writes=[t_mm[i]])
                    P.op("pool", lambda e: e.tensor_tensor(out=mm_[0][:], in0=mm_[0][:], in1=mm_[1][:], op=ALU.add),
                         reads=[t_mm[0], t_mm[1]], writes=[t_mm[0]])
                    P.op("pool", lambda e, b=b, c=c: e.tensor_tensor(out=mg[b][:, c, :], in0=mm_[0][:], in1=mm_[2][:], op=ALU.add),
                         reads=[t_mm[0], t_mm[2]], writes=[t_mg[b]])
                for c2 in range(8):
                    bD = 4 + c2 % 2
                    for c in range(8):
                        P.op("pe", lambda e, c=c, c2=c2, b=b, bD=bD: e.matmul(
                            self.ps[bD][:, 0:TW], lhsT=Wo[:, c, c2 * 128:(c2 + 1) * 128], rhs=mg[b][:, c, :], start=(c == 0), stop=(c == 7)),
                            reads=[t_wo, t_mg[b]], writes=[self.pst[bD]])
                    P.op("dve", lambda e, c2=c2, b=b, bD=bD: e.tensor_tensor(out=hT[b][:, c2, :], in0=self.ps[bD][:, 0:TW], in1=hT[b][:, c2, :], op=ALU.add),
                         reads=[self.pst[bD], t_h[b]], writes=[t_h[b]])
                P.op("sp", lambda e, b=b, tsl=tsl: e.dma_start(out=self.hT_d[:, :, tsl].rearrange("c p t -> p c t"), in_=hT[b][:]),
                     reads=[t_h[b]], dma=True)
            P.barrier()

    def phase_F(self, l):
        P = self.P
        for half in range(2):
            with ExitStack() as es:
                Wu = self.sb(es, "Wu", [128, 8, 2048], BF16)
                Wd = self.sb(es, "Wd", [128, 16, D], BF16)
                t_w = Tok()
                t_wd = Tok()
                self.load_w(Wu, self.w_up[l], half * 2048, 2048, 0, t_w)
                srcd = self.w_down[l][half * 2048:(half + 1) * 2048, :].rearrange("(kc p) m -> p kc m", p=128)
                P.op("pool", lambda e, srcd=srcd, Wd=Wd: e.dma_start(out=Wd[:], in_=srcd), writes=[t_wd], dma=True)
                hT = [self.sb(es, "hTf%d" % i, [128, 8, 512], F32) for i in range(2)]
                t_h = toks(2)
                u2 = [self.sb(es, "u2%d" % i, [128, 8, 512], BF16) for i in range(2)]
                t_u = toks(2)
                sq = [self.sb(es, "sqf%d" % i, [128, 8, 512], BF16) for i in range(2)]
                t_sq = toks(2)
                rs = [self.sb(es, "rsf%d" % i, [128, 512], F32) for i in range(2)]
                t_rs = toks(2)
                hid = [self.sb(es, "hid%d" % i, [128, 16, 512], BF16) for i in range(2)]
                t_hid = toks(2)
                rl = [self.sb(es, "rl%d" % i, [128, 512], F32) for i in range(2)]
                t_rl = toks(2)
                k_ = 0
                for tc in range(8):
                    b = tc % 2
                    tsl = slice(tc * 512, (tc + 1) * 512)
                    P.op("sp", lambda e, b=b, tsl=tsl: e.dma_start(out=hT[b][:], in_=self.hT_d[:, :, tsl].rearrange("c p t -> p c t")),
                         writes=[t_h[b]], dma=True)
                    if half == 0:
                        self.norm_chunk(None, hT[b], t_h[b], lambda c: self.g_mlp[:, l, c:c + 1],
                                        lambda c, b=b: u2[b][:, c, :], t_u[b], sq[b], t_sq[b], rs[b], t_rs[b], 6 + b, l)
                        P.op("sp", lambda e, b=b, tsl=tsl: e.dma_start(out=self.uT_d[:, :, tsl].rearrange("c p t -> p c t"), in_=u2[b][:]),
                             reads=[t_u[b]], dma=True)
                    else:
                        P.op("sp", lambda e, b=b, tsl=tsl: e.dma_start(out=u2[b][:], in_=self.uT_d[:, :, tsl].rearrange("c p t -> p c t")),
                             writes=[t_u[b]], dma=True)
                    for f in range(16):
                        bU = k_ % 4
                        rb = k_ % 2
                        k_ += 1
                        for k in range(8):
                            P.op("pe", lambda e, k=k, f=f, b=b, bU=bU: e.matmul(
                                self.ps[bU][:], lhsT=Wu[:, k, f * 128:(f + 1) * 128], rhs=u2[b][:, k, :], start=(k == 0), stop=(k == 7)),
                                reads=[t_w, t_u[b]], writes=[self.pst[bU]])
                        P.op("act", lambda e, bU=bU, rb=rb: e.activation(out=rl[rb][:], in_=self.ps[bU][:], func=AF.Relu),
                             reads=[self.pst[bU]], writes=[t_rl[rb]])
                        eng = "dve" if f % 2 == 0 else "pool"
                        P.op(eng, lambda e, rb=rb, b=b, f=f: e.tensor_tensor(out=hid[b][:, f, :], in0=rl[rb][:], in1=rl[rb][:], op=ALU.mult),
                             reads=[t_rl[rb]], writes=[t_hid[b]])
                    for c2 in range(8):
                        bD = 4 + c2 % 2
                        for f in range(16):
                            P.op("pe", lambda e, f=f, c2=c2, b=b, bD=bD: e.matmul(
                                self.ps[bD][:], lhsT=Wd[:, f, c2 * 128:(c2 + 1) * 128], rhs=hid[b][:, f, :], start=(f == 0), stop=(f == 15)),
                                reads=[t_wd, t_hid[b]], writes=[self.pst[bD]])
                        P.op("dve", lambda e, c2=c2, b=b, bD=bD: e.tensor_tensor(out=hT[b][:, c2, :], in0=self.ps[bD][:], in1=hT[b][:, c2, :], op=ALU.add),
                             reads=[self.pst[bD], t_h[b]], writes=[t_h[b]])
                    P.op("sp", lambda e, b=b, tsl=tsl: e.dma_start(out=self.hT_d[:, :, tsl].rearrange("c p t -> p c t"), in_=hT[b][:]),
                         reads=[t_h[b]], dma=True)
                if half == 1 and l == 0:
                    self.dump("d_hid", hid[0][:], [128, 16, 512], BF16, t_hid[0])
                    self.dump("d_wu", Wu[:], [128, 8, 2048], BF16, t_w)
                    self.dump("d_wd", Wd[:], [128, 16, D], BF16, t_w)
                    self.dump("d_u2", u2[0][:], [128, 8, 512], BF16, t_u[0])
                P.barrier()

    def phase_O(self):
        P = self.P
        with ExitStack() as es:
            hT = [self.sb(es, "hTo%d" % i, [128, 8, 512], F32) for i in range(2)]
            t_h = toks(2)
            y = [self.sb(es, "yo%d" % i, [128, 8, 512], F32) for i in range(2)]
            t_y = toks(2)
            sq = [self.sb(es, "sqo%d" % i, [128, 8, 512], BF16) for i in range(2)]
            t_sq = toks(2)
            rs = [self.sb(es, "rso%d" % i, [128, 512], F32) for i in range(2)]
            t_rs = toks(2)
            ot = [self.sb(es, "ot%d" % i, [128, D], F32) for i in range(3)]
            t_ot = toks(3)
            oi = 0
            for tc in range(8):
                b = tc % 2
                tsl = slice(tc * 512, (tc + 1) * 512)
                P.op("sp", lambda e, b=b, tsl=tsl: e.dma_start(out=hT[b][:], in_=self.hT_d[:, :, tsl].rearrange("c p t -> p c t")),
                     writes=[t_h[b]], dma=True)
                self.norm_chunk(None, hT[b], t_h[b], lambda c: self.g_fin[:, c:c + 1],
                                lambda c, b=b: y[b][:, c, :], t_y[b], sq[b], t_sq[b], rs[b], t_rs[b], 6 + b, 0)
                for j in range(4):
                    o3 = oi % 3
                    oi += 1
                    for hh in range(2):
                        pb = (2 * j + hh) % 4
                        for q in range(4):
                            c = hh * 4 + q
                            P.op("pe", lambda e, b=b, c=c, j=j, q=q, pb=pb: e.transpose(
                                out=self.ps[pb][:, q * 128:(q + 1) * 128], in_=y[b][:, c, j * 128:(j + 1) * 128], identity=self.ident_f),
                                reads=[t_y[b], self.t_const], writes=[self.pst[pb]])
                        if hh == 0:
                            P.op("dve", lambda e, o3=o3, pb=pb: e.tensor_copy(out=ot[o3][:, 0:512], in_=self.ps[pb][:]),
                                 reads=[self.pst[pb]], writes=[t_ot[o3]])
                        else:
                            P.op("act", lambda e, o3=o3, pb=pb: e.activation(out=ot[o3][:, 512:1024], in_=self.ps[pb][:], func=AF.Copy),
                                 reads=[self.pst[pb]], writes=[t_ot[o3]])
                    r0 = tc * 512 + j * 128
                    P.op("sp", lambda e, o3=o3, r0=r0: e.dma_start(out=self.out[r0:r0 + 128, :], in_=ot[o3][:]),
                         reads=[t_ot[o3]], dma=True)
            P.barrier()


def make_consts():
    p = np.arange(128)
    half = 32
    inv = (10000.0 ** (-(np.arange(half, dtype=np.float32)) / half)).astype(np.float32)
    cvec = np.zeros((128, 4), np.float32)
    cvec[:, 0] = inv[p % 32]
    cvec[:, 1] = np.where((p % 64) < 32, -1.0, 1.0)
    cmat = np.zeros((128, 6, 128), np.float32)
    cmat[:, 0, :] = np.eye(128, dtype=np.float32)
    r = np.arange(128)[:, None]
    c = np.arange(128)[None, :]
    cmat[:, 1, :] = np.where(c > r, NEG, 0.0)
    cmat[:, 2, :] = np.where(r > c, NEG, 0.0)
    cmat[:, 3, :] = np.where(r < c, NEG, 0.0)
    partner = np.where((p % 64) < 32, p + 32, p - 32)
    cmat[partner, 5, p] = 1.0
    cmat[64:, 4, :] = 1.0
    cvec[:, 2] = (p >= 64).astype(np.float32)
    cvec[:, 3] = (p < 64).astype(np.float32)
    masks = np.zeros((128, 9, 128), np.float32)
    masks[:, 0, :] = np.where(r > c, NEG, 0.0)
    masks[:, 1, :] = np.where(r < c, NEG, 0.0)
    for base, dl in ((2, 4), (5, 16)):
        res = ((c - r) % dl) == 0
        masks[:, base + 0, :] = np.where(res & (r <= c), 0.0, NEG)
        masks[:, base + 1, :] = np.where(res, 0.0, NEG)
        masks[:, base + 2, :] = np.where(res & (c <= r), 0.0, NEG)
    masks[:, 8, :] = np.where(r <= c, NEG, 0.0)
    return cvec, cmat, masks


def build_inputs(inputs, b):
    cvec, cmat, masks = make_consts()
    m = {
        "x": np.ascontiguousarray(inputs["x"][b]),
        "pos": np.ascontiguousarray(inputs["positions"][b]).astype(np.int32),
        "cvec": cvec, "cmat": cmat, "masks": masks,
    }
    for k in ("attn_norm", "w_in", "idx_k_norm", "sinks", "w_a", "w_b", "w_c", "w_o", "mlp_norm", "w_up",
              "w_down", "final_norm"):
        m[k] = np.ascontiguousarray(np.asarray(inputs[k], dtype=np.float32))
    return m


def kernel(**inputs):
    bld = Builder()
    nc = bld.build()
    n = 8
    in_maps = [build_inputs(inputs, b) for b in range(n)]
    res = run_bass_kernel_spmd(nc, in_maps, core_ids=list(range(n)))
    return np.stack([r["out"] for r in res.results], axis=0)
```

```python
import math
import os
from contextlib import ExitStack

import numpy as np
import concourse.bass as bass
import concourse.mybir as mybir
from concourse.bass_utils import run_bass_kernel_spmd

F32 = mybir.dt.float32
BF16 = mybir.dt.bfloat16
I32 = mybir.dt.int32
AF = mybir.ActivationFunctionType
ALU = mybir.AluOpType

L = 4096
D = 1024
DEPTH = 2
NEG = -30000.0
EPS = 1e-6
N_BISECT = 14

C_AQ, C_AK, C_AV, C_IQ, C_IK, C_IW = 0, 384, 448, 512, 1024, 1088
C_BQ, C_BK, C_BV, C_CQ, C_CK, C_CV, C_G = 1096, 1864, 2632, 3400, 3912, 4040, 4168

R_AQ, R_AKIK, R_IQ, R_BQ, R_BK, R_CQ, R_CK = 0, 384, 512, 1024, 1792, 2560, 3072
N_ROPED = 3200


class Tok:
    __slots__ = ("w", "rs", "rd")

    def __init__(self):
        self.w = None
        self.rs = {}
        self.rd = []


def toks(n):
    return [Tok() for _ in range(n)]


class _Op:
    __slots__ = ("eng", "fn", "waits", "sem", "val", "inc", "is_dma")


class Prog:
    ENGS = ("pe", "act", "dve", "pool", "sp")

    def __init__(self, nc, ndma=36):
        self.nc = nc
        self.ops = {e: [] for e in self.ENGS}
        self.cnt = {e: 0 for e in self.ENGS}
        self.waited = {}
        self.ndma = ndma
        self.dma_uses = [0] * ndma
        self.dma_rr = 0
        self.nops = 0

    def _wait(self, X, sem, val):
        key = (X.eng, sem)
        if self.waited.get(key, 0) >= val:
            return
        self.waited[key] = val
        X.waits.append((sem, val))

    def op(self, eng, fn, reads=(), writes=(), dma=False):
        X = _Op()
        X.eng = eng
        X.fn = fn
        X.waits = []
        X.is_dma = dma
        deps = []
        for t in reads:
            if t.w is not None:
                deps.append((t.w, 0))
        for t in writes:
            if t.w is not None:
                deps.append((t.w, 1))
            for r in t.rs.values():
                deps.append((r, 1))
            for r in t.rd:
                deps.append((r, 1))
        if dma and eng == "pool" and not os.environ.get("NOUSEM"):
            self.n_usem = getattr(self, "n_usem", 0) + 1
            X.sem = ("u", self.n_usem - 1)
            X.val = 16
            X.inc = 16
        elif dma:
            j = self.dma_rr
            self.dma_rr = (j + 1) % self.ndma
            k = self.dma_uses[j]
            self.dma_uses[j] += 1
            X.sem = ("d", j)
            X.val = 16 * (k + 1)
            X.inc = 16
            if k > 0:
                self._wait(X, ("d", j), 16 * k)
        else:
            self.cnt[eng] += 1
            X.sem = ("e", eng)
            X.val = self.cnt[eng]
            X.inc = 1
        for d, hz in deps:
            if d is X:
                continue
            if (not d.is_dma) and (not dma) and d.eng == eng and hz == 1:
                continue
            self._wait(X, d.sem, d.val)
        for t in reads:
            if dma:
                t.rd.append(X)
            else:
                t.rs[eng] = X
        for t in writes:
            t.w = X
            t.rs = {}
            t.rd = []
        self.ops[eng].append(X)
        self.nops += 1
        return X

    def barrier(self):
        snap = dict(self.cnt)
        sd = list(self.dma_uses)
        nu = getattr(self, "n_usem", 0)
        for e in self.ENGS:
            X = _Op()
            X.eng = e
            X.fn = None
            X.waits = []
            X.is_dma = False
            X.sem = None
            X.val = 0
            X.inc = 0
            for e2 in self.ENGS:
                if e2 != e and snap[e2] > 0:
                    self._wait(X, ("e", e2), snap[e2])
            for j in range(self.ndma):
                if sd[j] > 0:
                    self._wait(X, ("d", j), 16 * sd[j])
            for j in range(nu):
                self._wait(X, ("u", j), 16)
            self.ops[e].append(X)

    def emit(self):
        nc = self.nc
        with ExitStack() as es:
            sems = {}
            for e in self.ENGS:
                sems[("e", e)] = es.enter_context(nc.semaphore("s_" + e))
            for j in range(self.ndma):
                sems[("d", j)] = es.enter_context(nc.semaphore("d_%d" % j))
            for j in range(getattr(self, "n_usem", 0)):
                sems[("u", j)] = es.enter_context(nc.semaphore("u_%d" % j))
            block = es.enter_context(nc.Block())

            def run(ename):
                def body(eng):
                    for X in self.ops[ename]:
                        for (s, v) in X.waits:
                            eng.wait_ge(sems[s], v)
                        if X.fn is None:
                            continue
                        ins = X.fn(eng)
                        ins.then_inc(sems[X.sem], X.inc)
                return body

            block.tensor(run("pe"))
            block.scalar(run("act"))
            block.vector(run("dve"))
            block.gpsimd(run("pool"))
            block.sync(run("sp"))


def sap(t, off, dims, npart=128, pstart=0):
    fs = 1
    for s in list(t.shape)[1:]:
        fs *= int(s)
    return bass.AP(t, pstart * fs + off, [[fs, npart]] + [list(d) for d in dims])


class Builder:
    def __init__(self, dbg=None, stop_after=None, skip=()):
        self.skip = skip
        self.dbg = dbg or ()
        self.stop_after = stop_after
        nc = bass.Bass("TRN2", target_bir_lowering=False)
        self.nc = nc
        self.P = Prog(nc)
        self.outs = []

        def din(name, shape, dt=F32):
            return nc.dram_tensor(name, list(shape), dt, kind="ExternalInput").ap()

        self.x = din("x", [L, D])
        self.pos = din("pos", [L], I32)
        self.attn_norm = din("attn_norm", [DEPTH, D])
        self.w_in = din("w_in", [DEPTH, D, 7240])
        self.idx_k_norm = din("idx_k_norm", [DEPTH, 64])
        self.sinks = din("sinks", [DEPTH, 8])
        self.w_a = din("w_a", [DEPTH, 384, D])
        self.w_b = din("w_b", [DEPTH, 256, D])
        self.w_c = din("w_c", [DEPTH, 512, D])
        self.w_o = din("w_o", [DEPTH, D, D])
        self.mlp_norm = din("mlp_norm", [DEPTH, D])
        self.w_up = din("w_up", [DEPTH, D, 4 * D])
        self.w_down = din("w_down", [DEPTH, 4 * D, D])
        self.final_norm = din("final_norm", [D])
        self.cvec = din("cvec", [128, 4])
        self.cmat = din("cmat", [128, 6, 128])
        self.masks = din("masks", [128, 9, 128])

        self.out = nc.dram_tensor("out", [L, D], F32, kind="ExternalOutput").ap()

        self.cos_d = self.scr("cos_d", [128, L], F32)
        self.sin_d = self.scr("sin_d", [128, L], F32)
        self.hT_d = self.scr("hT_d", [8, 128, L], F32)
        self.uT_d = self.scr("uT_d", [8, 128, L], BF16)
        self.zT_d = self.scr("zT_d", [N_ROPED, L], BF16)
        self.bv_d = self.scr("bv_d", [L, 12, 65], BF16)
        self.cv_d = self.scr("cv_d", [L, 2, 65], BF16)
        self.av_d = self.scr("av_d", [L, 65], BF16)
        self.iw_d = self.scr("iw_d", [L, 8], F32)
        self.oaT_d = self.scr("oaT_d", [384, L], BF16)
        self.obT_d = self.scr("obT_d", [256, L], BF16)
        self.ocT_d = self.scr("ocT_d", [512, L], BF16)

    def scr(self, name, shape, dt):
        kind = "ExternalOutput" if name in self.dbg else "Internal"
        t = self.nc.dram_tensor(name, list(shape), dt, kind=kind)
        if name in self.dbg:
            self.outs.append(name)
        return t.ap()

    def sb(self, es, name, shape, dt):
        self._sbn = getattr(self, "_sbn", 0) + 1
        return es.enter_context(self.nc.sbuf_tensor("%s_%d" % (name, self._sbn), list(shape), dt))

    def build(self):
        nc, P = self.nc, self.P
        with ExitStack() as es:
            self.ps = [es.enter_context(nc.psum_tensor("ps%d" % i, [128, 512], F32)) for i in range(8)]
            self.pst = toks(8)
            self.cvec_sb = self.sb(es, "cvec_sb", [128, 4], F32)
            self.cmat_sb = self.sb(es, "cmat_sb", [128, 6, 128], F32)
            self.ident_f = self.cmat_sb[:, 0, :]
            self.cb = self.sb(es, "cb", [128, 6, 128], BF16)
            self.ones_bf = self.sb(es, "ones_bf", [128, 128], BF16)
            self.ones_f = self.sb(es, "ones_f", [128, 128], F32)
            self.g_attn = self.sb(es, "g_attn", [128, DEPTH, 8], F32)
            self.g_mlp = self.sb(es, "g_mlp", [128, DEPTH, 8], F32)
            self.g_fin = self.sb(es, "g_fin", [128, 8], F32)
            self.gk = self.sb(es, "gk", [128, DEPTH, 2], F32)
            self.t_const = Tok()
            self.eps_t = self.sb(es, "eps_t", [128, 1], F32)
            P.op("dve", lambda e: e.memset(self.eps_t[:], EPS), writes=[self.t_const])
            self.phase_const()
            P.barrier()
            self.dump("d_gk", self.gk[:], [128, DEPTH, 2], F32, self.t_const)
            self.dump("d_gattn", self.g_attn[:], [128, DEPTH, 8], F32, self.t_const)
            if self.stop_after == "const":
                return self.finish()
            self.attn_consts(es)
            P.barrier()
            for l in range(DEPTH):
                self.phase_A(l)
                P.barrier()
                if self.stop_after in (("A", l), ("A1", l), ("A2a", l)):
                    return self.finish()
                for nm, fn in (("aC", self.phase_attnC), ("aB", self.phase_attnB), ("aA", self.phase_attnA),
                               ("M", self.phase_M), ("F", self.phase_F)):
                    if nm not in self.skip:
                        fn(l)
                    if self.stop_after == (nm, l):
                        return self.finish()
            self.phase_O()
            return self.finish()

    def dump(self, name, src_ap, shape, dt, tok):
        if name not in self.dbg:
            return
        t = self.nc.dram_tensor(name, list(shape), dt, kind="ExternalOutput").ap()
        self.outs.append(name)
        self.P.op("sp", lambda e: e.dma_start(out=t, in_=src_ap), reads=[tok], dma=True)

    def finish(self):
        self.P.barrier()
        self.P.emit()
        return self.nc

    def phase_const(self):
        nc, P = self.nc, self.P
        tc_ = self.t_const
        with ExitStack() as es:
            cosT = self.sb(es, "cosT", [128, L], F32)
            sinS = self.sb(es, "sinS", [128, L], F32)
            posi = self.sb(es, "posi", [128, L], I32)
            ang = self.sb(es, "ang", [128, L], F32)
            kk = self.sb(es, "kk", [128, L], F32)
            ki = self.sb(es, "ki", [128, L], I32)
            t1 = Tok(); t2 = Tok(); t3 = Tok(); t4 = Tok()
            P.op("sp", lambda e: e.dma_start(out=self.cvec_sb[:], in_=self.cvec), writes=[tc_], dma=True)
            P.op("sp", lambda e: e.dma_start(out=self.cmat_sb[:], in_=self.cmat), writes=[tc_], dma=True)
            P.op("sp", lambda e: e.dma_start(out=posi[:], in_=self.pos.partition_broadcast(128)), writes=[t1], dma=True)
            for (dst, src) in ((self.g_attn, self.attn_norm), (self.g_mlp, self.mlp_norm)):
                P.op("sp", lambda e, dst=dst, src=src: e.dma_start(
                    out=dst[:], in_=src.rearrange("l (c p) -> p l c", p=128),
                    allow_slow_non_contiguous=True), writes=[tc_], dma=True)
            P.op("sp", lambda e: e.dma_start(out=self.g_fin[:], in_=self.final_norm.rearrange("(c p) -> p c", p=128),
                                             allow_slow_non_contiguous=True), writes=[tc_], dma=True)
            P.op("dve", lambda e: e.memset(self.gk[:], 1.0), writes=[tc_])
            for l in range(DEPTH):
                src = self.idx_k_norm[l]
                P.op("sp", lambda e, l=l, src=src: e.dma_start(
                    out=self.gk[64:128, l, 0:1], in_=src.rearrange("(p o) -> p o", o=1),
                    allow_slow_non_contiguous=True), writes=[tc_], dma=True)
                P.op("sp", lambda e, l=l, src=src: e.dma_start(
                    out=self.gk[64:96, l, 1:2], in_=src[32:64].rearrange("(p o) -> p o", o=1),
                    allow_slow_non_contiguous=True), writes=[tc_], dma=True)
                P.op("sp", lambda e, l=l, src=src: e.dma_start(
                    out=self.gk[96:128, l, 1:2], in_=src[0:32].rearrange("(p o) -> p o", o=1),
                    allow_slow_non_contiguous=True), writes=[tc_], dma=True)
            P.op("dve", lambda e: e.memset(self.ones_bf[:], 1.0), writes=[tc_])
            P.op("dve", lambda e: e.memset(self.ones_f[:], 1.0), writes=[tc_])
            P.op("dve", lambda e: e.tensor_copy(out=self.cb[:], in_=self.cmat_sb[:]), reads=[tc_], writes=[tc_])
            P.op("dve", lambda e: e.tensor_copy(out=ang[:], in_=posi[:]), reads=[t1], writes=[t2])
            P.op("dve", lambda e: e.tensor_scalar(out=ang[:], in0=ang[:], scalar1=self.cvec_sb[:, 0:1], scalar2=None,
                                                  op0=ALU.mult), reads=[t2, tc_], writes=[t2])
            P.op("dve", lambda e: e.tensor_scalar(out=kk[:], in0=ang[:], scalar1=1.0 / (2 * math.pi), scalar2=0.5,
                                                  op0=ALU.mult, op1=ALU.add), reads=[t2], writes=[t3])
            P.op("dve", lambda e: e.tensor_copy(out=ki[:], in_=kk[:]), reads=[t3], writes=[t4])
            P.op("dve", lambda e: e.tensor_copy(out=kk[:], in_=ki[:]), reads=[t4], writes=[t3])
            C1 = 6.28125
            C2 = 2 * math.pi - C1
            P.op("dve", lambda e: e.scalar_tensor_tensor(out=ang[:], in0=kk[:], scalar=-C1, in1=ang[:],
                                                         op0=ALU.mult, op1=ALU.add), reads=[t3, t2], writes=[t2])
            P.op("dve", lambda e: e.scalar_tensor_tensor(out=ang[:], in0=kk[:], scalar=-C2, in1=ang[:],
                                                         op0=ALU.mult, op1=ALU.add), reads=[t3, t2], writes=[t2])
            P.op("dve", lambda e: e.tensor_scalar(out=kk[:], in0=ang[:], scalar1=-math.pi, scalar2=2 * math.pi,
                                                  op0=ALU.is_lt, op1=ALU.mult), reads=[t2], writes=[t3])
            P.op("dve", lambda e: e.tensor_tensor(out=ang[:], in0=ang[:], in1=kk[:], op=ALU.add),
                 reads=[t2, t3], writes=[t2])
            P.op("dve", lambda e: e.tensor_scalar(out=kk[:], in0=ang[:], scalar1=math.pi, scalar2=-2 * math.pi,
                                                  op0=ALU.is_gt, op1=ALU.mult), reads=[t2], writes=[t3])
            P.op("dve", lambda e: e.tensor_tensor(out=ang[:], in0=ang[:], in1=kk[:], op=ALU.add),
                 reads=[t2, t3], writes=[t2])
            P.op("dve", lambda e: e.tensor_scalar(out=ang[:], in0=ang[:], scalar1=-3.1415925, scalar2=3.1415925,
                                                  op0=ALU.max, op1=ALU.min), reads=[t2], writes=[t2])
            P.op("act", lambda e: e.activation(out=sinS[:], in_=ang[:], func=AF.Sin), reads=[t2], writes=[tc_])
            P.op("dve", lambda e: e.tensor_scalar(out=sinS[:], in0=sinS[:], scalar1=self.cvec_sb[:, 1:2],
                                                  scalar2=None, op0=ALU.mult), reads=[tc_], writes=[tc_])
            P.op("dve", lambda e: e.tensor_scalar(out=kk[:], in0=ang[:], scalar1=-1.0, scalar2=None,
                                                  op0=ALU.mult), reads=[t2], writes=[t3])
            P.op("dve", lambda e: e.tensor_tensor(out=kk[:], in0=kk[:], in1=ang[:], op=ALU.max),
                 reads=[t2, t3], writes=[t3])
            P.op("dve", lambda e: e.tensor_scalar(out=kk[:], in0=kk[:], scalar1=-1.0, scalar2=math.pi / 2,
                                                  op0=ALU.mult, op1=ALU.add), reads=[t3], writes=[t3])
            P.op("act", lambda e: e.activation(out=cosT[:], in_=kk[:], func=AF.Sin), reads=[t3], writes=[tc_])
            P.op("sp", lambda e: e.dma_start(out=self.cos_d, in_=cosT[:]), reads=[tc_], dma=True)
            P.op("sp", lambda e: e.dma_start(out=self.sin_d, in_=sinS[:]), reads=[tc_], dma=True)
            P.barrier()

    def load_w(self, dst, l_w_ap, col0, ncols, dcol0, tok):
        src = l_w_ap[:, col0:col0 + ncols].rearrange("(kc p) c -> p kc c", p=128)
        self.P.op("pool", lambda e: e.dma_start(out=dst[:, :, dcol0:dcol0 + ncols], in_=src),
                  writes=[tok], dma=True)

    def norm_chunk(self, es_names, hT, t_h, gcol, uT_out_fn, t_u, sq, t_sq, rs, t_rs, psb, l_tag):
        P = self.P
        P.op("act", lambda e: e.activation(out=sq[:], in_=hT[:], func=AF.Square), reads=[t_h], writes=[t_sq])
        for c in range(8):
            P.op("pe", lambda e, c=c: e.matmul(self.ps[psb][:], lhsT=self.ones_bf[:], rhs=sq[:, c, :],
                                               start=(c == 0), stop=(c == 7)),
                 reads=[t_sq, self.t_const], writes=[self.pst[psb]])
        P.op("act", lambda e: e.activation(out=rs[:], in_=self.ps[psb][:], func=AF.Ln, scale=1.0 / D, bias=self.eps_t[:, 0:1]),
             reads=[self.pst[psb], self.t_const], writes=[t_rs])
        P.op("act", lambda e: e.activation(out=rs[:], in_=rs[:], func=AF.Exp, scale=-0.5), reads=[t_rs], writes=[t_rs])
        for c in range(8):
            P.op("dve", lambda e, c=c: e.scalar_tensor_tensor(out=uT_out_fn(c), in0=hT[:, c, :], scalar=gcol(c),
                                                              in1=rs[:], op0=ALU.mult, op1=ALU.mult),
                 reads=[t_h, t_rs, self.t_const], writes=[t_u])

    def phase_A(self, l):
        nc, P = self.nc, self.P
        w_in = self.w_in[l]
        with ExitStack() as es:
            uT = self.sb(es, "uT", [128, 8, L], BF16)
            t_uT = toks(8)
            with ExitStack() as es1:
                hT = [self.sb(es1, "hT%d" % i, [128, 8, 512], F32) for i in range(2)]
                t_h = toks(2)
                sq = [self.sb(es1, "sq%d" % i, [128, 8, 512], BF16) for i in range(2)]
                t_sq = toks(2)
                rs = [self.sb(es1, "rs%d" % i, [128, 512], F32) for i in range(2)]
                t_rs = toks(2)
                if l == 0:
                    xt = [self.sb(es1, "xt%d" % i, [128, D], F32) for i in range(3)]
                    t_x = toks(3)
                xi = 0
                for tc in range(8):
                    b = tc % 2
                    if l == 0:
                        for j in range(4):
                            ti = tc * 4 + j
                            xb = xi % 3
                            xi += 1
                            P.op("sp", lambda e, xb=xb, ti=ti: e.dma_start(out=xt[xb][:], in_=self.x[ti * 128:(ti + 1) * 128, :]),
                                 writes=[t_x[xb]], dma=True)
                            for half in range(2):
                                pb = (2 * j + half) % 4
                                for q in range(4):
                                    c = half * 4 + q
                                    P.op("pe", lambda e, xb=xb, c=c, pb=pb, q=q: e.transpose(
                                        out=self.ps[pb][:, q * 128:(q + 1) * 128], in_=xt[xb][:, c * 128:(c + 1) * 128],
                                        identity=self.ident_f), reads=[t_x[xb], self.t_const], writes=[self.pst[pb]])
                                eng = "dve" if half == 0 else "act"
                                if eng == "dve":
                                    P.op("dve", lambda e, b=b, half=half, j=j, pb=pb: e.tensor_copy(
                                        out=hT[b][:, half * 4:half * 4 + 4, j * 128:(j + 1) * 128],
                                        in_=self.ps[pb][:].rearrange("p (q t) -> p q t", q=4)),
                                        reads=[self.pst[pb]], writes=[t_h[b]])
                                else:
                                    P.op("act", lambda e, b=b, half=half, j=j, pb=pb: e.activation(
                                        out=hT[b][:, half * 4:half * 4 + 4, j * 128:(j + 1) * 128],
                                        in_=self.ps[pb][:].rearrange("p (q t) -> p q t", q=4), func=AF.Copy),
                                        reads=[self.pst[pb]], writes=[t_h[b]])
                        P.op("sp", lambda e, b=b, tc=tc: e.dma_start(
                            out=self.hT_d[:, :, tc * 512:(tc + 1) * 512].rearrange("c p t -> p c t"), in_=hT[b][:]),
                            reads=[t_h[b]], dma=True)
                    else:
                        P.op("sp", lambda e, b=b, tc=tc: e.dma_start(
                            out=hT[b][:], in_=self.hT_d[:, :, tc * 512:(tc + 1) * 512].rearrange("c p t -> p c t")),
                            writes=[t_h[b]], dma=True)
                    self.norm_chunk(None, hT[b], t_h[b], lambda c: self.g_attn[:, l, c:c + 1],
                                    lambda c, tc=tc: uT[:, c, tc * 512:(tc + 1) * 512], t_uT[tc],
                                    sq[b], t_sq[b], rs[b], t_rs[b], 4 + b, l)
                    P.op("sp", lambda e, tc=tc: e.dma_start(
                        out=self.uT_d[:, :, tc * 512:(tc + 1) * 512].rearrange("c p t -> p c t"),
                        in_=uT[:, :, tc * 512:(tc + 1) * 512]), reads=[t_uT[tc]], dma=True)
                P.barrier()
            if self.stop_after == ("A1", l):
                return
            with ExitStack() as es2:
                cosT = self.sb(es2, "cosT", [128, L], F32)
                sinS = self.sb(es2, "sinS", [128, L], F32)
                P.op("sp", lambda e: e.dma_start(out=cosT[:], in_=self.cos_d), writes=[self.t_const], dma=True)
                P.op("sp", lambda e: e.dma_start(out=sinS[:], in_=self.sin_d), writes=[self.t_const], dma=True)
                W = [self.sb(es2, "W%d" % i, [128, 8, 512], BF16) for i in range(2)]
                Ws = [self.sb(es2, "Ws%d" % i, [128, 8, 512], BF16) for i in range(2)]
                t_W = toks(2)
                t_Ws = toks(2)
                r1 = [self.sb(es2, "r1_%d" % i, [128, 512], F32) for i in range(2)]
                r2 = [self.sb(es2, "r2_%d" % i, [128, 512], F32) for i in range(2)]
                t_r1 = toks(2)
                t_r2 = toks(2)
                ro = [self.sb(es2, "ro%d" % i, [128, 512], BF16) for i in range(3)]
                t_ro = toks(3)
                sqk = self.sb(es2, "sqk", [128, 512], BF16)
                t_sqk = Tok()
                fk = self.sb(es2, "fk", [128, 512], F32)
                t_fk = Tok()
                groups = [
                    (R_AQ, [(C_AQ, 384), (C_AK, 64), (C_IK, 64)]),
                    (R_IQ, [(C_IQ, 512)]),
                    (R_BQ, [(C_BQ, 512)]),
                    (R_BQ + 512, [(C_BQ + 512, 256), (C_BK, 256)]),
                    (R_BK + 256, [(C_BK + 256, 512)]),
                    (R_CQ, [(C_CQ, 512)]),
                    (R_CK, [(C_CK, 128)]),
                ]
                rr = 0
                ri = 0
                for gi, (row0, pieces) in enumerate(groups):
                    wb = gi % 2
                    dc = 0
                    for (c0, ncol) in pieces:
                        self.load_w(W[wb], w_in, c0, ncol, dc, t_W[wb])
                        dc += ncol
                    ncols = dc
                    nh = ncols // 64
                    wv = W[wb][:, :, 0:ncols].rearrange("p k (h two d) -> p k h two d", two=2, d=32)
                    wsv = Ws[wb][:, :, 0:ncols].rearrange("p k (h two d) -> p k h two d", two=2, d=32)
                    for k in range(8):
                        P.op("act", lambda e, k=k, wv=wv, wsv=wsv: e.activation(out=wsv[:, k, :, 0, :], in_=wv[:, k, :, 1, :], func=AF.Copy),
                             reads=[t_W[wb]], writes=[t_Ws[wb]])
                        P.op("pool", lambda e, k=k, wv=wv, wsv=wsv: e.tensor_copy(out=wsv[:, k, :, 1, :], in_=wv[:, k, :, 0, :]),
                             reads=[t_W[wb]], writes=[t_Ws[wb]])
                    for tc in range(8):
                        tsl = slice(tc * 512, (tc + 1) * 512)
                        for j in range(ncols // 128):
                            row = row0 + j * 128
                            is_kik = (row == R_AKIK)
                            pa, pb_ = 0 + (rr % 2) * 2, 1 + (rr % 2) * 2
                            rb = rr % 2
                            rr += 1
                            for k in range(8):
                                P.op("pe", lambda e, k=k, j=j, pa=pa, wb=wb, tsl=tsl: e.matmul(
                                    self.ps[pa][:], lhsT=W[wb][:, k, j * 128:(j + 1) * 128], rhs=uT[:, k, tsl],
                                    start=(k == 0), stop=(k == 7)), reads=[t_W[wb], t_uT[tc]], writes=[self.pst[pa]])
                            for k in range(8):
                                P.op("pe", lambda e, k=k, j=j, pb_=pb_, wb=wb, tsl=tsl: e.matmul(
                                    self.ps[pb_][:], lhsT=Ws[wb][:, k, j * 128:(j + 1) * 128], rhs=uT[:, k, tsl],
                                    start=(k == 0), stop=(k == 7)), reads=[t_Ws[wb], t_uT[tc]], writes=[self.pst[pb_]])
                            ob = ri % 3
                            ri += 1
                            if not is_kik:
                                P.op("dve", lambda e, pa=pa, rb=rb, tsl=tsl: e.tensor_tensor(
                                    out=r1[rb][:], in0=self.ps[pa][:], in1=cosT[:, tsl], op=ALU.mult),
                                    reads=[self.pst[pa], self.t_const], writes=[t_r1[rb]])
                                P.op("dve", lambda e, pb_=pb_, rb=rb, tsl=tsl: e.tensor_tensor(
                                    out=r2[rb][:], in0=self.ps[pb_][:], in1=sinS[:, tsl], op=ALU.mult),
                                    reads=[self.pst[pb_], self.t_const], writes=[t_r2[rb]])
                                P.op("pool", lambda e, rb=rb, ob=ob: e.tensor_tensor(
                                    out=ro[ob][:], in0=r1[rb][:], in1=r2[rb][:], op=ALU.add),
                                    reads=[t_r1[rb], t_r2[rb]], writes=[t_ro[ob]])
                            else:
                                P.op("act", lambda e, pa=pa: e.activation(out=sqk[:], in_=self.ps[pa][:], func=AF.Square),
                                     reads=[self.pst[pa]], writes=[t_sqk])
                                P.op("pe", lambda e: e.matmul(self.ps[6][:], lhsT=self.cb[:, 4, :], rhs=sqk[:],
                                                              start=True, stop=True),
                                     reads=[t_sqk, self.t_const], writes=[self.pst[6]])
                                P.op("act", lambda e: e.activation(out=fk[:], in_=self.ps[6][:], func=AF.Ln,
                                                                   scale=1.0 / 64, bias=self.eps_t[:, 0:1]),
                                     reads=[self.pst[6], self.t_const], writes=[t_fk])
                                P.op("act", lambda e: e.activation(out=fk[:], in_=fk[:], func=AF.Exp, scale=-0.5),
                                     reads=[t_fk], writes=[t_fk])
                                P.op("dve", lambda e: e.tensor_scalar(out=fk[:], in0=fk[:], scalar1=self.cvec_sb[:, 2:3],
                                                                      scalar2=self.cvec_sb[:, 3:4], op0=ALU.mult, op1=ALU.add),
                                     reads=[t_fk, self.t_const], writes=[t_fk])
                                P.op("dve", lambda e, pa=pa, rb=rb, tsl=tsl: e.scalar_tensor_tensor(
                                    out=r1[rb][:], in0=self.ps[pa][:], scalar=self.gk[:, l, 0:1], in1=cosT[:, tsl],
                                    op0=ALU.mult, op1=ALU.mult),
                                    reads=[self.pst[pa], self.t_const], writes=[t_r1[rb]])
                                P.op("dve", lambda e, pb_=pb_, rb=rb, tsl=tsl: e.scalar_tensor_tensor(
                                    out=r2[rb][:], in0=self.ps[pb_][:], scalar=self.gk[:, l, 1:2], in1=sinS[:, tsl],
                                    op0=ALU.mult, op1=ALU.mult),
                                    reads=[self.pst[pb_], self.t_const], writes=[t_r2[rb]])
                                P.op("pool", lambda e, rb=rb: e.tensor_tensor(
                                    out=r1[rb][:], in0=r1[rb][:], in1=r2[rb][:], op=ALU.add),
                                    reads=[t_r1[rb], t_r2[rb]], writes=[t_r1[rb]])
                                P.op("pool", lambda e, rb=rb, ob=ob: e.tensor_tensor(
                                    out=ro[ob][:], in0=r1[rb][:], in1=fk[:], op=ALU.mult),
                                    reads=[t_r1[rb], t_fk], writes=[t_ro[ob]])
                            P.op("sp", lambda e, ob=ob, row=row, tsl=tsl: e.dma_start(
                                out=self.zT_d[row:row + 128, tsl], in_=ro[ob][:]), reads=[t_ro[ob]], dma=True)
                P.barrier()
            if self.stop_after == ("A2a", l):
                return
            with ExitStack() as es3:
                WV = [self.sb(es3, "WV%d" % i, [128, 8, 512], BF16) for i in range(2)]
                t_WV = toks(2)
                self.load_w(WV[0], w_in, C_BV, 512, 0, t_WV[0])
                self.load_w(WV[1], w_in, C_BV + 512, 256, 0, t_WV[1])
                self.load_w(WV[1], w_in, C_CV, 128, 256, t_WV[1])
                self.load_w(WV[1], w_in, C_AV, 64, 384, t_WV[1])
                self.load_w(WV[1], w_in, C_IW, 8, 448, t_WV[1])
                bvs = [self.sb(es3, "bvs%d" % i, [128, 12, 65], BF16) for i in range(2)]
                cvs = [self.sb(es3, "cvs%d" % i, [128, 2, 65], BF16) for i in range(2)]
                avs = [self.sb(es3, "avs%d" % i, [128, 65], BF16) for i in range(2)]
                iws = [self.sb(es3, "iws%d" % i, [128, 8], F32) for i in range(2)]
                t_st = toks(2)
                for i in range(2):
                    P.op("dve", lambda e, i=i: e.memset(bvs[i][:], 1.0), writes=[t_st[i]])
                    P.op("dve", lambda e, i=i: e.memset(cvs[i][:], 1.0), writes=[t_st[i]])
                    P.op("dve", lambda e, i=i: e.memset(avs[i][:], 1.0), writes=[t_st[i]])
                for ti in range(32):
                    b = ti % 2
                    tcs = ti // 4
                    tk = slice(ti * 128, (ti + 1) * 128)
                    for k in range(8):
                        P.op("pe", lambda e, k=k, tk=tk, b=b: e.matmul(self.ps[b * 2][:], lhsT=uT[:, k, tk], rhs=WV[0][:, k, :],
                                                                     start=(k == 0), stop=(k == 7)),
                             reads=[t_uT[tcs], t_WV[0]], writes=[self.pst[b * 2]])
                    for k in range(8):
                        P.op("pe", lambda e, k=k, tk=tk, b=b: e.matmul(self.ps[b * 2 + 1][:, 0:456], lhsT=uT[:, k, tk], rhs=WV[1][:, k, 0:456],
                                                                     start=(k == 0), stop=(k == 7)),
                             reads=[t_uT[tcs], t_WV[1]], writes=[self.pst[b * 2 + 1]])
                    p0, p1 = self.ps[b * 2], self.ps[b * 2 + 1]
                    P.op("dve", lambda e, b=b, p0=p0: e.tensor_copy(out=bvs[b][:, 0:8, 0:64], in_=p0[:].rearrange("p (h d) -> p h d", d=64)),
                         reads=[self.pst[b * 2]], writes=[t_st[b]])
                    P.op("act", lambda e, b=b, p1=p1: e.activation(out=bvs[b][:, 8:12, 0:64], in_=p1[:, 0:256].rearrange("p (h d) -> p h d", d=64), func=AF.Copy),
                         reads=[self.pst[b * 2 + 1]], writes=[t_st[b]])
                    P.op("dve", lambda e, b=b, p1=p1: e.tensor_copy(out=cvs[b][:, :, 0:64], in_=p1[:, 256:384].rearrange("p (h d) -> p h d", d=64)),
                         reads=[self.pst[b * 2 + 1]], writes=[t_st[b]])
                    P.op("act", lambda e, b=b, p1=p1: e.activation(out=avs[b][:, 0:64], in_=p1[:, 384:448], func=AF.Copy),
                         reads=[self.pst[b * 2 + 1]], writes=[t_st[b]])
                    P.op("dve", lambda e, b=b, p1=p1: e.tensor_copy(out=iws[b][:], in_=p1[:, 448:456]),
                         reads=[self.pst[b * 2 + 1]], writes=[t_st[b]])
                    P.op("sp", lambda e, b=b, tk=tk: e.dma_start(out=self.bv_d[tk], in_=bvs[b][:]), reads=[t_st[b]], dma=True)
                    P.op("sp", lambda e, b=b, tk=tk: e.dma_start(out=self.cv_d[tk], in_=cvs[b][:]), reads=[t_st[b]], dma=True)
                    P.op("sp", lambda e, b=b, tk=tk: e.dma_start(out=self.av_d[tk], in_=avs[b][:]), reads=[t_st[b]], dma=True)
                    P.op("sp", lambda e, b=b, tk=tk: e.dma_start(out=self.iw_d[tk], in_=iws[b][:]), reads=[t_st[b]], dma=True)
                P.barrier()


    def attn_consts(self, es):
        P = self.P
        self.mrep = self.sb(es, "mrep", [128, 9, 4, 128], BF16)
        self.irep = self.sb(es, "irep", [128, 3, 128], BF16)
        self.zeros_bf = self.sb(es, "zeros_bf", [128, 128], BF16)
        mf = self.sb(es, "masks_f", [128, 9, 128], F32)
        t = Tok()
        P.op("sp", lambda e: e.dma_start(out=mf[:], in_=self.masks), writes=[t], dma=True)
        P.op("dve", lambda e: e.tensor_copy(out=self.mrep[:], in_=sap(mf, 0, [[128, 9], [0, 4], [1, 128]])),
             reads=[t], writes=[self.t_const])
        P.op("dve", lambda e: e.tensor_copy(out=self.irep[:], in_=sap(self.cmat_sb, 0, [[0, 3], [1, 128]])),
             reads=[self.t_const], writes=[self.t_const])
        P.op("dve", lambda e: e.memset(self.zeros_bf[:], 0.0), writes=[self.t_const])

    def normalize_out(self, psO, psD, tO, tD, n, rc, t_rc, out_ap, t_out):
        P = self.P
        P.op("act", lambda e: e.activation(out=rc[0:64, 0:n], in_=psD[0:64, 0:n], func=AF.Ln), reads=[tD], writes=[t_rc])
        P.op("act", lambda e: e.activation(out=rc[0:64, 0:n], in_=rc[0:64, 0:n], func=AF.Exp, scale=-1.0),
             reads=[t_rc], writes=[t_rc])
        P.op("dve", lambda e: e.tensor_tensor(out=out_ap, in0=psO[0:64, 0:n], in1=rc[0:64, 0:n], op=ALU.mult),
             reads=[tO, t_rc], writes=[t_out])


    def run_pipe(self, stages):
        pending = None
        for (s_fn, pv_fn) in stages:
            s_fn()
            if pending is not None:
                pending()
            pending = pv_fn
        if pending is not None:
            pending()

    def normalize_out2(self, psO, psD, tO, tD, n, rc, t_rc, osb, t_osb, out_ap, t_out):
        P = self.P
        P.op("act", lambda e: e.activation(out=rc[0:64, 0:n], in_=psD[0:64, 0:n], func=AF.Ln), reads=[tD], writes=[t_rc])
        P.op("act", lambda e: e.activation(out=rc[0:64, 0:n], in_=rc[0:64, 0:n], func=AF.Exp, scale=-1.0),
             reads=[t_rc], writes=[t_rc])
        P.op("act", lambda e: e.activation(out=osb[0:64, 0:n], in_=psO[0:64, 0:n], func=AF.Copy), reads=[tO], writes=[t_osb])
        P.op("pool", lambda e: e.tensor_tensor(out=out_ap, in0=osb[0:64, 0:n], in1=rc[0:64, 0:n], op=ALU.mult),
             reads=[t_osb, t_rc], writes=[t_out])

    def phase_attnC(self, l):
        P = self.P
        with ExitStack() as es:
            ckT = self.sb(es, "ckT", [128, 2, L], BF16)
            cv = self.sb(es, "cv", [128, 33, 2, 65], BF16)
            t_k = Tok(); t_v = Tok()
            P.op("dve", lambda e: e.memset(ckT[64:128], 0.0), writes=[t_k])
            P.op("dve", lambda e: e.memset(cv[:, 32], 0.0), writes=[t_v])
            P.op("sp", lambda e: e.dma_start(out=ckT[0:64], in_=self.zT_d[R_CK:R_CK + 128, :].rearrange("(g d) t -> d g t", d=64)),
                 writes=[t_k], dma=True)
            P.op("sp", lambda e: e.dma_start(out=cv[:, 0:32], in_=self.cv_d.rearrange("(b p) g e -> p b g e", p=128)),
                 writes=[t_v], dma=True)
            sk = self.sb(es, "sk", [1, 8], F32)
            skh = self.sb(es, "skh", [1, 8], BF16)
            skl = self.sb(es, "skl", [1, 8], F32)
            skrow = self.sb(es, "skrow", [128, 2, 8, 128], BF16)
            t_sk = Tok()
            P.op("sp", lambda e: e.dma_start(out=sk[:], in_=self.sinks[l:l + 1, :]), writes=[t_sk], dma=True)
            P.op("act", lambda e: e.activation(out=sk[:], in_=sk[:], func=AF.Exp), reads=[t_sk], writes=[t_sk])
            P.op("dve", lambda e: e.memset(skrow[:], 0.0), writes=[t_sk])
            P.op("dve", lambda e: e.tensor_copy(out=skh[:], in_=sk[:]), reads=[t_sk], writes=[t_sk])
            P.op("dve", lambda e: e.tensor_tensor(out=skl[:], in0=sk[:], in1=skh[:], op=ALU.subtract), reads=[t_sk], writes=[t_sk])
            P.op("dve", lambda e: e.tensor_copy(out=skrow[0:1, 0], in_=sap(skh, 0, [[1, 8], [0, 128]], npart=1)),
                 reads=[t_sk], writes=[t_sk])
            P.op("dve", lambda e: e.tensor_copy(out=skrow[0:1, 1], in_=sap(skl, 0, [[1, 8], [0, 128]], npart=1)),
                 reads=[t_sk], writes=[t_sk])
            q = [self.sb(es, "cq%d" % i, [128, 8, 128], BF16) for i in range(3)]
            t_q = toks(3)
            for i in range(3):
                P.op("dve", lambda e, i=i: e.memset(q[i][64:128], 0.0), writes=[t_q[i]])
            pT = [self.sb(es, "pT%d" % i, [128, 512], BF16) for i in range(3)]
            t_pT = toks(3)
            rc = [self.sb(es, "rc%d" % i, [64, 512], F32) for i in range(2)]
            t_rc = toks(2)
            osb = [self.sb(es, "osb%d" % i, [64, 512], F32) for i in range(2)]
            t_osb = toks(2)
            oT = [self.sb(es, "oT%d" % i, [64, 8, 128], BF16) for i in range(2)]
            t_oT = toks(2)
            SB = (0, 1, 6, 7)

            def load_q(qb):
                qi = qb % 3
                qs = slice(qb * 128, (qb + 1) * 128)
                P.op("sp", lambda e: e.dma_start(out=q[qi][0:64], in_=self.zT_d[R_CQ:R_CQ + 512, qs].rearrange("(h d) t -> d h t", d=64)),
                     writes=[t_q[qi]], dma=True)

            def make(n, m, qb, g, n_, nkb, kb, mi):
                qi, ob = qb % 3, qb % 2
                bS, pb = SB[n % 4], n % 3
                bO, bD, r2 = 2 + m % 2, 4 + m % 2, m % 2
                psS, tS = self.ps[bS], self.pst[bS]
                qs = slice(qb * 128, (qb + 1) * 128)

                def s_fn():
                    if g == 0 and n_ == 0 and qb + 1 < 32:
                        load_q(qb + 1)
                    P.op("pe", lambda e: e.matmul(psS[:], lhsT=self.cb[:, 0, :], rhs=self.mrep[:, mi].rearrange("p h t -> p (h t)"),
                                                  start=True, stop=False), reads=[self.t_const], writes=[tS])
                    P.op("pe", lambda e: e.matmul(psS[:], lhsT=ckT[:, g, kb * 128:(kb + 1) * 128], rhs=q[qi][:, 4 * g:4 * g + 4, :],
                                                  start=False, stop=True), reads=[t_k, t_q[qi]], writes=[tS])
                    P.op("act", lambda e: e.activation(out=pT[pb][:], in_=psS[:], func=AF.Exp, scale=0.125),
                         reads=[tS], writes=[t_pT[pb]])

                def pv_fn():
                    P.op("pe", lambda e: e.matmul(self.ps[bO][:, :], lhsT=sap(cv, (kb * 2 + g) * 65, [[1, 128]]), rhs=pT[pb][:],
                                                  start=(n_ == 0), stop=(n_ == nkb - 1)), reads=[t_v, t_pT[pb]], writes=[self.pst[bO]])
                    P.op("pe", lambda e: e.matmul(self.ps[bD][:, :], lhsT=self.ones_bf[:, :], rhs=pT[pb][:], start=(n_ == 0),
                                                  stop=False), reads=[self.t_const, t_pT[pb]], writes=[self.pst[bD]])
                    if n_ == nkb - 1:
                        for hl in range(2):
                            P.op("pe", lambda e, hl=hl: e.matmul(self.ps[bD][:, :], lhsT=self.ones_bf[:, :],
                                                               rhs=skrow[:, hl, 4 * g:4 * g + 4, :].rearrange("p h t -> p (h t)"),
                                                               start=False, stop=(hl == 1)),
                                 reads=[self.t_const, t_sk], writes=[self.pst[bD]])
                        self.normalize_out2(self.ps[bO], self.ps[bD], self.pst[bO], self.pst[bD], 512, rc[r2], t_rc[r2],
                                            osb[r2], t_osb[r2], oT[ob][:, 4 * g:4 * g + 4, :].rearrange("p h t -> p (h t)"), t_oT[ob])
                        if g == 1:
                            P.op("sp", lambda e: e.dma_start(out=self.ocT_d[:, qs].rearrange("(h d) t -> d h t", d=64), in_=oT[ob][:]), reads=[t_oT[ob]], dma=True)
                return s_fn, pv_fn

            load_q(0)
            stages = []
            n = 0
            m = 0
            for qb in range(32):
                for g in range(2):
                    kbs = ([(qb - 1, 8)] if qb > 0 else []) + [(qb, 0)]
                    for n_, (kb, mi) in enumerate(kbs):
                        stages.append(make(n, m, qb, g, n_, len(kbs), kb, mi))
                        n += 1
                    m += 1
            self.run_pipe(stages)
            P.barrier()

    def phase_attnB(self, l):
        P = self.P
        with ExitStack() as es:
            bkT = self.sb(es, "bkT", [128, 12, L], BF16)
            bv = self.sb(es, "bv", [128, 33, 12, 65], BF16)
            t_k = Tok(); t_v = Tok()
            P.op("dve", lambda e: e.memset(bkT[64:128], 0.0), writes=[t_k])
            P.op("dve", lambda e: e.memset(bv[:, 32], 0.0), writes=[t_v])
            for g in range(3):
                P.op("sp", lambda e, g=g: e.dma_start(
                    out=bkT[0:64, 4 * g:4 * g + 4, :],
                    in_=self.zT_d[R_BK + 256 * g:R_BK + 256 * (g + 1), :].rearrange("(h d) t -> d h t", d=64)),
                    writes=[t_k], dma=True)
            for b4 in range(4):
                P.op("sp", lambda e, b4=b4: e.dma_start(
                    out=bv[:, 8 * b4:8 * b4 + 8],
                    in_=self.bv_d[1024 * b4:1024 * (b4 + 1)].rearrange("(b p) h e -> p b h e", p=128)),
                    writes=[t_v], dma=True)
            q = [self.sb(es, "bq%d" % i, [128, 12, 128], BF16) for i in range(3)]
            t_q = toks(3)
            for i in range(3):
                P.op("dve", lambda e, i=i: e.memset(q[i][64:128], 0.0), writes=[t_q[i]])
            pT = [self.sb(es, "pT%d" % i, [128, 4, 128], BF16) for i in range(3)]
            t_pT = toks(3)
            rc = [self.sb(es, "rc%d" % i, [64, 512], F32) for i in range(2)]
            t_rc = toks(2)
            osb = [self.sb(es, "osb%d" % i, [64, 512], F32) for i in range(2)]
            t_osb = toks(2)
            oT = [self.sb(es, "oT%d" % i, [64, 4, 128], BF16) for i in range(2)]
            t_oT = toks(2)

            def load_q(qb):
                qi = qb % 3
                qs = slice(qb * 128, (qb + 1) * 128)
                P.op("sp", lambda e: e.dma_start(out=q[qi][0:64], in_=self.zT_d[R_BQ:R_BQ + 768, qs].rearrange("(h d) t -> d h t", d=64)),
                     writes=[t_q[qi]], dma=True)

            def make(n, qb, n_, nit, g, kb, mi):
                qi, ob = qb % 3, qb % 2
                bS, pb = n % 4, n % 3
                bO, bD = 4 + qb % 2, 6 + qb % 2
                psS, tS = self.ps[bS], self.pst[bS]
                qs = slice(qb * 128, (qb + 1) * 128)
                last = (n_ == nit - 1)

                def s_fn():
                    if n_ == 0 and qb + 1 < 32:
                        load_q(qb + 1)
                    P.op("pe", lambda e: e.matmul(psS[:], lhsT=self.cb[:, 0, :], rhs=self.mrep[:, mi].rearrange("p h t -> p (h t)"),
                                                  start=True, stop=False), reads=[self.t_const], writes=[tS])
                    for j in range(4):
                        P.op("pe", lambda e, j=j: e.matmul(psS[:, j * 128:(j + 1) * 128], lhsT=bkT[:, 4 * g + j, kb * 128:(kb + 1) * 128],
                                                           rhs=q[qi][:, 4 * g + j, :], start=False, stop=(j == 3)),
                             reads=[t_k, t_q[qi]], writes=[tS])
                    P.op("act", lambda e: e.activation(out=pT[pb][:].rearrange("p h t -> p (h t)"), in_=psS[:], func=AF.Exp, scale=0.125),
                         reads=[tS], writes=[t_pT[pb]])

                def pv_fn():
                    if n_ == 0:
                        for bb in (bO, bD):
                            P.op("pe", lambda e, bb=bb: e.matmul(self.ps[bb][:, :], lhsT=self.zeros_bf[:, :],
                                                               rhs=self.mrep[:, 0].rearrange("p h t -> p (h t)"), start=True, stop=False),
                                 reads=[self.t_const], writes=[self.pst[bb]])
                    for j in range(4):
                        P.op("pe", lambda e, j=j: e.matmul(self.ps[bO][:, j * 128:(j + 1) * 128], lhsT=sap(bv, (kb * 12 + 4 * g + j) * 65, [[1, 128]]),
                                                           rhs=pT[pb][:, j, :], start=False, stop=(last and j == 3)),
                             reads=[t_v, t_pT[pb]], writes=[self.pst[bO]])
                    P.op("pe", lambda e: e.matmul(self.ps[bD][:, :], lhsT=self.ones_bf[:, :], rhs=pT[pb][:].rearrange("p h t -> p (h t)"),
                                                  start=False, stop=last), reads=[self.t_const, t_pT[pb]], writes=[self.pst[bD]])
                    if last:
                        self.normalize_out2(self.ps[bO], self.ps[bD], self.pst[bO], self.pst[bD], 512, rc[ob], t_rc[ob],
                                            osb[ob], t_osb[ob], oT[ob][:].rearrange("p h t -> p (h t)"), t_oT[ob])
                        P.op("sp", lambda e: e.dma_start(out=self.obT_d[:, qs].rearrange("(h d) t -> d h t", d=64), in_=oT[ob][:]), reads=[t_oT[ob]], dma=True)
                return s_fn, pv_fn

            load_q(0)
            stages = []
            n = 0
            for qb in range(32):
                items = []
                for g, Dl in enumerate((1, 4, 16)):
                    for o in range(Dl + 1):
                        kb = qb - o
                        if kb < 0:
                            break
                        if Dl == 1:
                            mi = 0 if o == 0 else 1
                        else:
                            base = 2 if Dl == 4 else 5
                            mi = base + (0 if o == 0 else (2 if o == Dl else 1))
                        items.append((g, kb, mi))
                for n_, (g, kb, mi) in enumerate(items):
                    stages.append(make(n, qb, n_, len(items), g, kb, mi))
                    n += 1
            self.run_pipe(stages)
            P.barrier()

    def phase_attnA(self, l):
        P = self.P
        with ExitStack() as es:
            kaT = self.sb(es, "kaT", [128, L], BF16)
            ikT = self.sb(es, "ikT", [128, L], BF16)
            av = self.sb(es, "av", [128, 34, 65], BF16)
            iw = self.sb(es, "iw", [128, 32, 8], F32)
            t_k = Tok(); t_ik = Tok(); t_v = Tok(); t_iw = Tok()
            P.op("dve", lambda e: e.memset(kaT[64:128], 0.0), writes=[t_k])
            P.op("dve", lambda e: e.memset(ikT[64:128], 0.0), writes=[t_ik])
            P.op("dve", lambda e: e.memset(av[:, 32:34], 0.0), writes=[t_v])
            P.op("sp", lambda e: e.dma_start(out=kaT[0:64], in_=self.zT_d[R_AKIK:R_AKIK + 64, :]), writes=[t_k], dma=True)
            P.op("sp", lambda e: e.dma_start(out=ikT[0:64], in_=self.zT_d[R_AKIK + 64:R_AKIK + 128, :]), writes=[t_ik], dma=True)
            P.op("sp", lambda e: e.dma_start(out=av[:, 0:32], in_=self.av_d.rearrange("(b p) e -> p b e", p=128)), writes=[t_v], dma=True)
            P.op("sp", lambda e: e.dma_start(out=iw[:], in_=self.iw_d.rearrange("(b p) h -> p b h", p=128)), writes=[t_iw], dma=True)
            aq = [self.sb(es, "aq%d" % i, [128, 6, 128], BF16) for i in range(3)]
            iq = [self.sb(es, "iq%d" % i, [128, 8, 128], BF16) for i in range(3)]
            t_aq = toks(3); t_iq = toks(3)
            for i in range(3):
                P.op("dve", lambda e, i=i: e.memset(aq[i][64:128], 0.0), writes=[t_aq[i]])
                P.op("dve", lambda e, i=i: e.memset(iq[i][64:128], 0.0), writes=[t_iq[i]])
            Dg = [self.sb(es, "Dg%d" % i, [128, 8, 128], BF16) for i in range(2)]
            t_Dg = toks(2)
            R = [self.sb(es, "R%d" % i, [128, 512], BF16) for i in range(16)]
            t_R = toks(16)
            score = [self.sb(es, "score%d" % i, [128, L], F32) for i in range(2)]
            t_sc = toks(2)
            junk = self.sb(es, "junk", [128, L], BF16)
            t_junk = Tok()
            negm = [self.sb(es, "negm%d" % i, [128, L], BF16) for i in range(2)]
            t_ng = toks(2)
            st = [self.sb(es, "st%d" % i, [128, 8], F32) for i in range(2)]
            thr = [self.sb(es, "thr%d" % i, [128, 2], F32) for i in range(2)]
            rtab = [self.sb(es, "rtab%d" % i, [128, N_BISECT], F32) for i in range(2)]
            t_st = toks(2)
            p2 = self.sb(es, "p2", [128, N_BISECT], F32)
            for i in range(N_BISECT):
                P.op("dve", lambda e, i=i: e.memset(p2[:, i:i + 1], 2.0 ** -(i + 1)), writes=[self.t_const])
            pT = [self.sb(es, "pT%d" % i, [128, 384], BF16) for i in range(4)]
            t_pT = toks(4)
            rc = [self.sb(es, "rc%d" % i, [64, 512], F32) for i in range(2)]
            t_rc = toks(2)
            osb = [self.sb(es, "osb%d" % i, [64, 512], F32) for i in range(2)]
            t_osb = toks(2)
            oT = [self.sb(es, "oT%d" % i, [64, 6, 128], BF16) for i in range(2)]
            t_oT = toks(2)
            ctr = {"ri": 0}
            SB = (3, 4, 7)

            def emit_idx(qb):
                qs = slice(qb * 128, (qb + 1) * 128)
                qi = qb % 3
                b2 = qb % 2
                nk = (qb + 1) * 128
                P.op("sp", lambda e: e.dma_start(out=aq[qi][0:64], in_=self.zT_d[R_AQ:R_AQ + 384, qs].rearrange("(h d) t -> d h t", d=64)),
                     writes=[t_aq[qi]], dma=True)
                ng, t_n = negm[b2], t_ng[b2]
                if qb < 2:
                    if qb == 1:
                        P.op("dve", lambda e: e.memset(ng[:, 0:128], 0.0), writes=[t_n])
                    P.op("dve", lambda e: e.tensor_copy(out=ng[:, qb * 128:(qb + 1) * 128], in_=self.cb[:, 1, :]),
                         reads=[self.t_const], writes=[t_n])
                    return
                P.op("sp", lambda e: e.dma_start(out=iq[qi][0:64], in_=self.zT_d[R_IQ:R_IQ + 512, qs].rearrange("(h d) t -> d h t", d=64)),
                     writes=[t_iq[qi]], dma=True)
                sc, t_s = score[b2], t_sc[b2]
                for h in range(8):
                    P.op("pool", lambda e, h=h: e.tensor_scalar(out=Dg[b2][:, h, :], in0=self.cb[:, 0, :], scalar1=iw[:, qb, h:h + 1],
                                                                scalar2=None, op0=ALU.mult),
                         reads=[self.t_const, t_iw], writes=[t_Dg[b2]])
                for c0 in range(0, nk, 512):
                    w = min(512, nk - c0)
                    lastc = (c0 + w == nk)
                    rbase = (ctr["ri"] % 2) * 8
                    ctr["ri"] += 1
                    for h in range(8):
                        bR = h % 2
                        rb = rbase + h
                        P.op("pe", lambda e, h=h, bR=bR, c0=c0, w=w: e.matmul(self.ps[bR][:, 0:w], lhsT=iq[qi][:, h, :], rhs=ikT[:, c0:c0 + w],
                                                                  start=True, stop=True),
                             reads=[t_iq[qi], t_ik], writes=[self.pst[bR]])
                        P.op("act", lambda e, bR=bR, rb=rb, w=w: e.activation(out=R[rb][:, 0:w], in_=self.ps[bR][:, 0:w], func=AF.Relu),
                             reads=[self.pst[bR]], writes=[t_R[rb]])
                    for h in range(8):
                        rb = rbase + h
                        P.op("pe", lambda e, h=h, rb=rb, w=w, lastc=lastc: e.matmul(self.ps[2][:, 0:w], lhsT=Dg[b2][:, h, :], rhs=R[rb][:, 0:w],
                                                                  start=(h == 0), stop=(h == 7 and not lastc)),
                             reads=[t_Dg[b2], t_R[rb]], writes=[self.pst[2]])
                    if lastc:
                        P.op("pe", lambda e, w=w: e.matmul(self.ps[2][:, w - 128:w], lhsT=self.cb[:, 0, :], rhs=self.cb[:, 1, :],
                                                           start=False, stop=True), reads=[self.t_const], writes=[self.pst[2]])
                    P.op("act", lambda e, c0=c0, w=w: e.activation(out=sc[:, c0:c0 + w], in_=self.ps[2][:, 0:w], func=AF.Copy),
                         reads=[self.pst[2]], writes=[t_s])
                S_, T_, RT = st[b2], thr[b2], rtab[b2]
                t_t = t_st[b2]
                P.op("dve", lambda e: e.tensor_reduce(out=S_[:, 0:1], in_=sc[:, 0:nk], axis=mybir.AxisListType.X, op=ALU.max),
                     reads=[t_s], writes=[t_t])
                P.op("dve", lambda e: e.tensor_reduce(out=S_[:, 1:2], in_=sc[:, 0:nk - 128], axis=mybir.AxisListType.X, op=ALU.min),
                     reads=[t_s], writes=[t_t])
                P.op("dve", lambda e: e.tensor_tensor(out=S_[:, 2:3], in0=S_[:, 0:1], in1=S_[:, 1:2], op=ALU.subtract),
                     reads=[t_t], writes=[t_t])
                P.op("dve", lambda e: e.tensor_scalar(out=RT[:], in0=p2[:], scalar1=S_[:, 2:3], scalar2=None, op0=ALU.mult),
                     reads=[t_t, self.t_const], writes=[t_t])
                P.op("dve", lambda e: e.scalar_tensor_tensor(out=T_[:, 0:1], in0=S_[:, 2:3], scalar=0.5, in1=S_[:, 1:2],
                                                             op0=ALU.mult, op1=ALU.add), reads=[t_t], writes=[t_t])
                cur = 0
                for i in range(N_BISECT):
                    P.op("dve", lambda e, cur=cur: e.tensor_scalar(
                        out=junk[:, 0:nk], in0=sc[:, 0:nk], scalar1=T_[:, cur:cur + 1], scalar2=None, op0=ALU.is_ge, op1=ALU.add,
                        accum_out=S_[:, 3:4]), reads=[t_s, t_t], writes=[t_t, t_junk])
                    P.op("dve", lambda e: e.tensor_scalar(out=S_[:, 4:5], in0=S_[:, 3:4], scalar1=255.5, scalar2=-0.5,
                                                          op0=ALU.is_ge, op1=ALU.add), reads=[t_t], writes=[t_t])
                    P.op("dve", lambda e, i=i, cur=cur: e.scalar_tensor_tensor(
                        out=T_[:, 1 - cur:2 - cur], in0=S_[:, 4:5], scalar=RT[:, i:i + 1], in1=T_[:, cur:cur + 1],
                        op0=ALU.mult, op1=ALU.add), reads=[t_t], writes=[t_t])
                    cur = 1 - cur
                P.op("dve", lambda e, cur=cur: e.tensor_scalar(out=ng[:, 0:nk], in0=sc[:, 0:nk], scalar1=T_[:, cur:cur + 1], scalar2=NEG,
                                                               op0=ALU.is_lt, op1=ALU.mult), reads=[t_s, t_t], writes=[t_n])

            def make(n, m, qb, half, kb):
                qi, ob, b2 = qb % 3, qb % 2, qb % 2
                ng, t_n = negm[b2], t_ng[b2]
                bS, pb = SB[n % 3], n % 4
                bO, bD, r2 = 5, 6, m % 2
                psS, tS = self.ps[bS], self.pst[bS]
                qs = slice(qb * 128, (qb + 1) * 128)

                def s_fn():
                    if half == 0 and kb == 0 and qb + 1 < 32:
                        emit_idx(qb + 1)
                    P.op("pe", lambda e: e.matmul(psS[:, 0:384], lhsT=ng[:, kb * 128:(kb + 1) * 128],
                                                  rhs=self.irep[:].rearrange("p h t -> p (h t)"), start=True, stop=False),
                         reads=[t_n, self.t_const], writes=[tS])
                    P.op("pe", lambda e: e.matmul(psS[:, 0:384], lhsT=kaT[:, kb * 128:(kb + 1) * 128],
                                                  rhs=aq[qi][:, 3 * half:3 * half + 3, :], start=False, stop=True),
                         reads=[t_k, t_aq[qi]], writes=[tS])
                    P.op("act", lambda e: e.activation(out=pT[pb][:], in_=psS[:, 0:384], func=AF.Exp, scale=0.125),
                         reads=[tS], writes=[t_pT[pb]])

                def pv_fn():
                    P.op("pe", lambda e: e.matmul(self.ps[bO][:, 0:384], lhsT=sap(av, kb * 65, [[1, 128]]), rhs=pT[pb][:], start=(kb == 0),
                                                  stop=(kb == qb)), reads=[t_v, t_pT[pb]], writes=[self.pst[bO]])
                    P.op("pe", lambda e: e.matmul(self.ps[bD][:, 0:384], lhsT=self.ones_bf[:, :], rhs=pT[pb][:], start=(kb == 0),
                                                  stop=(kb == qb)), reads=[self.t_const, t_pT[pb]], writes=[self.pst[bD]])
                    if kb == qb:
                        self.normalize_out2(self.ps[bO], self.ps[bD], self.pst[bO], self.pst[bD], 384, rc[r2], t_rc[r2],
                                            osb[r2], t_osb[r2],
                                            oT[ob][:, 3 * half:3 * half + 3, :].rearrange("p h t -> p (h t)"), t_oT[ob])
                        if half == 1:
                            P.op("sp", lambda e: e.dma_start(out=self.oaT_d[:, qs].rearrange("(h d) t -> d h t", d=64), in_=oT[ob][:]), reads=[t_oT[ob]], dma=True)
                return s_fn, pv_fn

            emit_idx(0)
            stages = []
            n = 0
            m = 0
            for qb in range(32):
                for half in range(2):
                    for kb in range(qb + 1):
                        stages.append(make(n, m, qb, half, kb))
                        n += 1
                    m += 1
            self.run_pipe(stages)
            P.barrier()

    def phase_M(self, l):
        P = self.P
        TW = 256
        with ExitStack() as es:
            Wa = self.sb(es, "Wa", [128, 3, D], BF16)
            Wb = self.sb(es, "Wb", [128, 2, D], BF16)
            Wc = self.sb(es, "Wc", [128, 4, D], BF16)
            Wg = self.sb(es, "Wg", [128, 8, 3 * D], BF16)
            Wo = self.sb(es, "Wo", [128, 8, D], BF16)
            t_wa, t_wb, t_wc, t_wo = Tok(), Tok(), Tok(), Tok()
            t_wg = toks(3)
            for (dst, src, tk) in ((Wa, self.w_a[l], t_wa), (Wb, self.w_b[l], t_wb), (Wc, self.w_c[l], t_wc)):
                P.op("pool", lambda e, dst=dst, src=src: e.dma_start(out=dst[:], in_=src.rearrange("(c p) m -> p c m", p=128)),
                     writes=[tk], dma=True)
            for i in range(3):
                self.load_w(Wg, self.w_in[l], C_G + i * D, D, i * D, t_wg[i])
            self.load_w(Wo, self.w_o[l], 0, D, 0, t_wo)
            oa = [self.sb(es, "oa%d" % i, [128, 3, TW], BF16) for i in range(2)]
            ob_ = [self.sb(es, "ob%d" % i, [128, 2, TW], BF16) for i in range(2)]
            oc = [self.sb(es, "oc%d" % i, [128, 4, TW], BF16) for i in range(2)]
            uT = [self.sb(es, "uTm%d" % i, [128, 8, TW], BF16) for i in range(2)]
            hT = [self.sb(es, "hTm%d" % i, [128, 8, TW], F32) for i in range(2)]
            t_in = toks(2)
            t_h = toks(2)
            sig = [self.sb(es, "sig%d" % i, [128, TW], F32) for i in range(2)]
            t_sig = toks(2)
            mm_ = [self.sb(es, "mm%d" % i, [128, TW], F32) for i in range(3)]
            t_mm = toks(3)
            mg = [self.sb(es, "mg%d" % i, [128, 8, TW], BF16) for i in range(2)]
            t_mg = toks(2)
            branches = ((Wa, oa, 3, t_wa), (Wb, ob_, 2, t_wb), (Wc, oc, 4, t_wc))
            k_ = 0
            for tc in range(L // TW):
                b = tc % 2
                tsl = slice(tc * TW, (tc + 1) * TW)
                P.op("sp", lambda e, b=b, tsl=tsl: e.dma_start(out=oa[b][:], in_=self.oaT_d[:, tsl].rearrange("(c p) t -> p c t", p=128)), writes=[t_in[b]], dma=True)
                P.op("sp", lambda e, b=b, tsl=tsl: e.dma_start(out=ob_[b][:], in_=self.obT_d[:, tsl].rearrange("(c p) t -> p c t", p=128)), writes=[t_in[b]], dma=True)
                P.op("sp", lambda e, b=b, tsl=tsl: e.dma_start(out=oc[b][:], in_=self.ocT_d[:, tsl].rearrange("(c p) t -> p c t", p=128)), writes=[t_in[b]], dma=True)
                P.op("sp", lambda e, b=b, tsl=tsl: e.dma_start(out=uT[b][:], in_=self.uT_d[:, :, tsl].rearrange("c p t -> p c t")),
                     writes=[t_in[b]], dma=True)
                P.op("sp", lambda e, b=b, tsl=tsl: e.dma_start(out=hT[b][:], in_=self.hT_d[:, :, tsl].rearrange("c p t -> p c t")),
                     writes=[t_h[b]], dma=True)
                for c in range(8):
                    cs = slice(c * 128, (c + 1) * 128)
                    for i, (Wi, oi, nh, t_wi) in enumerate(branches):
                        bY = (k_ % 2) * 2
                        bG = (k_ % 2) * 2 + 1
                        sb_ = k_ % 2
                        k_ += 1
                        for h in range(nh):
                            P.op("pe", lambda e, Wi=Wi, oi=oi, h=h, cs=cs, b=b, bY=bY, nh=nh: e.matmul(
                                self.ps[bY][:, 0:TW], lhsT=Wi[:, h, cs], rhs=oi[b][:, h, :], start=(h == 0), stop=(h == nh - 1)),
                                reads=[t_wi, t_in[b]], writes=[self.pst[bY]])
                        for k in range(8):
                            P.op("pe", lambda e, k=k, i=i, c=c, b=b, bG=bG: e.matmul(
                                self.ps[bG][:, 0:TW], lhsT=Wg[:, k, i * D + c * 128:i * D + (c + 1) * 128], rhs=uT[b][:, k, :],
                                start=(k == 0), stop=(k == 7)), reads=[t_wg[i], t_in[b]], writes=[self.pst[bG]])
                        P.op("act", lambda e, bG=bG, sb_=sb_: e.activation(out=sig[sb_][:], in_=self.ps[bG][:, 0:TW], func=AF.Sigmoid),
                             reads=[self.pst[bG]], writes=[t_sig[sb_]])
                        P.op("dve", lambda e, bY=bY, sb_=sb_, i=i: e.tensor_tensor(out=mm_[i][:], in0=self.ps[bY][:, 0:TW], in1=sig[sb_][:], op=ALU.mult),
                             reads=[self.pst[bY], t_sig[sb_]], writes=[t_mm[i]])
                    P.op("pool", lambda e: e.tensor_tensor(out=mm_[0][:], in0=mm_[0][:], in1=mm_[1][:], op=ALU.add),
                         reads=[t_mm[0], t_mm[1]], writes=[t_mm[0]])
                    P.op("pool", lambda e, b=b, c=c: e.tensor_tensor(out=mg[b][:, c, :], in0=mm_[0][:], in1=mm_[2][:], op=ALU.add),
                         reads=[t_mm[0], t_mm[2]], writes=[t_mg[b]])
                for c2 in range(8):
                    bD = 4 + c2 % 2
                    for c in range(8):
                        P.op("pe", lambda e, c=c, c2=c2, b=b, bD=bD: e.matmul(
                            self.ps[bD][:, 0:TW], lhsT=Wo[:, c, c2 * 128:(c2 + 1) * 128], rhs=mg[b][:, c, :], start=(c == 0), stop=(c == 7)),
                            reads=[t_wo, t_mg[b]], writes=[self.pst[bD]])
                    P.op("dve", lambda e, c2=c2, b=b, bD=bD: e.tensor_tensor(out=hT[b][:, c2, :], in0=self.ps[bD][:, 0:TW], in1=hT[b][:, c2, :], op=ALU.add),
                         reads=[self.pst[bD], t_h[b]], writes=[t_h[b]])
                P.op("sp", lambda e, b=b, tsl=tsl: e.dma_start(out=self.hT_d[:, :, tsl].rearrange("c p t -> p c t"), in_=hT[b][:]),
                     reads=[t_h[b]], dma=True)
            P.barrier()

    def phase_F(self, l):
        P = self.P
        for half in range(2):
            with ExitStack() as es:
                Wu = self.sb(es, "Wu", [128, 8, 2048], BF16)
                Wd = self.sb(es, "Wd", [128, 16, D], BF16)
                t_w = Tok()
                t_wd = Tok()
                self.load_w(Wu, self.w_up[l], half * 2048, 2048, 0, t_w)
                srcd = self.w_down[l][half * 2048:(half + 1) * 2048, :].rearrange("(kc p) m -> p kc m", p=128)
                P.op("pool", lambda e, srcd=srcd, Wd=Wd: e.dma_start(out=Wd[:], in_=srcd), writes=[t_wd], dma=True)
                hT = [self.sb(es, "hTf%d" % i, [128, 8, 512], F32) for i in range(2)]
                t_h = toks(2)
                u2 = [self.sb(es, "u2%d" % i, [128, 8, 512], BF16) for i in range(2)]
                t_u = toks(2)
                sq = [self.sb(es, "sqf%d" % i, [128, 8, 512], BF16) for i in range(2)]
                t_sq = toks(2)
                rs = [self.sb(es, "rsf%d" % i, [128, 512], F32) for i in range(2)]
                t_rs = toks(2)
                hid = [self.sb(es, "hid%d" % i, [128, 16, 512], BF16) for i in range(2)]
                t_hid = toks(2)
                rl = [self.sb(es, "rl%d" % i, [128, 512], F32) for i in range(2)]
                t_rl = toks(2)
                k_ = 0
                for tc in range(8):
                    b = tc % 2
                    tsl = slice(tc * 512, (tc + 1) * 512)
                    P.op("sp", lambda e, b=b, tsl=tsl: e.dma_start(out=hT[b][:], in_=self.hT_d[:, :, tsl].rearrange("c p t -> p c t")),
                         writes=[t_h[b]], dma=True)
                    if half == 0:
                        self.norm_chunk(None, hT[b], t_h[b], lambda c: self.g_mlp[:, l, c:c + 1],
                                        lambda c, b=b: u2[b][:, c, :], t_u[b], sq[b], t_sq[b], rs[b], t_rs[b], 6 + b, l)
                        P.op("sp", lambda e, b=b, tsl=tsl: e.dma_start(out=self.uT_d[:, :, tsl].rearrange("c p t -> p c t"), in_=u2[b][:]),
                             reads=[t_u[b]], dma=True)
                    else:
                        P.op("sp", lambda e, b=b, tsl=tsl: e.dma_start(out=u2[b][:], in_=self.uT_d[:, :, tsl].rearrange("c p t -> p c t")),
                             writes=[t_u[b]], dma=True)
                    for f in range(16):
                        bU = k_ % 4
                        rb = k_ % 2
                        k_ += 1
                        for k in range(8):
                            P.op("pe", lambda e, k=k, f=f, b=b, bU=bU: e.matmul(
                                self.ps[bU][:], lhsT=Wu[:, k, f * 128:(f + 1) * 128], rhs=u2[b][:, k, :], start=(k == 0), stop=(k == 7)),
                                reads=[t_w, t_u[b]], writes=[self.pst[bU]])
                        P.op("act", lambda e, bU=bU, rb=rb: e.activation(out=rl[rb][:], in_=self.ps[bU][:], func=AF.Relu),
                             reads=[self.pst[bU]], writes=[t_rl[rb]])
                        eng = "dve" if f % 2 == 0 else "pool"
                        P.op(eng, lambda e, rb=rb, b=b, f=f: e.tensor_tensor(out=hid[b][:, f, :], in0=rl[rb][:], in1=rl[rb][:], op=ALU.mult),
                             reads=[t_rl[rb]], writes=[t_hid[b]])
                    for c2 in range(8):
                        bD = 4 + c2 % 2
                        for f in range(16):
                            P.op("pe", lambda e, f=f, c2=c2, b=b, bD=bD: e.matmul(
                                self.ps[bD][:], lhsT=Wd[:, f, c2 * 128:(c2 + 1) * 128], rhs=hid[b][:, f, :], start=(f == 0), stop=(f == 15)),
                                reads=[t_wd, t_hid[b]], writes=[self.pst[bD]])
                        P.op("dve", lambda e, c2=c2, b=b, bD=bD: e.tensor_tensor(out=hT[b][:, c2, :], in0=self.ps[bD][:], in1=hT[b][:, c2, :], op=ALU.add),
                             reads=[self.pst[bD], t_h[b]], writes=[t_h[b]])
                    P.op("sp", lambda e, b=b, tsl=tsl: e.dma_start(out=self.hT_d[:, :, tsl].rearrange("c p t -> p c t"), in_=hT[b][:]),
                         reads=[t_h[b]], dma=True)
                if half == 1 and l == 0:
                    self.dump("d_hid", hid[0][:], [128, 16, 512], BF16, t_hid[0])
                    self.dump("d_wu", Wu[:], [128, 8, 2048], BF16, t_w)
                    self.dump("d_wd", Wd[:], [128, 16, D], BF16, t_w)
                    self.dump("d_u2", u2[0][:], [128, 8, 512], BF16, t_u[0])
                P.barrier()

    def phase_O(self):
        P = self.P
        with ExitStack() as es:
            hT = [self.sb(es, "hTo%d" % i, [128, 8, 512], F32) for i in range(2)]
            t_h = toks(2)
            y = [self.sb(es, "yo%d" % i, [128, 8, 512], F32) for i in range(2)]
            t_y = toks(2)
            sq = [self.sb(es, "sqo%d" % i, [128, 8, 512], BF16) for i in range(2)]
            t_sq = toks(2)
            rs = [self.sb(es, "rso%d" % i, [128, 512], F32) for i in range(2)]
            t_rs = toks(2)
            ot = [self.sb(es, "ot%d" % i, [128, D], F32) for i in range(3)]
            t_ot = toks(3)
            oi = 0
            for tc in range(8):
                b = tc % 2
                tsl = slice(tc * 512, (tc + 1) * 512)
                P.op("sp", lambda e, b=b, tsl=tsl: e.dma_start(out=hT[b][:], in_=self.hT_d[:, :, tsl].rearrange("c p t -> p c t")),
                     writes=[t_h[b]], dma=True)
                self.norm_chunk(None, hT[b], t_h[b], lambda c: self.g_fin[:, c:c + 1],
                                lambda c, b=b: y[b][:, c, :], t_y[b], sq[b], t_sq[b], rs[b], t_rs[b], 6 + b, 0)
                for j in range(4):
                    o3 = oi % 3
                    oi += 1
                    for hh in range(2):
                        pb = (2 * j + hh) % 4
                        for q in range(4):
                            c = hh * 4 + q
                            P.op("pe", lambda e, b=b, c=c, j=j, q=q, pb=pb: e.transpose(
                                out=self.ps[pb][:, q * 128:(q + 1) * 128], in_=y[b][:, c, j * 128:(j + 1) * 128], identity=self.ident_f),
                                reads=[t_y[b], self.t_const], writes=[self.pst[pb]])
                        if hh == 0:
                            P.op("dve", lambda e, o3=o3, pb=pb: e.tensor_copy(out=ot[o3][:, 0:512], in_=self.ps[pb][:]),
                                 reads=[self.pst[pb]], writes=[t_ot[o3]])
                        else:
                            P.op("act", lambda e, o3=o3, pb=pb: e.activation(out=ot[o3][:, 512:1024], in_=self.ps[pb][:], func=AF.Copy),
                                 reads=[self.pst[pb]], writes=[t_ot[o3]])
                    r0 = tc * 512 + j * 128
                    P.op("sp", lambda e, o3=o3, r0=r0: e.dma_start(out=self.out[r0:r0 + 128, :], in_=ot[o3][:]),
                         reads=[t_ot[o3]], dma=True)
            P.barrier()


def make_consts():
    p = np.arange(128)
    half = 32
    inv = (10000.0 ** (-(np.arange(half, dtype=np.float32)) / half)).astype(np.float32)
    cvec = np.zeros((128, 4), np.float32)
    cvec[:, 0] = inv[p % 32]
    cvec[:, 1] = np.where((p % 64) < 32, -1.0, 1.0)
    cmat = np.zeros((128, 6, 128), np.float32)
    cmat[:, 0, :] = np.eye(128, dtype=np.float32)
    r = np.arange(128)[:, None]
    c = np.arange(128)[None, :]
    cmat[:, 1, :] = np.where(c > r, NEG, 0.0)
    cmat[:, 2, :] = np.where(r > c, NEG, 0.0)
    cmat[:, 3, :] = np.where(r < c, NEG, 0.0)
    partner = np.where((p % 64) < 32, p + 32, p - 32)
    cmat[partner, 5, p] = 1.0
    cmat[64:, 4, :] = 1.0
    cvec[:, 2] = (p >= 64).astype(np.float32)
    cvec[:, 3] = (p < 64).astype(np.float32)
    masks = np.zeros((128, 9, 128), np.float32)
    masks[:, 0, :] = np.where(r > c, NEG, 0.0)
    masks[:, 1, :] = np.where(r < c, NEG, 0.0)
    for base, dl in ((2, 4), (5, 16)):
        res = ((c - r) % dl) == 0
        masks[:, base + 0, :] = np.where(res & (r <= c), 0.0, NEG)
        masks[:, base + 1, :] = np.where(res, 0.0, NEG)
        masks[:, base + 2, :] = np.where(res & (c <= r), 0.0, NEG)
    masks[:, 8, :] = np.where(r <= c, NEG, 0.0)
    return cvec, cmat, masks


def build_inputs(inputs, b):
    cvec, cmat, masks = make_consts()
    m = {
        "x": np.ascontiguousarray(inputs["x"][b]),
        "pos": np.ascontiguousarray(inputs["positions"][b]).astype(np.int32),
        "cvec": cvec, "cmat": cmat, "masks": masks,
    }
    for k in ("attn_norm", "w_in", "idx_k_norm", "sinks", "w_a", "w_b", "w_c", "w_o", "mlp_norm", "w_up",
              "w_down", "final_norm"):
        m[k] = np.ascontiguousarray(np.asarray(inputs[k], dtype=np.float32))
    return m


def kernel(**inputs):
    bld = Builder()
    nc = bld.build()
    n = 8
    in_maps = [build_inputs(inputs, b) for b in range(n)]
    res = run_bass_kernel_spmd(nc, in_maps, core_ids=list(range(n)))
    return np.stack([r["out"] for r in res.results], axis=0)
```
